# Optimizing a Trainium2 kernel written in Bass

```python
import math
import jax, jax.numpy as jnp
from jax import lax
import numpy as np

D_MODEL = 1024
BATCH = 8
SEQ = 4096
DEPTH = 2

MEM_LEN = 256
NORM_EPS = 1e-6
N_BRANCH = 4
BRANCH_WIDTH = 256

MLA_HEADS = 4
MLA_Q_RANK = 256
MLA_KV_RANK = 128
MLA_NOPE = 64
MLA_ROPE = 32
MLA_V = 64
ROPE_THETA = 10000.0
ATTN_BLOCK = 128

SG_GROUPS = 4
SG_WIDTH = 256
SG_CHUNK = 128

GLA_HEADS = 4
GLA_DK = 32
GLA_DV = 64
GLA_GATE_RANK = 16
GLA_GATE_TEMP = 16.0
GLA_CHUNK = 64

DN_HEADS = 4
DN_DK = 64
DN_DV = 64
DN_CONV = 4
DN_CHUNK = 64

XATTN_HEADS = 4
XATTN_DIM = D_MODEL // XATTN_HEADS
MLP_HIDDEN = 4 * D_MODEL

IN_SIZES = (
    MLA_Q_RANK, MLA_KV_RANK, MLA_ROPE,
    2 * SG_WIDTH,
    GLA_HEADS * GLA_DK, GLA_HEADS * GLA_DK, GLA_HEADS * GLA_DV,
    GLA_GATE_RANK, GLA_HEADS * GLA_DV,
    DN_HEADS * DN_DK, DN_HEADS * DN_DK, DN_HEADS * DN_DV,
    DN_HEADS, DN_HEADS, DN_HEADS * DN_DV,
    N_BRANCH * D_MODEL,
)
IN_WIDTH = sum(IN_SIZES)

kernel_name = 'hybrid_parallel_mixer_decoder'


def rmsnorm(x, g, eps=NORM_EPS):
    xf = x.astype(jnp.float32)
    y = xf * lax.rsqrt(jnp.mean(xf * xf, axis=-1, keepdims=True) + eps)
    return (y * g).astype(x.dtype)


def layernorm(x, g, b, eps=NORM_EPS):
    xf = x.astype(jnp.float32)
    mu = jnp.mean(xf, axis=-1, keepdims=True)
    xc = xf - mu
    y = xc * lax.rsqrt(jnp.mean(xc * xc, axis=-1, keepdims=True) + eps)
    return (y * g + b).astype(x.dtype)


def l2norm(x, eps=NORM_EPS):
    return x * lax.rsqrt(jnp.sum(x * x, axis=-1, keepdims=True) + eps)


def rope_angles(positions, dim):
    inv_freq = ROPE_THETA ** (-jnp.arange(0, dim, 2, dtype=jnp.float32) / dim)
    ang = positions.astype(jnp.float32)[..., None] * inv_freq
    return jnp.cos(ang), jnp.sin(ang)


def apply_rope(x, cos, sin):
    half = x.shape[-1] // 2
    x1 = x[..., :half].astype(jnp.float32)
    x2 = x[..., half:].astype(jnp.float32)
    return jnp.concatenate([x1 * cos - x2 * sin, x2 * cos + x1 * sin], axis=-1).astype(x.dtype)


def to_chunks(t, chunk):
    b, s, h = t.shape[:3]
    t = t.reshape((b, s // chunk, chunk, h) + t.shape[3:])
    return t.transpose((1, 0, 3, 2) + tuple(range(4, t.ndim)))


def from_chunks(t):
    n, b, h, c = t.shape[:4]
    t = t.transpose((1, 0, 3, 2) + tuple(range(4, t.ndim)))
    return t.reshape((b, n * c, h) + t.shape[4:])


def causal_block_attention(q, k, v, scale):
    s_len = q.shape[2]
    outs = []
    for start in range(0, s_len, ATTN_BLOCK):
        end = start + ATTN_BLOCK
        s = jnp.einsum('bhqd,bhkd->bhqk', q[:, :, start:end], k[:, :, :end]).astype(jnp.float32) * scale
        qi = start + jnp.arange(ATTN_BLOCK)
        ki = jnp.arange(end)
        s = jnp.where(qi[:, None] >= ki[None, :], s, -jnp.inf)
        p = jax.nn.softmax(s, axis=-1).astype(v.dtype)
        outs.append(jnp.einsum('bhqk,bhkd->bhqd', p, v[:, :, :end]))
    return jnp.concatenate(outs, axis=2)


def mla_branch(cq, ckv, kpe, positions, norm_q, norm_kv, w_uq, w_ukv):
    b, s, _ = cq.shape
    h = MLA_HEADS
    q = (rmsnorm(cq, norm_q) @ w_uq).reshape(b, s, h, MLA_NOPE + MLA_ROPE)
    kv = (rmsnorm(ckv, norm_kv) @ w_ukv).reshape(b, s, h, MLA_NOPE + MLA_V)
    q_nope, q_pe = q[..., :MLA_NOPE], q[..., MLA_NOPE:]
    k_nope, v = kv[..., :MLA_NOPE], kv[..., MLA_NOPE:]
    cos, sin = rope_angles(positions, MLA_ROPE)
    q_pe = apply_rope(q_pe, cos[:, :, None, :], sin[:, :, None, :])
    k_pe = apply_rope(kpe, cos, sin)
    q = jnp.concatenate([q_nope, q_pe], axis=-1).transpose(0, 2, 1, 3)
    k = jnp.concatenate([k_nope, jnp.broadcast_to(k_pe[:, :, None, :], (b, s, h, MLA_ROPE))],
                        axis=-1).transpose(0, 2, 1, 3)
    v = v.transpose(0, 2, 1, 3)
    o = causal_block_attention(q, k, v, (MLA_NOPE + MLA_ROPE) ** -0.5)
    return o.transpose(0, 2, 1, 3).reshape(b, s, h * MLA_V)


def spatial_gating_branch(uv, ln_g, ln_b, w_s, b_s):
    b, s, _ = uv.shape
    uv = jax.nn.gelu(uv)
    u, v = uv[..., :SG_WIDTH], uv[..., SG_WIDTH:]
    v = layernorm(v, ln_g, ln_b)
    n = s // SG_CHUNK
    v = v.reshape(b, n, SG_CHUNK, SG_GROUPS, SG_WIDTH // SG_GROUPS)
    mask = jnp.tril(jnp.ones((SG_CHUNK, SG_CHUNK), dtype=w_s.dtype))
    mixed = jnp.einsum('gij,bnjgc->bnigc', w_s * mask, v) + b_s.T[:, :, None]
    return u * mixed.reshape(b, s, SG_WIDTH)


def gla_branch(q, k, v, gate_lr, out_gate, w_gate, b_gate, norm_o):
    b, s, _ = q.shape
    h, c = GLA_HEADS, GLA_CHUNK
    f32 = jnp.float32
    log_a = jax.nn.log_sigmoid((gate_lr @ w_gate + b_gate).astype(f32)) / GLA_GATE_TEMP
    q = q.astype(f32).reshape(b, s, h, GLA_DK) * (GLA_DK ** -0.5)
    k = k.astype(f32).reshape(b, s, h, GLA_DK)
    v = v.astype(f32).reshape(b, s, h, GLA_DV)
    log_a = log_a.reshape(b, s, h, GLA_DK)
    qc, kc, vc, ac = (to_chunks(t, c) for t in (q, k, v, log_a))
    idx = jnp.arange(c)
    incl = (idx[:, None] >= idx[None, :])[:, :, None]

    def step(state, inp):
        qi, ki, vi, ai = inp
        cum = jnp.cumsum(ai, axis=2)
        diff = cum[:, :, :, None, :] - cum[:, :, None, :, :]
        decay = jnp.where(incl, jnp.exp(jnp.where(incl, diff, 0.0)), 0.0)
        scores = jnp.einsum('bhik,bhjk,bhijk->bhij', qi, ki, decay)
        o = (jnp.einsum('bhik,bhkv->bhiv', qi * jnp.exp(cum), state)
             + jnp.einsum('bhij,bhjv->bhiv', scores, vi))
        last = cum[:, :, -1:, :]
        state = (jnp.exp(last[:, :, 0, :])[..., None] * state
                 + jnp.einsum('bhjk,bhjv->bhkv', ki * jnp.exp(last - cum), vi))
        return state, o

    state0 = jnp.zeros((b, h, GLA_DK, GLA_DV), f32)
    _, o = lax.scan(step, state0, (qc, kc, vc, ac))
    o = from_chunks(o)
    o = rmsnorm(o, norm_o) * jax.nn.silu(out_gate.astype(f32).reshape(b, s, h, GLA_DV))
    return o.reshape(b, s, h * GLA_DV)


def causal_depthwise_conv(x, w):
    return lax.conv_general_dilated(x, w[:, None, :], window_strides=(1,),
                                    padding=[(DN_CONV - 1, 0)],
                                    dimension_numbers=('NWC', 'WIO', 'NWC'),
                                    feature_group_count=x.shape[-1])


def deltanet_branch(q, k, v, a, bt, z, conv_w, a_log, dt_bias, norm_o):
    b, s, _ = q.shape
    h, c = DN_HEADS, DN_CHUNK
    f32 = jnp.float32
    qkv = jax.nn.silu(causal_depthwise_conv(jnp.concatenate([q, k, v], axis=-1), conv_w))
    q, k, v = jnp.split(qkv, [h * DN_DK, 2 * h * DN_DK], axis=-1)
    q = l2norm(q.astype(f32).reshape(b, s, h, DN_DK)) * (DN_DK ** -0.5)
    k = l2norm(k.astype(f32).reshape(b, s, h, DN_DK))
    v = v.astype(f32).reshape(b, s, h, DN_DV)
    beta = jax.nn.sigmoid(bt.astype(f32))
    g = -jnp.exp(a_log.astype(f32)) * jax.nn.softplus(a.astype(f32) + dt_bias.astype(f32))
    qc, kc, vc = (to_chunks(t, c) for t in (q, k, v))
    beta_c, g_c = to_chunks(beta, c), to_chunks(g, c)
    gam = jnp.cumsum(g_c, axis=-1)
    idx = jnp.arange(c)
    incl = idx[:, None] >= idx[None, :]
    strict = idx[:, None] > idx[None, :]
    diff = gam[..., :, None] - gam[..., None, :]
    dec_incl = jnp.where(incl, jnp.exp(jnp.where(incl, diff, 0.0)), 0.0)
    dec_strict = jnp.where(strict, dec_incl, 0.0)
    kk = jnp.einsum('nbhid,nbhjd->nbhij', kc, kc)
    lower = jnp.eye(c, dtype=f32) + beta_c[..., :, None] * kk * dec_strict
    rhs = jnp.concatenate([beta_c[..., None] * vc, (beta_c * jnp.exp(gam))[..., None] * kc], axis=-1)
    sol = lax.linalg.triangular_solve(lower, rhs, left_side=True, lower=True, unit_diagonal=True)
    w_c, x_c = sol[..., :DN_DV], sol[..., DN_DV:]
    attn = jnp.einsum('nbhid,nbhjd->nbhij', qc, kc) * dec_incl
    q_dec = qc * jnp.exp(gam)[..., None]
    k_dec = kc * jnp.exp(gam[..., -1:] - gam)[..., None]
    g_last = jnp.exp(gam[..., -1])

    def step(state, inp):
        w_i, x_i, attn_i, qd_i, kd_i, gl_i = inp
        u = w_i - jnp.einsum('bhck,bhkv->bhcv', x_i, state)
        o = jnp.einsum('bhck,bhkv->bhcv', qd_i, state) + jnp.einsum('bhij,bhjv->bhiv', attn_i, u)
        state = gl_i[..., None, None] * state + jnp.einsum('bhjk,bhjv->bhkv', kd_i, u)
        return state, o

    state0 = jnp.zeros((b, h, DN_DK, DN_DV), f32)
    _, o = lax.scan(step, state0, (w_c, x_c, attn, q_dec, k_dec, g_last))
    o = from_chunks(o)
    o = rmsnorm(o, norm_o) * jax.nn.silu(z.astype(f32).reshape(b, s, h, DN_DV))
    return o.reshape(b, s, h * DN_DV)


def mixer_block(h, positions, w_in, mla_norm_q, mla_norm_kv, mla_w_uq, mla_w_ukv,
                sg_ln_g, sg_ln_b, sg_w, sg_b, gla_w_gate, gla_b_gate, gla_norm,
                dn_conv, dn_a_log, dn_dt_bias, dn_norm, w_branch, w_out):
    b, s, _ = h.shape
    zin = h @ w_in
    splits = [int(i) for i in np.cumsum(IN_SIZES)[:-1]]
    (cq, ckv, kpe, sg_uv, gq, gk, gv, g_lr, g_og,
     dq, dk, dv, da, db, dz, gates) = jnp.split(zin, splits, axis=-1)
    o_a = mla_branch(cq, ckv, kpe, positions, mla_norm_q, mla_norm_kv, mla_w_uq, mla_w_ukv)
    o_b = spatial_gating_branch(sg_uv, sg_ln_g, sg_ln_b, sg_w, sg_b)
    o_c = gla_branch(gq, gk, gv, g_lr, g_og, gla_w_gate, gla_b_gate, gla_norm)
    o_d = deltanet_branch(dq, dk, dv, da, db, dz, dn_conv, dn_a_log, dn_dt_bias, dn_norm)
    branches = jnp.stack([o.astype(h.dtype) for o in (o_a, o_b, o_c, o_d)], axis=2)
    proj = jnp.einsum('bsnw,nwd->bsnd', branches, w_branch)
    gate = jax.nn.sigmoid(gates).reshape(b, s, N_BRANCH, D_MODEL)
    merged = jnp.sum(gate * proj, axis=2)
    return merged @ w_out


def cross_attention(h, mem_n, wq, wk, wv, wo):
    b, s, _ = h.shape
    m = mem_n.shape[1]
    q = (h @ wq).reshape(b, s, XATTN_HEADS, XATTN_DIM)
    k = (mem_n @ wk).reshape(b, m, XATTN_HEADS, XATTN_DIM)
    v = (mem_n @ wv).reshape(b, m, XATTN_HEADS, XATTN_DIM)
    sc = jnp.einsum('bshd,bmhd->bhsm', q, k).astype(jnp.float32) * (XATTN_DIM ** -0.5)
    p = jax.nn.softmax(sc, axis=-1).astype(v.dtype)
    o = jnp.einsum('bhsm,bmhd->bshd', p, v).reshape(b, s, XATTN_HEADS * XATTN_DIM)
    return o @ wo


def squared_relu_mlp(h, w_up, w_down):
    a = jax.nn.relu(h @ w_up)
    return (a * a) @ w_down


def setup_inputs(seed: int = 0) -> dict:
    key = jax.random.key(seed)
    ks = iter(jax.random.split(key, 48))
    f32 = jnp.float32
    L = DEPTH

    def nrm(shape, scale):
        return jax.random.normal(next(ks), shape, f32) * scale

    def gain(shape):
        return 1.0 + 0.1 * jax.random.normal(next(ks), shape, f32)

    x = nrm((BATCH, SEQ, D_MODEL), 1.0)
    mem = nrm((BATCH, MEM_LEN, D_MODEL), 1.0)
    offsets = jax.random.randint(next(ks), (BATCH, 1), 0, 1024, dtype=jnp.int32)
    positions = (offsets + jnp.arange(SEQ, dtype=jnp.int32)[None, :]).astype(jnp.int32)
    dn_ch = 2 * DN_HEADS * DN_DK + DN_HEADS * DN_DV
    a_init = jax.random.uniform(next(ks), (L, DN_HEADS), f32, 1.0, 16.0)
    dt = jnp.exp(jax.random.uniform(next(ks), (L, DN_HEADS), f32, math.log(1e-3), math.log(1e-1)))
    return {
        'x': x,
        'mem': mem,
        'positions': positions,
        'norm_mix': gain((L, D_MODEL)),
        'w_in': nrm((L, D_MODEL, IN_WIDTH), D_MODEL ** -0.5),
        'mla_norm_q': gain((L, MLA_Q_RANK)),
        'mla_norm_kv': gain((L, MLA_KV_RANK)),
        'mla_w_uq': nrm((L, MLA_Q_RANK, MLA_HEADS * (MLA_NOPE + MLA_ROPE)), MLA_Q_RANK ** -0.5),
        'mla_w_ukv': nrm((L, MLA_KV_RANK, MLA_HEADS * (MLA_NOPE + MLA_V)), MLA_KV_RANK ** -0.5),
        'sg_ln_g': gain((L, SG_WIDTH)),
        'sg_ln_b': nrm((L, SG_WIDTH), 0.02),
        'sg_w': nrm((L, SG_GROUPS, SG_CHUNK, SG_CHUNK), SG_CHUNK ** -0.5),
        'sg_b': 1.0 + nrm((L, SG_GROUPS, SG_CHUNK), 0.1),
        'gla_w_gate': nrm((L, GLA_GATE_RANK, GLA_HEADS * GLA_DK), GLA_GATE_RANK ** -0.5),
        'gla_b_gate': nrm((L, GLA_HEADS * GLA_DK), 0.1),
        'gla_norm': gain((L, GLA_DV)),
        'dn_conv': nrm((L, DN_CONV, dn_ch), DN_CONV ** -0.5),
        'dn_a_log': jnp.log(a_init),
        'dn_dt_bias': dt + jnp.log(-jnp.expm1(-dt)),
        'dn_norm': gain((L, DN_DV)),
        'w_branch': nrm((L, N_BRANCH, BRANCH_WIDTH, D_MODEL), BRANCH_WIDTH ** -0.5),
        'w_out': nrm((L, D_MODEL, D_MODEL), D_MODEL ** -0.5),
        'norm_xattn': gain((L, D_MODEL)),
        'norm_mem': gain((L, D_MODEL)),
        'xattn_wq': nrm((L, D_MODEL, XATTN_HEADS * XATTN_DIM), D_MODEL ** -0.5),
        'xattn_wk': nrm((L, D_MODEL, XATTN_HEADS * XATTN_DIM), D_MODEL ** -0.5),
        'xattn_wv': nrm((L, D_MODEL, XATTN_HEADS * XATTN_DIM), D_MODEL ** -0.5),
        'xattn_wo': nrm((L, XATTN_HEADS * XATTN_DIM, D_MODEL), D_MODEL ** -0.5),
        'norm_mlp': gain((L, D_MODEL)),
        'w_up': nrm((L, D_MODEL, MLP_HIDDEN), D_MODEL ** -0.5),
        'w_down': nrm((L, MLP_HIDDEN, D_MODEL), MLP_HIDDEN ** -0.5),
        'norm_final': gain((D_MODEL,)),
    }


def reference(x, mem, positions, norm_mix, w_in, mla_norm_q, mla_norm_kv, mla_w_uq, mla_w_ukv,
              sg_ln_g, sg_ln_b, sg_w, sg_b, gla_w_gate, gla_b_gate, gla_norm,
              dn_conv, dn_a_log, dn_dt_bias, dn_norm, w_branch, w_out,
              norm_xattn, norm_mem, xattn_wq, xattn_wk, xattn_wv, xattn_wo,
              norm_mlp, w_up, w_down, norm_final):
    for l in range(DEPTH):
        h = rmsnorm(x, norm_mix[l])
        x = x + mixer_block(h, positions, w_in[l], mla_norm_q[l], mla_norm_kv[l], mla_w_uq[l], mla_w_ukv[l],
                            sg_ln_g[l], sg_ln_b[l], sg_w[l], sg_b[l], gla_w_gate[l], gla_b_gate[l], gla_norm[l],
                            dn_conv[l], dn_a_log[l], dn_dt_bias[l], dn_norm[l], w_branch[l], w_out[l])
        x = x + cross_attention(rmsnorm(x, norm_xattn[l]), rmsnorm(mem, norm_mem[l]),
                                xattn_wq[l], xattn_wk[l], xattn_wv[l], xattn_wo[l])
        x = x + squared_relu_mlp(rmsnorm(x, norm_mlp[l]), w_up[l], w_down[l])
    return rmsnorm(x, norm_final)
```

```python
from contextlib import ExitStack
import numpy as np
import concourse.bass as bass
import concourse.mybir as mybir
from concourse.bass_utils import run_bass_kernel_spmd

F32 = mybir.dt.float32
BF16 = mybir.dt.bfloat16
I32 = mybir.dt.int32
AF = mybir.ActivationFunctionType
ALU = mybir.AluOpType

D = 1024
SEQ = 4096
NL = 2
TT = 512
NT = SEQ // TT
EPS = 1e-6
HID = 4096
INW = 6840
DBG = 9
DNSTAGE = 9
DNV = ''


class Sched:
    ENGS = ("pe", "act", "dve", "pool", "sp")
    EPOCH = 12000
    RING = 8

    def __init__(self, nc):
        self.nc = nc
        self.stream = {e: [] for e in self.ENGS}
        self.vt = {e: 0 for e in self.ENGS}
        self.known = {e: {} for e in self.ENGS}
        self.need_sig = {e: set() for e in self.ENGS}
        self.last_write = {}
        self.readers = {}
        self.op_sig = []
        self.op_clock = []
        self.op_eng = []
        self.op_dma = []
        self.dma_n = {e: 0 for e in self.ENGS}
        self.last_sig = {}

    def _emit_wait(self, eng, key, val):
        self.stream[eng].append(("wait", key, val))
        if key[0] == "eng":
            self.need_sig[key[1]].add(val)

    def _wait(self, eng, dep):
        key, val = self.op_sig[dep]
        kn = self.known[eng]
        if kn.get(key, 0) >= val:
            return
        self._emit_wait(eng, key, val)
        for k, v in self.op_clock[dep].items():
            if kn.get(k, 0) < v:
                kn[k] = v
        kn[key] = max(kn.get(key, 0), val)

    def add(self, eng, fn, reads=(), writes=(), signal=True, dma=False):
        deps = set()
        for r in reads:
            w = self.last_write.get(r)
            if w is not None:
                deps.add(w)
        for w_ in writes:
            w = self.last_write.get(w_)
            if w is not None:
                deps.add(w)
            for r in self.readers.get(w_, ()):
                deps.add(r)
        opid = len(self.op_sig)
        for d in sorted(deps):
            if (not dma) and (not self.op_dma[d]) and self.op_eng[d] == eng:
                if eng == "pe":
                    continue
                israw = any(self.last_write.get(r) == d for r in reads)
                if not israw:
                    continue
            self._wait(eng, d)
        if dma:
            n = self.dma_n[eng]
            self.dma_n[eng] += 1
            slot = n % self.RING
            key = ("dma", eng, slot)
            val = 16 * (n // self.RING + 1)
            if val > 16:
                kn = self.known[eng]
                if kn.get(key, 0) < val - 16:
                    self._emit_wait(eng, key, val - 16)
                    kn[key] = val - 16
            self.stream[eng].append(("op", fn, None, key))
        else:
            self.vt[eng] += 1
            key = ("eng", eng)
            val = self.vt[eng]
            self.stream[eng].append(("op", fn, val, None))
        clock = dict(self.known[eng])
        clock[key] = val
        self.op_sig.append((key, val))
        self.op_clock.append(clock)
        self.op_eng.append(eng)
        self.op_dma.append(dma)
        self.last_sig[key] = max(self.last_sig.get(key, 0), val)
        for r in reads:
            self.readers.setdefault(r, []).append(opid)
        for w_ in writes:
            self.last_write[w_] = opid
            self.readers[w_] = []
        return opid

    def barrier(self):
        for eng in self.ENGS:
            kn = self.known[eng]
            for key, val in list(self.last_sig.items()):
                if kn.get(key, 0) < val:
                    self._emit_wait(eng, key, val)
                    kn[key] = val
        self.last_write = {}
        self.readers = {}

    def emit(self):
        nc = self.nc
        vmap = {}
        semkeys = set()
        for e in self.ENGS:
            cnt = 0
            ep = 0
            for vt in sorted(self.need_sig[e]):
                if cnt >= self.EPOCH:
                    ep += 1
                    cnt = 0
                cnt += 1
                vmap[(e, vt)] = (("eng", e, ep), cnt)
                semkeys.add(("eng", e, ep))
            for it in self.stream[e]:
                if it[0] == "op" and it[3] is not None:
                    semkeys.add(it[3])
        with ExitStack() as es:
            sems = {}
            for key in sorted(semkeys, key=str):
                sems[key] = es.enter_context(nc.semaphore("s_" + "_".join(str(k) for k in key)))
            block = es.enter_context(nc.Block())
            engmap = {"pe": block.tensor, "act": block.scalar, "dve": block.vector,
                      "pool": block.gpsimd, "sp": block.sync}
            for ename in self.ENGS:
                items = self.stream[ename]

                def body(eng, items=items, ename=ename):
                    for it in items:
                        if it[0] == "wait":
                            key, val = it[1], it[2]
                            if key[0] == "eng":
                                key, val = vmap[(key[1], val)]
                            eng.wait_ge(sems[key], val)
                        else:
                            ins = it[1](eng)
                            if it[3] is not None:
                                ins.then_inc(sems[it[3]], 16)
                            elif (ename, it[2]) in vmap:
                                ins.then_inc(sems[vmap[(ename, it[2])][0]], 1)
                engmap[ename](body)


class Rot:
    def __init__(self, es, alloc, name, shape, dtype, n):
        self.tiles = [es.enter_context(alloc(f"{name}{i}", shape, dtype)) for i in range(n)]
        self.name = name
        self.i = 0

    def next(self):
        k = self.i % len(self.tiles)
        self.i += 1
        return self.tiles[k], (self.name, k)


def mm(S, out, lhsT, rhs, start, stop, r, w):
    S.add("pe", lambda e: e.matmul(out, lhsT, rhs, start=start, stop=stop), reads=r, writes=w)


def tr(S, out, in_, ident, r, w):
    S.add("pe", lambda e: e.transpose(out, in_, ident), reads=r, writes=w)


def act(S, out, in_, func, r, w, **kw):
    S.add("act", lambda e: e.activation(out=out, in_=in_, func=func, **kw), reads=r, writes=w)


def tt(S, eng, out, a, b, op, r, w):
    S.add(eng, lambda e: e.tensor_tensor(out=out, in0=a, in1=b, op=op), reads=r, writes=w)


def ts(S, eng, out, a, s1, op0, r, w, s2=None, op1=None):
    if op1 is None:
        S.add(eng, lambda e: e.tensor_scalar(out=out, in0=a, scalar1=s1, scalar2=None, op0=op0), reads=r, writes=w)
    else:
        S.add(eng, lambda e: e.tensor_scalar(out=out, in0=a, scalar1=s1, scalar2=s2, op0=op0, op1=op1), reads=r, writes=w)


def stt(S, out, a, scalar, b, op0, op1, r, w):
    S.add("dve", lambda e: e.scalar_tensor_tensor(out=out, in0=a, scalar=scalar, in1=b, op0=op0, op1=op1), reads=r, writes=w)


def cp(S, eng, out, in_, r, w):
    if eng == "act":
        S.add("act", lambda e: e.activation(out=out, in_=in_, func=AF.Copy), reads=r, writes=w)
    else:
        S.add(eng, lambda e: e.tensor_copy(out=out, in_=in_), reads=r, writes=w)


def dma(S, q, out, in_, r, w, slow=False):
    if slow:
        S.add(q, lambda e: e.dma_start(out=out, in_=in_, allow_slow_non_contiguous=True), reads=r, writes=w, dma=True)
    else:
        S.add(q, lambda e: e.dma_start(out=out, in_=in_), reads=r, writes=w, dma=True)


def memset(S, eng, ap, val, w, r=()):
    S.add(eng, lambda e: e.memset(ap, val), reads=r, writes=w)


class Ctx:
    count = 0

    def __init__(self, nc, S, es, prefix):
        Ctx.count += 1
        self.nc, self.S, self.es, self.p = nc, S, es, f"{prefix}{Ctx.count}_"
        self.n = 0

    def sb(self, name, shape, dtype=F32):
        return self.es.enter_context(self.nc.sbuf_tensor(self.p + name, shape, dtype))

    def ps(self, name, shape, dtype=F32):
        return self.es.enter_context(self.nc.psum_tensor(self.p + name, shape, dtype))

    def rot(self, name, shape, dtype, n, psum=False):
        alloc = (lambda a, b, c: self.nc.psum_tensor(a, b, c)) if psum else (lambda a, b, c: self.nc.sbuf_tensor(a, b, c))
        return Rot(self.es, alloc, self.p + name, shape, dtype, n)


def rms_fm(S, src, src_res, nk, nfeat, gcol, ones_bf, sq_rot, psr, tmp, tmp_res, rstd, rstd_res, dst, dst_res, width, pn=128):
    pt, pres = psr.next()
    for k in range(nk):
        sq, sqr = sq_rot.next()
        act(S, sq[:pn, :width], src[:pn, k, :width], AF.Square, [src_res[k]], [sqr])
        mm(S, pt[:pn, :width], ones_bf[:pn, :pn], sq[:pn, :width], k == 0, k == nk - 1, [sqr, "ones"], [pres])
    act(S, tmp[:pn, :width], pt[:pn, :width], AF.Sqrt, [pres], [tmp_res], scale=1.0 / nfeat, bias=EPS)
    S.add("dve", lambda e: e.reciprocal(out=rstd[:pn, :width], in_=tmp[:pn, :width]), reads=[tmp_res], writes=[rstd_res])
    for k in range(nk):
        stt(S, dst[:pn, k, :width], src[:pn, k, :width], gcol[:pn, k:k + 1], rstd[:pn, :width], ALU.mult, ALU.mult,
            [src_res[k], rstd_res], [dst_res[k]])


def phase_mlp(S, nc, l, xin, xout, norm_mlp, w_up, w_down, ntiles=NT):
    with ExitStack() as es:
        C = Ctx(nc, S, es, "m_")
        ones_bf = C.sb("ones", [128, 128], BF16)
        gcol = C.sb("g", [128, 8])
        xt_rot = C.rot("xt", [128, 8, TT], F32, 2)
        sq_rot = C.rot("sq", [128, TT], BF16, 2)
        hT = C.sb("hT", [128, 8, TT], BF16)
        tmp = C.sb("tmp", [128, TT])
        rstd = C.sb("rstd", [128, TT])
        r_rot = C.rot("r", [128, TT], BF16, 3)
        aT = C.sb("aT", [128, 32, TT], BF16)
        wup_rot = C.rot("wup", [128, 8, 512], BF16, 2)
        wdn_rot = C.rot("wdn", [128, 32, 256], BF16, 2)
        psr = C.rot("ps", [128, TT], F32, 7, psum=True)
        memset(S, "pool", ones_bf[:, :], 1.0, ["ones"])
        dma(S, "sp", gcol[:, :], norm_mlp[l].rearrange("(c p) -> p c", p=128), [], ["g"], slow=True)
        xin_v = xin.rearrange("(c p) n -> p c n", p=128)
        xout_v = xout.rearrange("(c p) n -> p c n", p=128)
        wup_v = w_up[l].rearrange("(c p) n -> p c n", p=128)
        wdn_v = w_down[l].rearrange("(c p) n -> p c n", p=128)
        for t in range(ntiles):
            tok = slice(t * TT, (t + 1) * TT)
            xt, x0 = xt_rot.next()
            xres = [(x0, d) for d in range(8)]
            dma(S, "sp", xt[:, :, :], xin_v[:, :, tok], [("xin", t)], xres)
            hres = [("m_hT", k) for k in range(8)]
            rms_fm(S, xt, xres, 8, D, gcol, ones_bf, sq_rot, psr, tmp, "m_tmp", rstd, "m_rstd", hT, hres, TT)
            for jb in range(8):
                wu, wures = wup_rot.next()
                dma(S, "pool", wu[:, :, :], wup_v[:, :, jb * 512:(jb + 1) * 512], [], [wures])
                for jj in range(4):
                    j = jb * 4 + jj
                    pt, pres = psr.next()
                    for k in range(8):
                        mm(S, pt[:, :], wu[:, k, jj * 128:(jj + 1) * 128], hT[:, k, :], k == 0, k == 7, [wures, hres[k]], [pres])
                    r, rres = r_rot.next()
                    act(S, r[:, :], pt[:, :], AF.Relu, [pres], [rres])
                    tt(S, "dve", aT[:, j, :], r[:, :], r[:, :], ALU.mult, [rres], [("m_aT", j)])
            for db in range(4):
                wd, wdres = wdn_rot.next()
                dma(S, "pool", wd[:, :, :], wdn_v[:, :, db * 256:(db + 1) * 256], [], [wdres])
                for dd in range(2):
                    d = db * 2 + dd
                    pt, pres = psr.next()
                    for j in range(32):
                        mm(S, pt[:, :], wd[:, j, dd * 128:(dd + 1) * 128], aT[:, j, :], j == 0, j == 31, [wdres, ("m_aT", j)], [pres])
                    tt(S, "dve", xt[:, d, :], xt[:, d, :], pt[:, :], ALU.add, [pres, xres[d]], [xres[d]])
            dma(S, "sp", xout_v[:, :, tok], xt[:, :, :], xres, [("xout", t)])
    S.barrier()


def phase_final(S, nc, xin, xout, norm_final, ntiles=NT):
    with ExitStack() as es:
        C = Ctx(nc, S, es, "f_")
        ones_bf = C.sb("ones", [128, 128], BF16)
        gcol = C.sb("g", [128, 8])
        xt_rot = C.rot("xt", [128, 8, TT], F32, 2)
        ot_rot = C.rot("ot", [128, 8, TT], F32, 2)
        sq_rot = C.rot("sq", [128, TT], BF16, 2)
        tmp = C.sb("tmp", [128, TT])
        rstd = C.sb("rstd", [128, TT])
        psr = C.rot("ps", [128, TT], F32, 2, psum=True)
        memset(S, "pool", ones_bf[:, :], 1.0, ["ones"])
        dma(S, "sp", gcol[:, :], norm_final.rearrange("(c p) -> p c", p=128), [], ["g"], slow=True)
        xin_v = xin.rearrange("(c p) n -> p c n", p=128)
        xout_v = xout.rearrange("(c p) n -> p c n", p=128)
        for t in range(ntiles):
            tok = slice(t * TT, (t + 1) * TT)
            xt, x0 = xt_rot.next()
            xres = [(x0, d) for d in range(8)]
            dma(S, "sp", xt[:, :, :], xin_v[:, :, tok], [("xin", t)], xres)
            ot, o0 = ot_rot.next()
            ores = [(o0, d) for d in range(8)]
            rms_fm(S, xt, xres, 8, D, gcol, ones_bf, sq_rot, psr, tmp, "f_tmp", rstd, "f_rstd", ot, ores, TT)
            dma(S, "sp", xout_v[:, :, tok], ot[:, :, :], ores, [("xout", t)])
    S.barrier()


def phase_xattn(S, nc, l, xin, xout, memT, norm_x, norm_mem, wq, wk, wv, wo, ntiles=NT):
    with ExitStack() as es:
        C = Ctx(nc, S, es, "x_")
        ones_bf = C.sb("ones", [128, 128], BF16)
        gx = C.sb("gx", [128, 8])
        gm = C.sb("gm", [128, 8])
        mt = C.sb("mt", [128, 8, 256])
        mn = C.sb("mn", [128, 8, 256], BF16)
        wq_t = C.sb("wq", [128, 8, 1024], BF16)
        wo_t = C.sb("wo", [128, 8, 1024], BF16)
        wtmp_rot = C.rot("wtmp", [128, 8, 1024], BF16, 1)
        KT = C.sb("KT", [128, 8, 256], BF16)
        V = C.sb("V", [128, 2, 1024], BF16)
        xt_rot = C.rot("xt", [128, 8, TT], F32, 2)
        sq_rot = C.rot("sq", [128, TT], BF16, 2)
        hT = C.sb("hT", [128, 8, TT], BF16)
        qT = C.sb("qT", [128, 8, TT], BF16)
        oT = C.sb("oT", [128, 8, TT], BF16)
        pT_rot = C.rot("pT", [128, 2, TT], BF16, 2)
        tmp = C.sb("tmp", [128, TT])
        rstd = C.sb("rstd", [128, TT])
        rinv = C.sb("rinv", [128, TT])
        psr = C.rot("ps", [128, TT], F32, 7, psum=True)
        memset(S, "pool", ones_bf[:, :], 1.0, ["ones"])
        dma(S, "sp", gx[:, :], norm_x[l].rearrange("(c p) -> p c", p=128), [], ["gx"], slow=True)
        dma(S, "sp", gm[:, :], norm_mem[l].rearrange("(c p) -> p c", p=128), [], ["gm"], slow=True)
        dma(S, "sp", mt[:, :, :], memT.rearrange("(c p) n -> p c n", p=128), [], [("mt", k) for k in range(8)])
        dma(S, "pool", wq_t[:, :, :], wq[l].rearrange("(c p) n -> p c n", p=128), [], ["wq"])
        dma(S, "pool", wo_t[:, :, :], wo[l].rearrange("(c p) n -> p c n", p=128), [], ["wo"])
        mres = [("mn", k) for k in range(8)]
        rms_fm(S, mt, [("mt", k) for k in range(8)], 8, D, gm, ones_bf, sq_rot, psr, tmp, "x_tmp", rstd, "x_rstd", mn, mres, 256)
        wt, wres = wtmp_rot.next()
        dma(S, "pool", wt[:, :, :], wk[l].rearrange("(c p) n -> p c n", p=128), [], [wres])
        for c in range(8):
            pt, pres = psr.next()
            for k in range(8):
                mm(S, pt[:, :256], wt[:, k, c * 128:(c + 1) * 128], mn[:, k, :], k == 0, k == 7, [wres, mres[k]], [pres])
            cp(S, "act", KT[:, c, :], pt[:, :256], [pres], [("KT", c)])
        wt, wres = wtmp_rot.next()
        dma(S, "pool", wt[:, :, :], wv[l].rearrange("(c p) n -> p c n", p=128), [], [wres])
        for ms in range(2):
            for cb in range(2):
                pt, pres = psr.next()
                for k in range(8):
                    mm(S, pt[:, :], mn[:, k, ms * 128:(ms + 1) * 128], wt[:, k, cb * 512:(cb + 1) * 512], k == 0, k == 7, [wres, mres[k]], [pres])
                cp(S, "act", V[:, ms, cb * 512:(cb + 1) * 512], pt[:, :], [pres], [("V", ms, cb)])
        xin_v = xin.rearrange("(c p) n -> p c n", p=128)
        xout_v = xout.rearrange("(c p) n -> p c n", p=128)
        for t in range(ntiles):
            tok = slice(t * TT, (t + 1) * TT)
            xt, x0 = xt_rot.next()
            xres = [(x0, d) for d in range(8)]
            dma(S, "sp", xt[:, :, :], xin_v[:, :, tok], [("xin", t)], xres)
            hres = [("x_hT", k) for k in range(8)]
            rms_fm(S, xt, xres, 8, D, gx, ones_bf, sq_rot, psr, tmp, "x_tmp", rstd, "x_rstd", hT, hres, TT)
            for c in range(8):
                pt, pres = psr.next()
                for k in range(8):
                    mm(S, pt[:, :], wq_t[:, k, c * 128:(c + 1) * 128], hT[:, k, :], k == 0, k == 7, ["wq", hres[k]], [pres])
                cp(S, "act" if c % 2 else "dve", qT[:, c, :], pt[:, :], [pres], [("qT", c)])
            for h in range(4):
                pT, pTres = pT_rot.next()
                for ms in range(2):
                    pt, pres = psr.next()
                    for cc in range(2):
                        c = h * 2 + cc
                        mm(S, pt[:, :], KT[:, c, ms * 128:(ms + 1) * 128], qT[:, c, :], cc == 0, cc == 1, [("KT", c), ("qT", c)], [pres])
                    act(S, pT[:, ms, :], pt[:, :], AF.Exp, [pres], [(pTres, ms)], scale=1.0 / 16.0)
                pt, pres = psr.next()
                for ms in range(2):
                    mm(S, pt[:, :], ones_bf[:, :], pT[:, ms, :], ms == 0, ms == 1, ["ones", (pTres, ms)], [pres])
                S.add("dve", lambda e, pt=pt: e.reciprocal(out=rinv[:, :], in_=pt[:, :]), reads=[pres], writes=["rinv"])
                for cc in range(2):
                    c = h * 2 + cc
                    pt, pres = psr.next()
                    for ms in range(2):
                        mm(S, pt[:, :], V[:, ms, c * 128:(c + 1) * 128], pT[:, ms, :], ms == 0, ms == 1,
                           [("V", ms, c // 4), (pTres, ms)], [pres])
                    tt(S, "dve", oT[:, c, :], pt[:, :], rinv[:, :], ALU.mult, [pres, "rinv"], [("oT", c)])
            for d in range(8):
                pt, pres = psr.next()
                for c in range(8):
                    mm(S, pt[:, :], wo_t[:, c, d * 128:(d + 1) * 128], oT[:, c, :], c == 0, c == 7, ["wo", ("oT", c)], [pres])
                tt(S, "dve", xt[:, d, :], xt[:, d, :], pt[:, :], ALU.add, [pres, xres[d]], [xres[d]])
            dma(S, "sp", xout_v[:, :, tok], xt[:, :, :], xres, [("xout", t)])
    S.barrier()


def phase_merge(S, nc, l, xin, xout, hT_s, oaT_s, oT_s, w_in, w_branch, w_out, ntiles=NT):
    with ExitStack() as es:
        C = Ctx(nc, S, es, "g_")
        wba = C.sb("wba", [64, 4, 1024], BF16)
        wb = C.sb("wb", [128, 3, 2, 1024], BF16)
        wout = C.sb("wout", [128, 8, 1024], BF16)
        wg_rot = C.rot("wg", [128, 8, 1024], BF16, 2)
        xt_rot = C.rot("xt", [128, 8, TT], F32, 2)
        h_rot = C.rot("h", [128, 8, TT], BF16, 2)
        oa_rot = C.rot("oa", [64, 4, TT], BF16, 2)
        ob_rot = C.rot("ob", [128, 3, 2, TT], BF16, 2)
        sig_rot = C.rot("sig", [128, TT], BF16, 3)
        prod_rot = C.rot("prod", [128, TT], F32, 3)
        acc = C.sb("acc", [128, 8, TT])
        mT = C.sb("mT", [128, 8, TT], BF16)
        psr = C.rot("ps", [128, TT], F32, 7, psum=True)
        dma(S, "pool", wba[:, :, :], w_branch[l, 0].rearrange("(h p) n -> p h n", p=64), [], ["wba"])
        for n in range(3):
            dma(S, "pool", wb[:, n, :, :], w_branch[l, n + 1].rearrange("(c p) n -> p c n", p=128), [], [("wb", n)])
        dma(S, "pool", wout[:, :, :], w_out[l].rearrange("(c p) n -> p c n", p=128), [], ["wout"])
        xin_v = xin.rearrange("(c p) n -> p c n", p=128)
        xout_v = xout.rearrange("(c p) n -> p c n", p=128)
        hv = hT_s.rearrange("(c p) n -> p c n", p=128)
        win_v = w_in[l].rearrange("(c p) n -> p c n", p=128)
        for t in range(ntiles):
            tok = slice(t * TT, (t + 1) * TT)
            xt, x0 = xt_rot.next()
            xres = [(x0, d) for d in range(8)]
            dma(S, "sp", xt[:, :, :], xin_v[:, :, tok], [("xin", t)], xres)
            ht, hres = h_rot.next()
            dma(S, "sp", ht[:, :, :], hv[:, :, tok], [("hT_s", t)], [hres])
            oa, oares = oa_rot.next()
            dma(S, "sp", oa[:, :, :], oaT_s.rearrange("h p n -> p h n")[:, :, tok], [("oaT_s", t)], [oares])
            ob, obres = ob_rot.next()
            for n in range(3):
                dma(S, "sp", ob[:, n, :, :], oT_s[n].rearrange("(c p) n -> p c n", p=128)[:, :, tok], [("oT_s", n, t)], [(obres, n)])
            for n in range(4):
                wg, wgres = wg_rot.next()
                dma(S, "pool", wg[:, :, :], win_v[:, :, 2744 + n * 1024: 2744 + (n + 1) * 1024], [], [wgres])
                for d in range(8):
                    pt, pres = psr.next()
                    for k in range(8):
                        mm(S, pt[:, :], wg[:, k, d * 128:(d + 1) * 128], ht[:, k, :], k == 0, k == 7, [wgres, hres], [pres])
                    sg, sgres = sig_rot.next()
                    act(S, sg[:, :], pt[:, :], AF.Sigmoid, [pres], [sgres])
                    pt2, pres2 = psr.next()
                    if n == 0:
                        for h in range(4):
                            mm(S, pt2[:, :], wba[:, h, d * 128:(d + 1) * 128], oa[:, h, :], h == 0, h == 3, ["wba", oares], [pres2])
                    else:
                        for c in range(2):
                            mm(S, pt2[:, :], wb[:, n - 1, c, d * 128:(d + 1) * 128], ob[:, n - 1, c, :], c == 0, c == 1,
                               [("wb", n - 1), (obres, n - 1)], [pres2])
                    if n == 0:
                        tt(S, "dve", acc[:, d, :], pt2[:, :], sg[:, :], ALU.mult, [pres2, sgres], [("acc", d)])
                    else:
                        pr, prres = prod_rot.next()
                        tt(S, "dve", pr[:, :], pt2[:, :], sg[:, :], ALU.mult, [pres2, sgres], [prres])
                        if n < 3:
                            tt(S, "pool", acc[:, d, :], acc[:, d, :], pr[:, :], ALU.add, [("acc", d), prres], [("acc", d)])
                        else:
                            tt(S, "pool", mT[:, d, :], acc[:, d, :], pr[:, :], ALU.add, [("acc", d), prres], [("mT", d)])
            for d in range(8):
                pt, pres = psr.next()
                for c in range(8):
                    mm(S, pt[:, :], wout[:, c, d * 128:(d + 1) * 128], mT[:, c, :], c == 0, c == 7, ["wout", ("mT", c)], [pres])
                tt(S, "dve", xt[:, d, :], xt[:, d, :], pt[:, :], ALU.add, [pres, xres[d]], [xres[d]])
            dma(S, "sp", xout_v[:, :, tok], xt[:, :, :], xres, [("xout", t)])
    S.barrier()


def phase_mla(S, nc, qT_s, kT_s, v_s, oaT_s, cmask_d, nq=NT):
    with ExitStack() as es:
        C = Ctx(nc, S, es, "a_")
        KT = C.sb("KT", [96, SEQ], BF16)
        V = C.sb("V", [128, SEQ // 128, 65], BF16)
        q_rot = C.rot("q", [96, TT], BF16, 2)
        pT_rot = C.rot("pT", [128, TT], BF16, 4)
        cm = C.sb("cm", [128, 128], BF16)
        ones1 = C.sb("ones1", [65, 64])
        oS = C.sb("oS", [65, TT])
        rinv = C.sb("rinv", [65, TT])
        o_rot = C.rot("o", [64, TT], BF16, 2)
        psr = C.rot("ps", [128, TT], F32, 4, psum=True)
        pso = C.rot("pso", [128, TT], F32, 2, psum=True)
        psb = C.rot("psb", [128, TT], F32, 1, psum=True)
        dma(S, "pool", cm[:, :], cmask_d, [], ["cm"])
        memset(S, "pool", ones1[:, :], 1.0, ["ones1"])
        scale = float(96 ** -0.5)
        for h in range(4):
            dma(S, "sp", KT[:, :], kT_s[h], [("kT_s",)], ["KT"])
            dma(S, "sp", V[:, :, :], v_s.rearrange("(n p) (h e) -> p n h e", p=128, e=65)[:, :, h, :], [("v_s",)], ["V"], slow=True)
            for qt in range(nq):
                q, qres = q_rot.next()
                dma(S, "sp", q[:, :], qT_s[h][:, qt * TT:(qt + 1) * TT], [("qT_s",)], [qres])
                po, pores = pso.next()
                nkb = 4 * qt + 4
                for kb in range(nkb):
                    r = kb - 4 * qt
                    q0 = max(r, 0) * 128
                    pt, pres = psr.next()
                    mm(S, pt[:, q0:], KT[:, kb * 128:(kb + 1) * 128], q[:, q0:], True, True, ["KT", qres], [pres])
                    pT, pTres = pT_rot.next()
                    act(S, pT[:, q0:], pt[:, q0:], AF.Exp, [pres], [pTres], scale=scale)
                    if r >= 0:
                        tt(S, "pool", pT[:, q0:q0 + 128], pT[:, q0:q0 + 128], cm[:, :], ALU.mult, [pTres, "cm"], [pTres])
                    mm(S, po[:65, q0:], V[:, kb, :], pT[:, q0:], kb == 0, kb == nkb - 1, ["V", pTres], [pores])
                cp(S, "act", oS[:, :], po[:65, :], [pores], ["oS"])
                S.add("dve", lambda e: e.reciprocal(out=rinv[64:65, :], in_=oS[64:65, :]), reads=["oS"], writes=["rinv"])
                pb, pbres = psb.next()
                mm(S, pb[:64, :], ones1[64:65, :], rinv[64:65, :], True, True, ["ones1", "rinv"], [pbres])
                o, ores = o_rot.next()
                tt(S, "dve", o[:, :], pb[:64, :], oS[:64, :], ALU.mult, [pbres, "oS"], [ores])
                dma(S, "sp", oaT_s[h][:, qt * TT:(qt + 1) * TT], o[:, :], [ores], [("oaT_s", qt)])
    S.barrier()

def phase_rope(S, nc, posrep, cconst, cos_s, sin_s):
    with ExitStack() as es:
        C = Ctx(nc, S, es, "r_")
        pi_ = C.sb("pi", [96, TT], I32)
        pf = C.sb("pf", [96, TT])
        ang = C.sb("ang", [96, TT])
        ni = C.sb("ni", [96, TT], I32)
        nf = C.sb("nf", [96, TT])
        y = C.sb("y", [96, TT])
        cc = C.sb("cc", [96, 2])
        dma(S, "sp", cc[:, :], cconst, [], ["cc"])
        for t in range(NT):
            tok = slice(t * TT, (t + 1) * TT)
            dma(S, "sp", pi_[:, :], posrep[:, tok], [], ["pi"])
            cp(S, "dve", pf[:, :], pi_[:, :], ["pi"], ["pf"])
            for which in range(2):
                off = 0.0 if which == 0 else float(np.pi / 2)
                ts(S, "dve", ang[:, :], pf[:, :], cc[:, 0:1], ALU.mult, ["pf", "cc"], ["ang"], s2=off, op1=ALU.add)
                ts(S, "dve", ni[:, :], ang[:, :], float(1 / (2 * np.pi)), ALU.mult, ["ang"], ["ni"])
                cp(S, "dve", nf[:, :], ni[:, :], ["ni"], ["nf"])
                stt(S, y[:, :], nf[:, :], float(-2 * np.pi), ang[:, :], ALU.mult, ALU.add, ["nf", "ang"], ["y"])
                ts(S, "dve", nf[:, :], y[:, :], float(np.pi), ALU.is_gt, ["y"], ["nf2"], s2=float(-2 * np.pi), op1=ALU.mult)
                tt(S, "dve", y[:, :], y[:, :], nf[:, :], ALU.add, ["y", "nf2"], ["y2"])
                ts(S, "dve", y[:, :], y[:, :], float(np.pi), ALU.min, ["y2"], ["y3"], s2=float(-np.pi), op1=ALU.max)
                act(S, ang[:, :], y[:, :], AF.Sin, ["y3"], ["sv"])
                if which == 0:
                    ts(S, "dve", y[:, :], ang[:, :], cc[:, 1:2], ALU.mult, ["sv", "cc"], ["yo"])
                    dma(S, "sp", sin_s[:, tok], y[:, :], ["yo"], [("sin_s", t)])
                else:
                    dma(S, "sp", cos_s[:, tok], ang[:, :], ["sv"], [("cos_s", t)])
    S.barrier()


def bcast_row(S, C, dst, src_row, n, ones_row, psr, tag):
    st = C.sb("bst_" + tag, [1, n])
    dma(S, "sp", st[:, :], src_row, [], ["bst_" + tag])
    pt, pres = psr.next()
    mm(S, pt[:, :n], ones_row[0:1, :], st[0:1, :], True, True, ["ones_row", "bst_" + tag], [pres])
    cp(S, "act", dst, pt[:, :n], [pres], ["bc_" + tag])


def phase_A(S, nc, l, xin, P, K, sc, ntiles=None, do=("mla", "sg", "gla", "dn")):
    TA = 256
    NCH = TA // 64
    NSUB = TA // 128
    if ntiles is None:
        ntiles = SEQ // TA
    with ExitStack() as es:
        C = Ctx(nc, S, es, "A_")
        sb = C.sb
        ones_bf = sb("ones", [128, 128], BF16)
        ones_row = sb("ones_row", [1, 128])
        ident = sb("ident", [128, 128])
        memset(S, "pool", ones_bf[:, :], 1.0, ["ones"])
        memset(S, "pool", ones_row[:, :], 1.0, ["ones_row"])
        dma(S, "sp", ident[:, :], K["ident"], [], ["ident"])
        Win = sb("Win", [128, 8, 2744], BF16)
        win_v = P["w_in"][l].rearrange("(c p) n -> p c n", p=128)
        for cb in range(0, 2744, 512):
            ce = min(cb + 512, 2744)
            dma(S, "pool", Win[:, :, cb:ce], win_v[:, :, cb:ce], [], ["Win"])
        gmix = sb("gmix", [128, 8])
        dma(S, "sp", gmix[:, :], P["norm_mix"][l].rearrange("(c p) -> p c", p=128), [], ["gmix"], slow=True)
        xt_rot = C.rot("xt", [128, 8, TA], F32, 1)
        sq_rot = C.rot("sq", [128, TA], BF16, 2)
        hT = sb("hT", [128, 8, TA], BF16)
        tmp = sb("tmp", [128, TA])
        rstd = sb("rstd", [128, TA])
        psr = C.rot("ps", [128, 512], F32, 8, psum=True)
        xin_v = xin.rearrange("(c p) n -> p c n", p=128)
        hv = sc["hT_s"].rearrange("(c p) n -> p c n", p=128)
        hres = [("A_hT", k) for k in range(8)]

        def fproj(pt, pres, c0, n, rows=None):
            for k in range(8):
                mm(S, pt[:n, :TA], Win[:, k, c0:c0 + n], hT[:, k, :], k == 0, k == 7, ["Win", hres[k]], [pres])

        def tproj(pt, pres, s, c0, n, o0=0):
            for k in range(8):
                mm(S, pt[:, o0:o0 + n], hT[:, k, s * 128:(s + 1) * 128], Win[:, k, c0:c0 + n], k == 0, k == 7, ["Win", hres[k]], [pres])

        if "mla" in do:
            gq = sb("gq", [128, 2])
            gkv = sb("gkv", [128, 1])
            dma(S, "sp", gq[:, :], P["mla_norm_q"][l].rearrange("(c p) -> p c", p=128), [], ["gq"], slow=True)
            dma(S, "sp", gkv[:, :], P["mla_norm_kv"][l].rearrange("(c p) -> p c", p=128), [], ["gkv"], slow=True)
            Wkr = sb("Wkr", [128, 8, 96], BF16)
            Wkt = sb("Wkt", [128, 8, 96], BF16)
            memset(S, "pool", Wkr[:, :, :], 0.0, ["Wkr"])
            memset(S, "pool", Wkt[:, :, :], 0.0, ["Wkt"])
            dma(S, "pool", Wkr[:, :, 64:96], win_v[:, :, 384:416], [], ["Wkr"])
            dma(S, "pool", Wkt[:, :, 64:80], win_v[:, :, 400:416], [], ["Wkt"])
            dma(S, "pool", Wkt[:, :, 80:96], win_v[:, :, 384:400], [], ["Wkt"])
            Wuq = sb("Wuq", [128, 2, 384], BF16)
            dma(S, "pool", Wuq[:, :, :], P["mla_w_uq"][l].rearrange("(c p) n -> p c n", p=128), [], ["Wuq"])
            Wuqt = sb("Wuqt", [128, 2, 4, 96], BF16)
            memset(S, "pool", Wuqt[:, :, :, :], 0.0, ["Wuqt"])
            uqv = P["mla_w_uq"][l].rearrange("(c p) (h e) -> p c h e", p=128, e=96)
            for c in range(2):
                dma(S, "pool", Wuqt[:, c, :, 64:80], uqv[:, c, :, 80:96], [], ["Wuqt"])
                dma(S, "pool", Wuqt[:, c, :, 80:96], uqv[:, c, :, 64:80], [], ["Wuqt"])
            Wkn = sb("Wkn", [128, 4, 96], BF16)
            memset(S, "pool", Wkn[:, :, :], 0.0, ["Wkn"])
            ukv = P["mla_w_ukv"][l].rearrange("k (h e) -> k h e", e=128)
            dma(S, "pool", Wkn[:, :, 0:64], ukv[:, :, 0:64], [], ["Wkn"])
            Wv = sb("Wv", [128, 4, 64], BF16)
            dma(S, "pool", Wv[:, :, :], ukv[:, :, 64:128], [], ["Wv"])
            cqs = sb("cqs", [128, 2, TA])
            cqn = sb("cqn", [128, 2, TA], BF16)
            ckvs = sb("ckvs", [128, 1, TA])
            ckvn = sb("ckvn", [128, 1, TA], BF16)
            cosT = sb("cosT", [96, TA])
            sinT = sb("sinT", [96, TA])
            tmpK = sb("tmpK", [96, TA])
            t1_rot = C.rot("t1", [96, TA], F32, 1)
            t2_rot = C.rot("t2", [96, TA], F32, 1)
            qk_rot = C.rot("qk", [96, TA], BF16, 2)
            vt = sb("vt", [128, NSUB, 4, 65], BF16)
            memset(S, "pool", vt[:, :, :, :], 1.0, ["vt"])
        if "sg" in do:
            WmT = sb("WmT", [128, 4, 128])
            sg_t = sb("sg_t", [128, 512])
            sgw = sg_t[:, :].rearrange("p (g j) -> p g j", g=4)
            sgmask = sb("sgmask", [128, 128])
            dma(S, "sp", sgmask[:, :], K["trilT"], [], ["sgmask"])
            dma(S, "sp", sgw, P["sg_w"][l].rearrange("g i j -> i g j"), [], ["sg_t"])
            for g in range(4):
                pt, pres = psr.next()
                tr(S, pt[:, :128], sgw[:, g, :], ident[:, :], ["sg_t", "ident"], [pres])
                tt(S, "dve", WmT[:, g, :], pt[:, :128], sgmask[:, :], ALU.mult, [pres, "sgmask"], ["WmT"])
            sgb = sb("sgb", [128, 4])
            dma(S, "sp", sgb[:, :], P["sg_b"][l].rearrange("g i -> i g"), [], ["sgb"], slow=True)
            lng = sb("lng", [128, 256])
            lnb = sb("lnb", [128, 256])
            bcast_row(S, C, lng[:, :], P["sg_ln_g"][l:l + 1, :], 256, ones_row, psr, "lng")
            bcast_row(S, C, lnb[:, :], P["sg_ln_b"][l:l + 1, :], 256, ones_row, psr, "lnb")
            sg_i = sb("sg_i", [128, 512])
            sg_g = sb("sg_g", [128, 512])
            sg_s = sb("sg_s", [128, 8])
            sg_vc = sb("sg_vc", [128, 256])
            sg_junk = sb("sg_junk", [128, 256])
            sg_vn = sb("sg_vn", [128, 256])
            sg_o = sb("sg_o", [128, 256])
            obT = sb("obT", [128, 2, TA], BF16)
        if "gla" in do:
            wgate = sb("wgate", [16, 128])
            dma(S, "sp", wgate[:, :], P["gla_w_gate"][l], [], ["wgate"])
            nbg = sb("nbg", [128, 1])
            dma(S, "sp", nbg[:, :], P["gla_b_gate"][l].rearrange("(p o) -> p o", o=1), [], ["nbg0"], slow=True)
            ts(S, "dve", nbg[:, :], nbg[:, :], -1.0, ALU.mult, ["nbg0"], ["nbg"])
            rm = sb("rm", [128, TA])
            dma(S, "sp", rm[:, :], K["rm"][:, 0:TA], [], ["rm"])
            mg = sb("mg", [128, 128])
            dma(S, "sp", mg[:, :], K["mg"], [], ["mg"])
            hm = sb("hm", [128, 256])
            dma(S, "sp", hm[:, :], K["hm"], [], ["hm"])
            hcol = sb("hcol", [128, 4])
            dma(S, "sp", hcol[:, :], K["hcol"], [], ["hcol"])
            topbot = sb("topbot", [128, 2])
            dma(S, "sp", topbot[:, :], K["topbot"], [], ["topbot"])
            cmA = sb("cmA", [128, TA])
            dma(S, "sp", cmA[:, :], K["cmA"][:, 0:TA], [], ["cmA"])
            gnb = sb("gnb", [128, 256])
            for h in range(4):
                bcast_row(S, C, gnb[:, h * 64:(h + 1) * 64], P["gla_norm"][l:l + 1, :], 64, ones_row, psr, f"gn{h}")
            glr = sb("glr", [16, TA])
            g_l = sb("g_l", [128, TA])
            g_cum = sb("g_cum", [128, TA])
            g_ec = sb("g_ec", [128, TA])
            g_en = sb("g_en", [128, TA])
            g_qe = sb("g_qe", [128, TA])
            g_qA = sb("g_qA", [128, TA])
            g_qB = sb("g_qB", [128, TA])
            g_ke = sb("g_ke", [128, TA])
            g_kl = sb("g_kl", [128, TA])
            g_qp = sb("g_qp", [128, 4, TA])
            g_klA = sb("g_klA", [128, 128])
            g_klB = sb("g_klB", [128, 128])
            g_v = sb("g_v", [128, 256])
            g_og = sb("g_og", [128, 256])
            g_sc = C.rot("g_sc", [128, 128], F32, 4)
            g_S = sb("g_S", [128, 256])
            g_tmp = sb("g_tmp", [128, 256])
            g_ss = sb("g_ss", [128, 8])
            g_junk = sb("g_junk", [128, 64])
            g_o = sb("g_o", [128, 256])
            ocT = sb("ocT", [128, 2, TA], BF16)
            memset(S, "pool", g_S[:, :], 0.0, ["g_S"])
        if "dn" in do:
            if "gla" not in do:
                rm = sb("rm", [128, TA])
                dma(S, "sp", rm[:, :], K["rm"][:, 0:TA], [], ["rm"])
                topbot = sb("topbot", [128, 2])
                dma(S, "sp", topbot[:, :], K["topbot"], [], ["topbot"])
                cmA = sb("cmA", [128, TA])
                dma(S, "sp", cmA[:, :], K["cmA"][:, 0:TA], [], ["cmA"])
            ones64 = ones_bf
            cU = sb("cU", [128, 128]); dma(S, "sp", cU[:, :], K["U"], [], ["cU"])
            cL = sb("cL", [128, 128]); dma(S, "sp", cL[:, :], K["L"], [], ["cL"])
            cst = sb("cst", [128, 128]); dma(S, "sp", cst[:, :], K["strict"], [], ["cst"])
            sel4 = sb("sel4", [4, 4, 128]); dma(S, "sp", sel4[:, :, :], K["sel4"], [], ["sel4"])
            zrow = sb("zrow", [1, 256]); memset(S, "pool", zrow[:, :], 0.0, ["zrow"])
            cw = sb("cw", [64, 12, 4])
            for kk in range(4):
                dma(S, "sp", cw[:, :, kk], P["dn_conv"][l, kk].rearrange("(i p) -> p i", p=64), [], ["cw"], slow=True)
            alog = sb("alog", [4, 1]); dma(S, "sp", alog[:, :], P["dn_a_log"][l].rearrange("(p o) -> p o", o=1), [], ["alog0"], slow=True)
            dtb = sb("dtb", [4, 1]); dma(S, "sp", dtb[:, :], P["dn_dt_bias"][l].rearrange("(p o) -> p o", o=1), [], ["dtb"], slow=True)
            negA = sb("negA", [4, 1])
            act(S, negA[:, :], alog[:, :], AF.Exp, ["alog0"], ["negA0"])
            ts(S, "dve", negA[:, :], negA[:, :], -1.0, ALU.mult, ["negA0"], ["negA"])
            dnb = sb("dnb", [128, 256])
            for h in range(4):
                bcast_row(S, C, dnb[:, h * 64:(h + 1) * 64], P["dn_norm"][l:l + 1, :], 64, ones_row, psr, f"dnn{h}")
            xc = sb("xc", [64, 12, 3 + TA])
            memset(S, "pool", xc[:, :, 0:3], 0.0, [("xc", i) for i in range(12)])
            d_y = sb("d_y", [64, 12, TA])
            d_sq = C.rot("d_sq", [64, TA], BF16, 2)
            d_t = sb("d_t", [64, TA])
            d_r = sb("d_r", [64, TA])
            d_a = sb("d_a", [4, TA]); d_e = sb("d_e", [4, TA]); d_g = sb("d_g", [4, TA]); d_gam = sb("d_gam", [4, TA])
            d_b = sb("d_b", [4, TA]); d_eg = sb("d_eg", [4, TA]); d_bk = sb("d_bk", [4, TA]); d_lg = sb("d_lg", [4, TA])
            d_elb = sb("d_elb", [64, 4, NCH])
            d_gbc = sb("d_gbc", [128, 4, TA])
            d_kp = sb("d_kp", [64, 4, TA])
            d_qA = sb("d_qA", [64, 4, TA]); d_qB = sb("d_qB", [64, 4, TA])
            d_cols = sb("d_cols", [128, 12])
            d_Vp = sb("d_Vp", [128, 256])
            d_kdA = sb("d_kdA", [128, 4, 64]); d_kdB = sb("d_kdB", [128, 4, 64])
            d_Dm = C.rot("d_Dm", [128, 128], F32, 2)
            d_d1 = C.rot("d_d1", [128, 128], F32, 2)
            d_at = sb("d_at", [128, 4, 128])
            d_Bp = [sb(f"d_Bp{h}", [128, 2, 128]) for h in range(4)]
            d_Bt = [sb(f"d_Bt{h}", [128, 2, 128]) for h in range(4)]
            d_R = [sb(f"d_R{h}", [128, 128]) for h in range(4)]
            d_rhs2 = sb("d_rhs2", [128, 256])
            d_u = sb("d_u", [128, 256])
            d_S = sb("d_S", [64, 4, 64])
            memset(S, "pool", d_S[:, :, :], 0.0, ["d_S"])
            memset(S, "pool", d_u[:, :], 0.0, ["d_u"])
            d_z = sb("d_z", [128, 256])
            d_ss = sb("d_ss", [128, 8])
            d_junk = sb("d_junk", [128, 64])
            d_junk2 = sb("d_junk2", [128, 64])
            d_o = sb("d_o", [128, 256])
            odT = sb("odT", [128, 2, TA], BF16)

        def post_norm_gate(o_ps, o_res, gate_sb, gate_res, ss, junk, o_out, tagp, oT_tile, s):
            for h in range(4):
                act(S, junk[:, :], o_ps[:, h * 64:(h + 1) * 64], AF.Square, [o_res], [tagp + "junk"], accum_out=ss[:, h:h + 1])
            S.stream
            act(S, ss[:, 4:8], ss[:, 0:4], AF.Sqrt, [tagp + "junk"], [tagp + "ss1"], scale=1.0 / 64, bias=EPS)
            S.add("dve", lambda e: e.reciprocal(out=ss[:, 0:4], in_=ss[:, 4:8]), reads=[tagp + "ss1"], writes=[tagp + "ss2"])
            for h in range(4):
                stt(S, o_out[:, h * 64:(h + 1) * 64], o_ps[:, h * 64:(h + 1) * 64], ss[:, h:h + 1], gate_sb[:, h * 64:(h + 1) * 64],
                    ALU.mult, ALU.mult, [o_res, tagp + "ss2", gate_res], [tagp + "oo"])
            for c in range(2):
                pt, pres = psr.next()
                tr(S, pt[:, :128], o_out[:, c * 128:(c + 1) * 128], ident[:, :], [tagp + "oo", "ident"], [pres])
                cp(S, "act", oT_tile[:, c, s * 128:(s + 1) * 128], pt[:, :128], [pres], [tagp + "oT"])

        for t in range(ntiles):
            tok = slice(t * TA, (t + 1) * TA)
            xt, x0 = xt_rot.next()
            xres = [(x0, d) for d in range(8)]
            dma(S, "sp", xt[:, :, :], xin_v[:, :, tok], [], xres)
            rms_fm(S, xt, xres, 8, D, gmix, ones_bf, sq_rot, psr, tmp, "A_tmp", rstd, "A_rstd", hT, hres, TA)
            dma(S, "sp", hv[:, :, tok], hT[:, :, :], hres, [("hT_s", t)])
            if "mla" in do:
                dma(S, "sp", cosT[:, :], sc["cos_s"][:, tok], [], ["cosT"])
                dma(S, "sp", sinT[:, :], sc["sin_s"][:, tok], [], ["sinT"])
                for c in range(2):
                    pt, pres = psr.next()
                    fproj(pt, pres, c * 128, 128)
                    cp(S, "act", cqs[:, c, :], pt[:, :TA], [pres], [("cqs", c)])
                rms_fm(S, cqs, [("cqs", 0), ("cqs", 1)], 2, 256, gq, ones_bf, sq_rot, psr, tmp, "A_tmp", rstd, "A_rstd",
                       cqn, [("cqn", 0), ("cqn", 1)], TA)
                pt, pres = psr.next()
                fproj(pt, pres, 256, 128)
                cp(S, "act", ckvs[:, 0, :], pt[:, :TA], [pres], [("ckvs", 0)])
                rms_fm(S, ckvs, [("ckvs", 0)], 1, 128, gkv, ones_bf, sq_rot, psr, tmp, "A_tmp", rstd, "A_rstd",
                       ckvn, [("ckvn", 0)], TA)
                pt, pres = psr.next()
                for k in range(8):
                    mm(S, pt[:96, :TA], Wkt[:, k, :], hT[:, k, :], k == 0, k == 7, ["Wkt", hres[k]], [pres])
                tt(S, "dve", tmpK[:, :], pt[:96, :TA], sinT[:, :], ALU.mult, [pres, "sinT"], ["tmpK"])
                for h in range(4):
                    pt, pres = psr.next()
                    for c in range(2):
                        mm(S, pt[:96, :TA], Wuq[:, c, h * 96:(h + 1) * 96], cqn[:, c, :], c == 0, c == 1, ["Wuq", ("cqn", c)], [pres])
                    pt2, pres2 = psr.next()
                    for c in range(2):
                        mm(S, pt2[:96, :TA], Wuqt[:, c, h, :], cqn[:, c, :], c == 0, c == 1, ["Wuqt", ("cqn", c)], [pres2])
                    t1, t1r = t1_rot.next()
                    t2, t2r = t2_rot.next()
                    tt(S, "dve", t1[:, :], pt[:96, :TA], cosT[:, :], ALU.mult, [pres, "cosT"], [t1r])
                    tt(S, "dve", t2[:, :], pt2[:96, :TA], sinT[:, :], ALU.mult, [pres2, "sinT"], [t2r])
                    qk, qkr = qk_rot.next()
                    tt(S, "pool", qk[:, :], t1[:, :], t2[:, :], ALU.add, [t1r, t2r], [qkr])
                    dma(S, "sp", sc["qT_s"][h][:, tok], qk[:, :], [qkr], [("qT_s", h, t)])
                    pt, pres = psr.next()
                    for k in range(8):
                        mm(S, pt[:96, :TA], Wkr[:, k, :], hT[:, k, :], k == 0, False, ["Wkr", hres[k]], [pres])
                    mm(S, pt[:96, :TA], Wkn[:, h, :], ckvn[:, 0, :], False, True, ["Wkn", ("ckvn", 0)], [pres])
                    t1, t1r = t1_rot.next()
                    tt(S, "dve", t1[:, :], pt[:96, :TA], cosT[:, :], ALU.mult, [pres, "cosT"], [t1r])
                    qk, qkr = qk_rot.next()
                    tt(S, "pool", qk[:, :], t1[:, :], tmpK[:, :], ALU.add, [t1r, "tmpK"], [qkr])
                    dma(S, "sp", sc["kT_s"][h][:, tok], qk[:, :], [qkr], [("kT_s", h, t)])
                for s in range(NSUB):
                    pt, pres = psr.next()
                    mm(S, pt[:, :256], ckvn[:, 0, s * 128:(s + 1) * 128], Wv[:, :, :].rearrange("p h e -> p (h e)"), True, True,
                       ["Wv", ("ckvn", 0)], [pres])
                    cp(S, "act", vt[:, s, :, 0:64], pt[:, :256].rearrange("p (h e) -> p h e", e=64), [pres], ["vt"])
                dma(S, "sp", sc["v_s"][tok, :].rearrange("(s p) n -> p s n", p=128), vt[:, :, :, :].rearrange("p s h e -> p s (h e)"),
                    ["vt"], [("v_s", t)])
            if "gla" in do:
                pt, pres = psr.next()
                fproj(pt, pres, 1440, 16)
                cp(S, "act", glr[:, :], pt[:16, :TA], [pres], ["glr"])
                pt, pres = psr.next()
                mm(S, pt[:, :TA], wgate[:, :], glr[:, :], True, True, ["wgate", "glr"], [pres])
                act(S, g_l[:, :], pt[:, :TA], AF.Exp, [pres], ["g_l0"], scale=-1.0, bias=nbg[:, 0:1])
                act(S, g_l[:, :], g_l[:, :], AF.Ln, ["g_l0", "nbg"], ["g_l"], bias=1.0)
                S.add("dve", lambda e: e.tensor_tensor_scan(out=g_cum[:, :], data0=rm[:, :], data1=g_l[:, :], initial=0.0,
                                                            op0=ALU.mult, op1=ALU.add), reads=["rm", "g_l"], writes=["g_cum"])
                act(S, g_ec[:, :], g_cum[:, :], AF.Exp, ["g_cum"], ["g_ec"], scale=-1.0 / 16)
                act(S, g_en[:, :], g_cum[:, :], AF.Exp, ["g_cum"], ["g_en"], scale=1.0 / 16)
                pt, pres = psr.next()
                fproj(pt, pres, 928, 128)
                stt(S, g_qe[:, :], pt[:, :TA], float(32 ** -0.5), g_ec[:, :], ALU.mult, ALU.mult, [pres, "g_ec"], ["g_qe"])
                pt, pres = psr.next()
                fproj(pt, pres, 1056, 128)
                tt(S, "dve", g_ke[:, :], pt[:, :TA], g_en[:, :], ALU.mult, [pres, "g_en"], ["g_ke"])
                for c in range(NCH):
                    ts(S, "dve", g_kl[:, c * 64:(c + 1) * 64], g_ke[:, c * 64:(c + 1) * 64], g_ec[:, c * 64 + 63:c * 64 + 64], ALU.mult,
                       ["g_ke", "g_ec"], [("g_kl", c // 2)])
                for h in range(4):
                    ts(S, "pool", g_qp[:, h, :], g_qe[:, :], hcol[:, h:h + 1], ALU.mult, ["g_qe", "hcol"], [("g_qp", h)])
                tt(S, "pool", g_qA[:, :], g_qe[:, :], cmA[:, :], ALU.mult, ["g_qe", "cmA"], ["g_qA"])
                tt(S, "pool", g_qB[:, :], g_qe[:, :], g_qA[:, :], ALU.subtract, ["g_qe", "g_qA"], ["g_qB"])
            if "dn" in do:
                for i in range(12):
                    pt, pres = psr.next()
                    fproj(pt, pres, 1712 + i * 64, 64)
                    cp(S, "act", xc[:, i, 3:3 + TA], pt[:64, :TA], [pres], [("xc", i)])
                    yi = d_y[:, i, :]
                    ts(S, "dve", yi, xc[:, i, 3:3 + TA], cw[:, i, 3:4], ALU.mult, [("xc", i), "cw"], [("d_y", i)])
                    for kk in range(3):
                        stt(S, yi, xc[:, i, kk:kk + TA], cw[:, i, kk:kk + 1], yi, ALU.mult, ALU.add, [("xc", i), ("d_y", i), "cw"], [("d_y", i)])
                    cp(S, "pool", xc[:, i, 0:3], xc[:, i, TA:TA + 3], [("xc", i)], [("xc", i)])
                    act(S, yi, yi, AF.Silu, [("d_y", i)], [("d_y", i)])
                    if i < 8:
                        sq, sqr = d_sq.next()
                        act(S, sq[:, :], yi, AF.Square, [("d_y", i)], [sqr])
                        pt, pres = psr.next()
                        mm(S, pt[:64, :TA], ones64[:64, :64], sq[:, :], True, True, ["ones", sqr], [pres])
                        act(S, d_t[:, :], pt[:64, :TA], AF.Sqrt, [pres], ["d_t"], bias=EPS)
                        S.add("dve", lambda e: e.reciprocal(out=d_r[:, :], in_=d_t[:, :]), reads=["d_t"], writes=["d_r"])
                        stt(S, yi, yi, 0.125 if i < 4 else 1.0, d_r[:, :], ALU.mult, ALU.mult, [("d_y", i), "d_r"], [("d_y", i)])
                pt, pres = psr.next()
                fproj(pt, pres, 2480, 4)
                act(S, d_e[:, :], pt[:4, :TA], AF.Exp, [pres, "dtb"], ["d_e0"], bias=dtb[:, 0:1])
                act(S, d_e[:, :], d_e[:, :], AF.Ln, ["d_e0"], ["d_e"], bias=1.0)
                ts(S, "dve", d_g[:, :], d_e[:, :], negA[:, 0:1], ALU.mult, ["d_e", "negA"], ["d_g"])
                S.add("dve", lambda e: e.tensor_tensor_scan(out=d_gam[:, :], data0=rm[0:4, :], data1=d_g[:, :], initial=0.0,
                                                            op0=ALU.mult, op1=ALU.add), reads=["rm", "d_g"], writes=["d_gam"])
                pt, pres = psr.next()
                fproj(pt, pres, 2484, 4)
                act(S, d_b[:, :], pt[:4, :TA], AF.Sigmoid, [pres], ["d_b"])
                act(S, d_eg[:, :], d_gam[:, :], AF.Exp, ["d_gam"], ["d_eg"])
                tt(S, "dve", d_bk[:, :], d_b[:, :], d_eg[:, :], ALU.mult, ["d_b", "d_eg"], ["d_bk"])
                for c in range(NCH):
                    ts(S, "dve", d_lg[:, c * 64:(c + 1) * 64], d_gam[:, c * 64:(c + 1) * 64], d_gam[:, c * 64 + 63:c * 64 + 64], ALU.subtract,
                       ["d_gam"], ["d_lg0"])
                act(S, d_lg[:, :], d_lg[:, :], AF.Exp, ["d_lg0"], ["d_lg"], scale=-1.0)
                pt, pres = psr.next()
                for h in range(4):
                    mm(S, pt[:64, h * NCH:(h + 1) * NCH], sel4[:, h, 0:64], d_eg[:, 63:TA:64], True, True, ["sel4", "d_eg"], [pres])
                cp(S, "act", d_elb[:, :, :], pt[:64, 0:4 * NCH].rearrange("p (h c) -> p h c", c=NCH), [pres], ["d_elb"])
                for h in range(4):
                    pt, pres = psr.next()
                    mm(S, pt[:, :TA], sel4[:, h, :], d_gam[:, :], True, True, ["sel4", "d_gam"], [pres])
                    cp(S, "act", d_gbc[:, h, :], pt[:, :TA], [pres], [("d_gbc", h)])
                    pt, pres = psr.next()
                    mm(S, pt[:64, :TA], sel4[:, h, 0:64], d_bk[:, :], True, True, ["sel4", "d_bk"], [pres])
                    tt(S, "dve", d_kp[:, h, :], pt[:64, :TA], d_y[:, 4 + h, :], ALU.mult, [pres, ("d_y", 4 + h)], [("d_kp", h)])
                    pt, pres = psr.next()
                    mm(S, pt[:64, :TA], sel4[:, h, 0:64], d_eg[:, :], True, True, ["sel4", "d_eg"], [pres])
                    tt(S, "dve", d_qB[:, h, :], pt[:64, :TA], d_y[:, h, :], ALU.mult, [pres, ("d_y", h)], [("d_qB0", h)])
                    tt(S, "pool", d_qA[:, h, :], d_qB[:, h, :], cmA[:64, :], ALU.mult, [("d_qB0", h), "cmA"], [("d_qA", h)])
                    tt(S, "pool", d_qB[:, h, :], d_qB[:, h, :], d_qA[:, h, :], ALU.subtract, [("d_qB0", h), ("d_qA", h)], [("d_qB", h)])
            for s in range(NSUB):
                ssl = slice(s * 128, (s + 1) * 128)
                if "sg" in do:
                    pu, pures = psr.next()
                    tproj(pu, pures, s, 416, 512)
                    act(S, sg_t[:, :], pu[:, :], AF.Square, [pures], ["sg_t"])
                    ts(S, "dve", sg_i[:, :], sg_t[:, :], 0.044715, ALU.mult, ["sg_t"], ["sg_i0"], s2=1.0, op1=ALU.add)
                    tt(S, "dve", sg_i[:, :], sg_i[:, :], pu[:, :], ALU.mult, ["sg_i0", pures], ["sg_i"])
                    act(S, sg_t[:, :], sg_i[:, :], AF.Sigmoid, ["sg_i"], ["sg_t2"], scale=1.5957691216057308)
                    tt(S, "dve", sg_g[:, :], sg_t[:, :], pu[:, :], ALU.mult, ["sg_t2", pures], ["sg_g"])
                    S.add("dve", lambda e: e.reduce_sum(out=sg_s[:, 0:1], in_=sg_g[:, 256:512], axis=mybir.AxisListType.X),
                          reads=["sg_g"], writes=["sg_s0"])
                    ts(S, "dve", sg_s[:, 1:2], sg_s[:, 0:1], -1.0 / 256, ALU.mult, ["sg_s0"], ["sg_s1"])
                    ts(S, "dve", sg_vc[:, :], sg_g[:, 256:512], sg_s[:, 1:2], ALU.add, ["sg_g", "sg_s1"], ["sg_vc"])
                    act(S, sg_junk[:, :], sg_vc[:, :], AF.Square, ["sg_vc"], ["sg_junk"], accum_out=sg_s[:, 2:3])
                    act(S, sg_s[:, 3:4], sg_s[:, 2:3], AF.Sqrt, ["sg_junk"], ["sg_s3"], scale=1.0 / 256, bias=EPS)
                    S.add("dve", lambda e: e.reciprocal(out=sg_s[:, 4:5], in_=sg_s[:, 3:4]), reads=["sg_s3"], writes=["sg_s4"])
                    stt(S, sg_vn[:, :], sg_vc[:, :], sg_s[:, 4:5], lng[:, :], ALU.mult, ALU.mult, ["sg_vc", "sg_s4", "bc_lng"], ["sg_vn0"])
                    tt(S, "dve", sg_vn[:, :], sg_vn[:, :], lnb[:, :], ALU.add, ["sg_vn0", "bc_lnb"], ["sg_vn"])
                    pt, pres = psr.next()
                    for g in range(4):
                        mm(S, pt[:, g * 64:(g + 1) * 64], WmT[:, g, :], sg_vn[:, g * 64:(g + 1) * 64], True, True, ["WmT", "sg_vn"], [pres])
                    for g in range(4):
                        stt(S, sg_o[:, g * 64:(g + 1) * 64], pt[:, g * 64:(g + 1) * 64], sgb[:, g:g + 1], sg_g[:, g * 64:(g + 1) * 64],
                            ALU.add, ALU.mult, [pres, "sgb", "sg_g"], ["sg_o"])
                    for c in range(2):
                        pt, pres = psr.next()
                        tr(S, pt[:, :128], sg_o[:, c * 128:(c + 1) * 128], ident[:, :], ["sg_o", "ident"], [pres])
                        cp(S, "act", obT[:, c, ssl], pt[:, :128], [pres], ["obT"])
                if "gla" in do:
                    pv, pvres = psr.next()
                    tproj(pv, pvres, s, 1184, 256, 0)
                    tproj(pv, pvres, s, 1456, 256, 256)
                    cp(S, "act", g_v[:, :], pv[:, 0:256], [pvres], ["g_v"])
                    act(S, g_og[:, :], pv[:, 256:512], AF.Silu, [pvres], ["g_og0"])
                    tt(S, "pool", g_og[:, :], g_og[:, :], gnb[:, :], ALU.mult, ["g_og0"] + [f"bc_gn{h}" for h in range(4)], ["g_og"])
                    pt, pres = psr.next()
                    tr(S, pt[:, :128], g_kl[:, ssl], ident[:, :], [("g_kl", s), "ident"], [pres])
                    ts(S, "dve", g_klA[:, :], pt[:, :128], topbot[:, 0:1], ALU.mult, [pres, "topbot"], ["g_klA"])
                    ts(S, "dve", g_klB[:, :], pt[:, :128], topbot[:, 1:2], ALU.mult, [pres, "topbot"], ["g_klB"])
                    scs = []
                    for h in range(4):
                        pt, pres = psr.next()
                        mm(S, pt[:, :128], g_ke[:, ssl], g_qp[:, h, ssl], True, True, ["g_ke", ("g_qp", h)], [pres])
                        sct, scr = g_sc.next()
                        tt(S, "dve", sct[:, :], pt[:, :128], mg[:, :], ALU.mult, [pres, "mg"], [scr])
                        scs.append((sct, scr))
                    po, pores = psr.next()
                    mm(S, po[:, :256], g_qA[:, ssl], g_S[:, :], True, False, ["g_qA", "g_S"], [pores])
                    for (klt, klr, isB) in ((g_klA, "g_klA", False), (g_klB, "g_klB", True)):
                        if isB:
                            mm(S, po[:, :256], g_qB[:, ssl], g_S[:, :], False, False, ["g_qB", "g_S"], [pores])
                        pt, pres = psr.next()
                        mm(S, pt[:, :256], klt[:, :], g_v[:, :], True, True, [klr, "g_v"], [pres])
                        tt(S, "dve", g_tmp[:, :], pt[:, :256], hm[:, :], ALU.mult, [pres, "hm"], ["g_tmp"])
                        cidx = s * 2 + (1 if isB else 0)
                        stt(S, g_S[:, :], g_S[:, :], g_ec[:, cidx * 64 + 63:cidx * 64 + 64], g_tmp[:, :], ALU.mult, ALU.add,
                            ["g_S", "g_ec", "g_tmp"], ["g_S"])
                    for h in range(4):
                        mm(S, po[:, h * 64:(h + 1) * 64], scs[h][0][:, :], g_v[:, h * 64:(h + 1) * 64], False, h == 3, [scs[h][1], "g_v"], [pores])
                    post_norm_gate(po, pores, g_og, "g_og", g_ss, g_junk, g_o, "g_", ocT, s)
                if "dn" in do and DNSTAGE >= 1:
                    pz, pzres = psr.next()
                    tproj(pz, pzres, s, 2488, 256, 0)
                    act(S, d_z[:, :], pz[:, 0:256], AF.Silu, [pzres], ["d_z0"])
                    tt(S, "pool", d_z[:, :], d_z[:, :], dnb[:, :], ALU.mult, ["d_z0"] + [f"bc_dnn{h}" for h in range(4)], ["d_z"])
                    pt, pres = psr.next()
                    mm(S, pt[:, 0:4], d_gam[:, ssl], ident[0:4, 0:4], True, True, ["d_gam", "ident"], [pres])
                    mm(S, pt[:, 4:8], d_b[:, ssl], ident[0:4, 0:4], True, True, ["d_b", "ident"], [pres])
                    mm(S, pt[:, 8:12], d_lg[:, ssl], ident[0:4, 0:4], True, True, ["d_lg", "ident"], [pres])
                    cp(S, "act", d_cols[:, :], pt[:, 0:12], [pres], ["d_cols"])
                    for h in range(4 if DNSTAGE >= 1.2 else 0):
                        kTh = d_y[:, 4 + h, ssl]
                        qTh = d_y[:, h, ssl]
                        vTh = d_y[:, 8 + h, ssl]
                        pt, pres = psr.next()
                        mm(S, pt[:, 0:64], kTh, ident[0:64, 0:64], True, True, [("d_y", 4 + h), "ident"], [pres])
                        mm(S, pt[:, 64:128], vTh, ident[0:64, 0:64], True, True, [("d_y", 8 + h), "ident"], [pres])
                        ts(S, "dve", d_Vp[:, h * 64:(h + 1) * 64], pt[:, 64:128], d_cols[:, 4 + h:5 + h], ALU.mult, [pres, "d_cols"], [("d_Vp", h)])
                        ts(S, "dve", d_junk[:, :], pt[:, 0:64], d_cols[:, 8 + h:9 + h], ALU.mult, [pres, "d_cols"], ["d_kdf"])
                        ts(S, "pool", d_kdA[:, h, :], d_junk[:, :], topbot[:, 0:1], ALU.mult, ["d_kdf", "topbot"], [("d_kdA", h)])
                        ts(S, "pool", d_kdB[:, h, :], d_junk[:, :], topbot[:, 1:2], ALU.mult, ["d_kdf", "topbot"], [("d_kdB", h)])
                        if DNSTAGE < 1.3:
                            continue
                        pkk, pkkres = psr.next()
                        mm(S, pkk[:, 0:128], kTh, kTh, True, True, [("d_y", 4 + h)], [pkkres])
                        mm(S, pkk[:, 128:256], kTh, qTh, True, True, [("d_y", 4 + h), ("d_y", h)], [pkkres])
                        d1, d1r = d_d1.next()
                        stt(S, d1[:, :], d_gbc[:, h, ssl], d_cols[:, h:h + 1], cU[:, :], ALU.subtract, ALU.max, [("d_gbc", h), "d_cols", "cU"], [d1r])
                        Dm, Dmr = d_Dm.next()
                        act(S, Dm[:, :], d1[:, :], AF.Exp, [d1r], [Dmr], scale=-1.0)
                        Bp, Bt, Rr = d_Bp[h], d_Bt[h], d_R[h]
                        stt(S, Bp[:, 0, :], pkk[:, 0:128], d_cols[:, 4 + h:5 + h], Dm[:, :], ALU.mult, ALU.mult, [pkkres, "d_cols", Dmr], [("Bp", h, 0)])
                        tt(S, "pool", Bp[:, 0, :], Bp[:, 0, :], cst[:, :], ALU.mult, [("Bp", h, 0), "cst"], [("Bp", h, 0)])
                        d2, d2r = d_d1.next()
                        stt(S, d2[:, :], d_gbc[:, h, ssl], d_cols[:, h:h + 1], cL[:, :], ALU.subtract, ALU.min, [("d_gbc", h), "d_cols", "cL"], [d2r])
                        DmT, DmTr = d_Dm.next()
                        act(S, DmT[:, :], d2[:, :], AF.Exp, [d2r], [DmTr])
                        tt(S, "dve", d_at[:, h, :], pkk[:, 128:256], DmT[:, :], ALU.mult, [pkkres, DmTr], [("d_at", h)])
                        if DNSTAGE < 1.4:
                            continue
                        pt, pres = psr.next()
                        mm(S, pt[:, :128], Bp[:, 0, :], ident[:, :], True, True, [("Bp", h, 0), "ident"], [pres])
                        if DNV != "a":
                            cp(S, "act", Bt[:, 0, :], pt[:, :128], [pres], [("Bt", h, 0)])
                        tt(S, "pool", Rr[:, :], ident[:, :], Bt[:, 0, :], ALU.subtract, [("Bt", h, 0), "ident"], [("R", h)])
                    for lev in range(1, 6 if DNSTAGE >= 2 else 1):
                        a, b = (lev - 1) % 2, lev % 2
                        for h in range(4):
                            Bp, Bt, Rr = d_Bp[h], d_Bt[h], d_R[h]
                            pt, pres = psr.next()
                            mm(S, pt[:, 0:128], Bt[:, a, :], Bp[:, a, :], True, True, [("Bt", h, a), ("Bp", h, a)], [pres])
                            if lev < 5:
                                mm(S, pt[:, 128:256], Bp[:, a, :], Bt[:, a, :], True, True, [("Bt", h, a), ("Bp", h, a)], [pres])
                            cp(S, "act", Bp[:, b, :], pt[:, 0:128], [pres], [("Bp", h, b)])
                            if lev < 5:
                                cp(S, "act", Bt[:, b, :], pt[:, 128:256], [pres], [("Bt", h, b)])
                            pt2, pres2 = psr.next()
                            mm(S, pt2[:, 0:128], Bp[:, b, :], Rr[:, :], True, True, [("Bp", h, b), ("R", h)], [pres2])
                            tt(S, "dve", Rr[:, :], Rr[:, :], pt2[:, 0:128], ALU.add, [pres2, ("R", h)], [("R", h)])
                    if DNSTAGE < 1.5:
                        continue
                    po, pores = psr.next()
                    mm(S, po[:, :256], zrow[0:1, 0:128], zrow[0:1, :], True, False, ["zrow"], [pores])
                    for ci, (kd, kdn, qd, qdn) in enumerate(((d_kdA, "d_kdA", d_qA, "d_qA"), (d_kdB, "d_kdB", d_qB, "d_qB")) if DNSTAGE >= 3 else ()):
                        rows = slice(ci * 64, (ci + 1) * 64)
                        pks, pksres = psr.next()
                        for h in range(4):
                            mm(S, pks[:, h * 64:(h + 1) * 64], d_kp[:, h, ssl], d_S[:, h, :], True, True, [("d_kp", h), "d_S"], [pksres])
                            mm(S, po[:, h * 64:(h + 1) * 64], qd[:, h, ssl], d_S[:, h, :], False, False, [(qdn, h), "d_S"], [pores])
                        tt(S, "dve", d_rhs2[:, :], d_Vp[:, :], pks[:, :256], ALU.subtract, [pksres] + [("d_Vp", h) for h in range(4)], ["d_rhs2"])
                        pu, pures = psr.next()
                        for h in range(4):
                            mm(S, pu[:, h * 64:(h + 1) * 64], d_R[h][:, :], d_rhs2[:, h * 64:(h + 1) * 64], True, True, [("R", h), "d_rhs2"], [pures])
                        cp(S, "act", d_u[rows, :], pu[rows, :256], [pures], ["d_u"])
                        psu, psures = psr.next()
                        for h in range(4):
                            mm(S, psu[:64, h * 64:(h + 1) * 64], kd[:, h, :], d_u[:, h * 64:(h + 1) * 64], True, True, [(kdn, h), "d_u"], [psures])
                        cidx = s * 2 + ci
                        for h in range(4):
                            stt(S, d_S[:, h, :], d_S[:, h, :], d_elb[:, h, cidx:cidx + 1], psu[:64, h * 64:(h + 1) * 64], ALU.mult, ALU.add,
                                ["d_S", "d_elb", psures], ["d_S"])
                    for h in range(4):
                        mm(S, po[:, h * 64:(h + 1) * 64], d_at[:, h, :], d_u[:, h * 64:(h + 1) * 64], False, h == 3, [("d_at", h), "d_u"], [pores])
                    post_norm_gate(po, pores, d_z, "d_z", d_ss, d_junk2, d_o, "d_", odT, s)
            if "sg" in do:
                dma(S, "sp", sc["oT_s"][0].rearrange("(c p) n -> p c n", p=128)[:, :, tok], obT[:, :, :], ["obT"], [("oT_s", 0, t)])
            if "gla" in do:
                dma(S, "sp", sc["oT_s"][1].rearrange("(c p) n -> p c n", p=128)[:, :, tok], ocT[:, :, :], ["g_oT"], [("oT_s", 1, t)])
            if "dn" in do:
                dma(S, "sp", sc["oT_s"][2].rearrange("(c p) n -> p c n", p=128)[:, :, tok], odT[:, :, :], ["d_oT"], [("oT_s", 2, t)])
    S.barrier()

PARAM_SHAPES = {
    "norm_mix": [NL, D], "w_in": [NL, D, INW], "mla_norm_q": [NL, 256], "mla_norm_kv": [NL, 128],
    "mla_w_uq": [NL, 256, 384], "mla_w_ukv": [NL, 128, 512], "sg_ln_g": [NL, 256], "sg_ln_b": [NL, 256],
    "sg_w": [NL, 4, 128, 128], "sg_b": [NL, 4, 128], "gla_w_gate": [NL, 16, 128], "gla_b_gate": [NL, 128],
    "gla_norm": [NL, 64], "dn_conv": [NL, 4, 768], "dn_a_log": [NL, 4], "dn_dt_bias": [NL, 4], "dn_norm": [NL, 64],
    "w_branch": [NL, 4, 256, D], "w_out": [NL, D, D], "norm_xattn": [NL, D], "norm_mem": [NL, D],
    "xattn_wq": [NL, D, D], "xattn_wk": [NL, D, D], "xattn_wv": [NL, D, D], "xattn_wo": [NL, D, D],
    "norm_mlp": [NL, D], "w_up": [NL, D, HID], "w_down": [NL, HID, D], "norm_final": [D],
}


def make_consts():
    p = np.arange(128)
    same = (p[:, None] // 64) == (p[None, :] // 64)
    c = {}
    c["ident"] = np.eye(128, dtype=np.float32)
    c["trilT"] = (p[:, None] <= p[None, :]).astype(np.float32)
    t = np.arange(TT)
    c["rm"] = np.broadcast_to((t % 64 != 0).astype(np.float32), (128, TT)).copy()
    c["mg"] = (same & (p[:, None] <= p[None, :])).astype(np.float32)
    c["hm"] = ((p[:, None] // 32) == (np.arange(256)[None, :] // 64)).astype(np.float32)
    c["hcol"] = ((p[:, None] // 32) == np.arange(4)[None, :]).astype(np.float32)
    c["topbot"] = np.stack([(p < 64), (p >= 64)], axis=1).astype(np.float32)
    c["cmA"] = np.broadcast_to(((t % 128) < 64).astype(np.float32), (128, TT)).copy()
    c["U"] = np.where(same & (p[None, :] <= p[:, None]), 0.0, 80.0).astype(np.float32)
    c["L"] = np.where(same & (p[None, :] >= p[:, None]), 0.0, -80.0).astype(np.float32)
    c["strict"] = (same & (p[None, :] < p[:, None])).astype(np.float32)
    sel = np.zeros((4, 4, 128), np.float32)
    for h in range(4):
        sel[h, h, :] = 1.0
    c["sel4"] = sel
    cc = np.zeros((96, 2), np.float32)
    f = (10000.0 ** (-np.arange(0, 32, 2, dtype=np.float32) / 32)).astype(np.float32)
    cc[64:80, 0] = f
    cc[80:96, 0] = f
    cc[64:80, 1] = -1.0
    cc[80:96, 1] = 1.0
    c["cconst"] = cc
    return c


CONST_SHAPES = {k: list(v.shape) for k, v in make_consts().items()}


def build(nl=NL, ntiles=NT, phases=("rope", "A", "mla", "merge", "xattn", "mlp", "final"), dbg=(), doA=("mla", "sg", "gla", "dn")):
    nc = bass.Bass("TRN2", target_bir_lowering=False)

    def dt(name, shape, dtype, kind):
        return nc.dram_tensor(name, shape, dtype, kind=kind).ap()

    xT = dt("xT", [D, SEQ], F32, "ExternalInput")
    memT = dt("memT", [D, 256], F32, "ExternalInput")
    posrep = dt("posrep", [96, SEQ], I32, "ExternalInput")
    P = {k: dt(k, s, F32, "ExternalInput") for k, s in PARAM_SHAPES.items()}
    K = {k: dt("c_" + k, s, F32, "ExternalInput") for k, s in CONST_SHAPES.items()}
    outT = dt("outT", [D, SEQ], F32, "ExternalOutput")

    def scr(name, shape, dtype):
        return dt(name, shape, dtype, "ExternalOutput" if name in dbg else "Internal")

    sc = {
        "hT_s": scr("hT_s", [D, SEQ], BF16), "qT_s": scr("qT_s", [4, 96, SEQ], BF16), "kT_s": scr("kT_s", [4, 96, SEQ], BF16),
        "v_s": scr("v_s", [SEQ, 260], BF16), "oaT_s": scr("oaT_s", [4, 64, SEQ], BF16), "oT_s": scr("oT_s", [3, 256, SEQ], BF16),
        "cos_s": scr("cos_s", [96, SEQ], F32), "sin_s": scr("sin_s", [96, SEQ], F32),
        "xa": scr("xa", [D, SEQ], F32), "xb": scr("xb", [D, SEQ], F32), "xc": scr("xc", [D, SEQ], F32),
    }
    S = Sched(nc)
    if "rope" in phases:
        phase_rope(S, nc, posrep, K["cconst"], sc["cos_s"], sc["sin_s"])
    cur = xT
    for l in range(nl):
        if "A" in phases:
            phase_A(S, nc, l, cur, P, K, sc, ntiles=(None if ntiles == NT else 2 * ntiles), do=doA)
        if "mla" in phases:
            phase_mla(S, nc, sc["qT_s"], sc["kT_s"], sc["v_s"], sc["oaT_s"], K["trilT"], nq=ntiles)
        if "merge" in phases:
            phase_merge(S, nc, l, cur, sc["xa"], sc["hT_s"], sc["oaT_s"], sc["oT_s"], P["w_in"], P["w_branch"], P["w_out"], ntiles=ntiles)
            cur = sc["xa"]
        if "xattn" in phases:
            phase_xattn(S, nc, l, cur, sc["xb"], memT, P["norm_xattn"], P["norm_mem"], P["xattn_wq"], P["xattn_wk"], P["xattn_wv"], P["xattn_wo"], ntiles=ntiles)
            cur = sc["xb"]
        if "mlp" in phases:
            phase_mlp(S, nc, l, cur, sc["xc"], P["norm_mlp"], P["w_up"], P["w_down"], ntiles=ntiles)
            cur = sc["xc"]
    if "final" in phases:
        phase_final(S, nc, cur, outT, P["norm_final"], ntiles=ntiles)
    S.emit()
    return nc


def make_in_maps(inputs, cores):
    consts = make_consts()
    x = np.asarray(inputs["x"], dtype=np.float32)
    mem = np.asarray(inputs["mem"], dtype=np.float32)
    pos = np.asarray(inputs["positions"]).astype(np.int32)
    shared = {k: np.ascontiguousarray(np.asarray(inputs[k], dtype=np.float32)) for k in PARAM_SHAPES}
    for k, v in consts.items():
        shared["c_" + k] = v
    in_maps = []
    for b in cores:
        m = dict(shared)
        m["xT"] = np.ascontiguousarray(x[b].T)
        m["memT"] = np.ascontiguousarray(mem[b].T)
        m["posrep"] = np.ascontiguousarray(np.broadcast_to(pos[b][None, :], (96, SEQ)))
        in_maps.append(m)
    return in_maps


def kernel(**inputs):
    B = np.asarray(inputs["x"]).shape[0]
    nc = build()
    in_maps = make_in_maps(inputs, list(range(B)))
    res = run_bass_kernel_spmd(nc, in_maps, core_ids=list(range(B)))
    out = np.stack([np.ascontiguousarray(np.asarray(r["outT"]).T) for r in res.results], axis=0)
    return out.astype(np.float32)
```

```python
from contextlib import ExitStack
import numpy as np
import concourse.bass as bass
import concourse.mybir as mybir
from concourse.bass_utils import run_bass_kernel_spmd

F32 = mybir.dt.float32
BF16 = mybir.dt.bfloat16
I32 = mybir.dt.int32
AF = mybir.ActivationFunctionType
ALU = mybir.AluOpType

D = 1024
SEQ = 4096
NL = 2
TT = 512
NT = SEQ // TT
EPS = 1e-6
HID = 4096
INW = 6840
DBG = 9
DNSTAGE = 9
DNV = ''


class Sched:
    ENGS = ("pe", "act", "dve", "pool", "sp")
    EPOCH = 12000
    RING = 8

    def __init__(self, nc):
        self.nc = nc
        self.stream = {e: [] for e in self.ENGS}
        self.vt = {e: 0 for e in self.ENGS}
        self.known = {e: {} for e in self.ENGS}
        self.need_sig = {e: set() for e in self.ENGS}
        self.last_write = {}
        self.readers = {}
        self.op_sig = []
        self.op_clock = []
        self.op_eng = []
        self.op_dma = []
        self.dma_n = {e: 0 for e in self.ENGS}
        self.last_sig = {}

    def _emit_wait(self, eng, key, val):
        self.stream[eng].append(("wait", key, val))
        if key[0] == "eng":
            self.need_sig[key[1]].add(val)

    def _wait(self, eng, dep):
        key, val = self.op_sig[dep]
        kn = self.known[eng]
        if kn.get(key, 0) >= val:
            return
        self._emit_wait(eng, key, val)
        for k, v in self.op_clock[dep].items():
            if kn.get(k, 0) < v:
                kn[k] = v
        kn[key] = max(kn.get(key, 0), val)

    def add(self, eng, fn, reads=(), writes=(), signal=True, dma=False):
        deps = set()
        for r in reads:
            w = self.last_write.get(r)
            if w is not None:
                deps.add(w)
        for w_ in writes:
            w = self.last_write.get(w_)
            if w is not None:
                deps.add(w)
            for r in self.readers.get(w_, ()):
                deps.add(r)
        opid = len(self.op_sig)
        for d in sorted(deps):
            if (not dma) and (not self.op_dma[d]) and self.op_eng[d] == eng:
                if eng == "pe":
                    continue
                israw = any(self.last_write.get(r) == d for r in reads)
                if not israw:
                    continue
            self._wait(eng, d)
        if dma:
            n = self.dma_n[eng]
            self.dma_n[eng] += 1
            slot = n % self.RING
            key = ("dma", eng, slot)
            val = 16 * (n // self.RING + 1)
            if val > 16:
                kn = self.known[eng]
                if kn.get(key, 0) < val - 16:
                    self._emit_wait(eng, key, val - 16)
                    kn[key] = val - 16
            self.stream[eng].append(("op", fn, None, key))
        else:
            self.vt[eng] += 1
            key = ("eng", eng)
            val = self.vt[eng]
            self.stream[eng].append(("op", fn, val, None))
        clock = dict(self.known[eng])
        clock[key] = val
        self.op_sig.append((key, val))
        self.op_clock.append(clock)
        self.op_eng.append(eng)
        self.op_dma.append(dma)
        self.last_sig[key] = max(self.last_sig.get(key, 0), val)
        for r in reads:
            self.readers.setdefault(r, []).append(opid)
        for w_ in writes:
            self.last_write[w_] = opid
            self.readers[w_] = []
        return opid

    def barrier(self):
        for eng in self.ENGS:
            kn = self.known[eng]
            for key, val in list(self.last_sig.items()):
                if kn.get(key, 0) < val:
                    self._emit_wait(eng, key, val)
                    kn[key] = val
        self.last_write = {}
        self.readers = {}

    def emit(self):
        nc = self.nc
        vmap = {}
        semkeys = set()
        for e in self.ENGS:
            cnt = 0
            ep = 0
            for vt in sorted(self.need_sig[e]):
                if cnt >= self.EPOCH:
                    ep += 1
                    cnt = 0
                cnt += 1
                vmap[(e, vt)] = (("eng", e, ep), cnt)
                semkeys.add(("eng", e, ep))
            for it in self.stream[e]:
                if it[0] == "op" and it[3] is not None:
                    semkeys.add(it[3])
        with ExitStack() as es:
            sems = {}
            for key in sorted(semkeys, key=str):
                sems[key] = es.enter_context(nc.semaphore("s_" + "_".join(str(k) for k in key)))
            block = es.enter_context(nc.Block())
            engmap = {"pe": block.tensor, "act": block.scalar, "dve": block.vector,
                      "pool": block.gpsimd, "sp": block.sync}
            for ename in self.ENGS:
                items = self.stream[ename]

                def body(eng, items=items, ename=ename):
                    for it in items:
                        if it[0] == "wait":
                            key, val = it[1], it[2]
                            if key[0] == "eng":
                                key, val = vmap[(key[1], val)]
                            eng.wait_ge(sems[key], val)
                        else:
                            ins = it[1](eng)
                            if it[3] is not None:
                                ins.then_inc(sems[it[3]], 16)
                            elif (ename, it[2]) in vmap:
                                ins.then_inc(sems[vmap[(ename, it[2])][0]], 1)
                engmap[ename](body)


class Rot:
    def __init__(self, es, alloc, name, shape, dtype, n):
        self.tiles = [es.enter_context(alloc(f"{name}{i}", shape, dtype)) for i in range(n)]
        self.name = name
        self.i = 0

    def next(self):
        k = self.i % len(self.tiles)
        self.i += 1
        return self.tiles[k], (self.name, k)


def mm(S, out, lhsT, rhs, start, stop, r, w):
    S.add("pe", lambda e: e.matmul(out, lhsT, rhs, start=start, stop=stop), reads=r, writes=w)


def tr(S, out, in_, ident, r, w):
    S.add("pe", lambda e: e.transpose(out, in_, ident), reads=r, writes=w)


def act(S, out, in_, func, r, w, **kw):
    S.add("act", lambda e: e.activation(out=out, in_=in_, func=func, **kw), reads=r, writes=w)


def tt(S, eng, out, a, b, op, r, w):
    S.add(eng, lambda e: e.tensor_tensor(out=out, in0=a, in1=b, op=op), reads=r, writes=w)


def ts(S, eng, out, a, s1, op0, r, w, s2=None, op1=None):
    if op1 is None:
        S.add(eng, lambda e: e.tensor_scalar(out=out, in0=a, scalar1=s1, scalar2=None, op0=op0), reads=r, writes=w)
    else:
        S.add(eng, lambda e: e.tensor_scalar(out=out, in0=a, scalar1=s1, scalar2=s2, op0=op0, op1=op1), reads=r, writes=w)


def stt(S, out, a, scalar, b, op0, op1, r, w):
    S.add("dve", lambda e: e.scalar_tensor_tensor(out=out, in0=a, scalar=scalar, in1=b, op0=op0, op1=op1), reads=r, writes=w)


def cp(S, eng, out, in_, r, w):
    if eng == "act":
        S.add("act", lambda e: e.activation(out=out, in_=in_, func=AF.Copy), reads=r, writes=w)
    else:
        S.add(eng, lambda e: e.tensor_copy(out=out, in_=in_), reads=r, writes=w)


def dma(S, q, out, in_, r, w, slow=False):
    if slow:
        S.add(q, lambda e: e.dma_start(out=out, in_=in_, allow_slow_non_contiguous=True), reads=r, writes=w, dma=True)
    else:
        S.add(q, lambda e: e.dma_start(out=out, in_=in_), reads=r, writes=w, dma=True)


def memset(S, eng, ap, val, w, r=()):
    S.add(eng, lambda e: e.memset(ap, val), reads=r, writes=w)


class Ctx:
    count = 0

    def __init__(self, nc, S, es, prefix):
        Ctx.count += 1
        self.nc, self.S, self.es, self.p = nc, S, es, f"{prefix}{Ctx.count}_"
        self.n = 0

    def sb(self, name, shape, dtype=F32):
        return self.es.enter_context(self.nc.sbuf_tensor(self.p + name, shape, dtype))

    def ps(self, name, shape, dtype=F32):
        return self.es.enter_context(self.nc.psum_tensor(self.p + name, shape, dtype))

    def rot(self, name, shape, dtype, n, psum=False):
        alloc = (lambda a, b, c: self.nc.psum_tensor(a, b, c)) if psum else (lambda a, b, c: self.nc.sbuf_tensor(a, b, c))
        return Rot(self.es, alloc, self.p + name, shape, dtype, n)


def rms_fm(S, src, src_res, nk, nfeat, gcol, ones_bf, sq_rot, psr, tmp, tmp_res, rstd, rstd_res, dst, dst_res, width, pn=128):
    pt, pres = psr.next()
    for k in range(nk):
        sq, sqr = sq_rot.next()
        act(S, sq[:pn, :width], src[:pn, k, :width], AF.Square, [src_res[k]], [sqr])
        mm(S, pt[:pn, :width], ones_bf[:pn, :pn], sq[:pn, :width], k == 0, k == nk - 1, [sqr, "ones"], [pres])
    act(S, tmp[:pn, :width], pt[:pn, :width], AF.Sqrt, [pres], [tmp_res], scale=1.0 / nfeat, bias=EPS)
    S.add("dve", lambda e: e.reciprocal(out=rstd[:pn, :width], in_=tmp[:pn, :width]), reads=[tmp_res], writes=[rstd_res])
    for k in range(nk):
        stt(S, dst[:pn, k, :width], src[:pn, k, :width], gcol[:pn, k:k + 1], rstd[:pn, :width], ALU.mult, ALU.mult,
            [src_res[k], rstd_res], [dst_res[k]])


def phase_mlp(S, nc, l, xin, xout, norm_mlp, w_up, w_down, ntiles=NT, wq_="sp"):
    with ExitStack() as es:
        C = Ctx(nc, S, es, "m_")
        ones_bf = C.sb("ones", [128, 128], BF16)
        gcol = C.sb("g", [128, 8])
        xt_rot = C.rot("xt", [128, 8, TT], F32, 2)
        sq_rot = C.rot("sq", [128, TT], BF16, 2)
        hT = C.sb("hT", [128, 8, TT], BF16)
        tmp = C.sb("tmp", [128, TT])
        rstd = C.sb("rstd", [128, TT])
        r_rot = C.rot("r", [128, TT], BF16, 3)
        aT = C.sb("aT", [128, 32, TT], BF16)
        wup_rot = C.rot("wup", [128, 8, 512], BF16, 2)
        wdn_rot = C.rot("wdn", [128, 32, 256], BF16, 2)
        psr = C.rot("ps", [128, TT], F32, 7, psum=True)
        memset(S, "pool", ones_bf[:, :], 1.0, ["ones"])
        dma(S, "sp", gcol[:, :], norm_mlp[l].rearrange("(c p) -> p c", p=128), [], ["g"], slow=True)
        xin_v = xin.rearrange("(c p) n -> p c n", p=128)
        xout_v = xout.rearrange("(c p) n -> p c n", p=128)
        wup_v = w_up[l].rearrange("(c p) n -> p c n", p=128)
        wdn_v = w_down[l].rearrange("(c p) n -> p c n", p=128)
        for t in range(ntiles):
            tok = slice(t * TT, (t + 1) * TT)
            xt, x0 = xt_rot.next()
            xres = [(x0, d) for d in range(8)]
            dma(S, "sp", xt[:, :, :], xin_v[:, :, tok], [("xin", t)], xres)
            hres = [("m_hT", k) for k in range(8)]
            rms_fm(S, xt, xres, 8, D, gcol, ones_bf, sq_rot, psr, tmp, "m_tmp", rstd, "m_rstd", hT, hres, TT)
            for jb in range(8):
                wu, wures = wup_rot.next()
                dma(S, wq_, wu[:, :, :], wup_v[:, :, jb * 512:(jb + 1) * 512], [], [wures])
                for jj in range(4):
                    j = jb * 4 + jj
                    pt, pres = psr.next()
                    for k in range(8):
                        mm(S, pt[:, :], wu[:, k, jj * 128:(jj + 1) * 128], hT[:, k, :], k == 0, k == 7, [wures, hres[k]], [pres])
                    r, rres = r_rot.next()
                    act(S, r[:, :], pt[:, :], AF.Relu, [pres], [rres])
                    tt(S, "dve", aT[:, j, :], r[:, :], r[:, :], ALU.mult, [rres], [("m_aT", j)])
            for db in range(4):
                wd, wdres = wdn_rot.next()
                dma(S, wq_, wd[:, :, :], wdn_v[:, :, db * 256:(db + 1) * 256], [], [wdres])
                for dd in range(2):
                    d = db * 2 + dd
                    pt, pres = psr.next()
                    for j in range(32):
                        mm(S, pt[:, :], wd[:, j, dd * 128:(dd + 1) * 128], aT[:, j, :], j == 0, j == 31, [wdres, ("m_aT", j)], [pres])
                    tt(S, "dve", xt[:, d, :], xt[:, d, :], pt[:, :], ALU.add, [pres, xres[d]], [xres[d]])
            dma(S, "sp", xout_v[:, :, tok], xt[:, :, :], xres, [("xout", t)])
    S.barrier()


def phase_final(S, nc, xin, xout, norm_final, ntiles=NT):
    with ExitStack() as es:
        C = Ctx(nc, S, es, "f_")
        ones_bf = C.sb("ones", [128, 128], BF16)
        gcol = C.sb("g", [128, 8])
        xt_rot = C.rot("xt", [128, 8, TT], F32, 2)
        ot_rot = C.rot("ot", [128, 8, TT], F32, 2)
        sq_rot = C.rot("sq", [128, TT], BF16, 2)
        tmp = C.sb("tmp", [128, TT])
        rstd = C.sb("rstd", [128, TT])
        psr = C.rot("ps", [128, TT], F32, 2, psum=True)
        memset(S, "pool", ones_bf[:, :], 1.0, ["ones"])
        dma(S, "sp", gcol[:, :], norm_final.rearrange("(c p) -> p c", p=128), [], ["g"], slow=True)
        xin_v = xin.rearrange("(c p) n -> p c n", p=128)
        xout_v = xout.rearrange("(c p) n -> p c n", p=128)
        for t in range(ntiles):
            tok = slice(t * TT, (t + 1) * TT)
            xt, x0 = xt_rot.next()
            xres = [(x0, d) for d in range(8)]
            dma(S, "sp", xt[:, :, :], xin_v[:, :, tok], [("xin", t)], xres)
            ot, o0 = ot_rot.next()
            ores = [(o0, d) for d in range(8)]
            rms_fm(S, xt, xres, 8, D, gcol, ones_bf, sq_rot, psr, tmp, "f_tmp", rstd, "f_rstd", ot, ores, TT)
            dma(S, "sp", xout_v[:, :, tok], ot[:, :, :], ores, [("xout", t)])
    S.barrier()


def phase_xattn(S, nc, l, xin, xout, memT, norm_x, norm_mem, wq, wk, wv, wo, ntiles=NT):
    with ExitStack() as es:
        C = Ctx(nc, S, es, "x_")
        ones_bf = C.sb("ones", [128, 128], BF16)
        gx = C.sb("gx", [128, 8])
        gm = C.sb("gm", [128, 8])
        mt = C.sb("mt", [128, 8, 256])
        mn = C.sb("mn", [128, 8, 256], BF16)
        wq_t = C.sb("wq", [128, 8, 1024], BF16)
        wo_t = C.sb("wo", [128, 8, 1024], BF16)
        wtmp_rot = C.rot("wtmp", [128, 8, 1024], BF16, 1)
        KT = C.sb("KT", [128, 8, 256], BF16)
        V = C.sb("V", [128, 2, 1024], BF16)
        xt_rot = C.rot("xt", [128, 8, TT], F32, 2)
        sq_rot = C.rot("sq", [128, TT], BF16, 2)
        hT = C.sb("hT", [128, 8, TT], BF16)
        qT = C.sb("qT", [128, 8, TT], BF16)
        oT = C.sb("oT", [128, 8, TT], BF16)
        pT_rot = C.rot("pT", [128, 2, TT], BF16, 2)
        tmp = C.sb("tmp", [128, TT])
        rstd = C.sb("rstd", [128, TT])
        rinv = C.sb("rinv", [128, TT])
        psr = C.rot("ps", [128, TT], F32, 7, psum=True)
        memset(S, "pool", ones_bf[:, :], 1.0, ["ones"])
        dma(S, "sp", gx[:, :], norm_x[l].rearrange("(c p) -> p c", p=128), [], ["gx"], slow=True)
        dma(S, "sp", gm[:, :], norm_mem[l].rearrange("(c p) -> p c", p=128), [], ["gm"], slow=True)
        dma(S, "sp", mt[:, :, :], memT.rearrange("(c p) n -> p c n", p=128), [], [("mt", k) for k in range(8)])
        dma(S, "pool", wq_t[:, :, :], wq[l].rearrange("(c p) n -> p c n", p=128), [], ["wq"])
        dma(S, "pool", wo_t[:, :, :], wo[l].rearrange("(c p) n -> p c n", p=128), [], ["wo"])
        mres = [("mn", k) for k in range(8)]
        rms_fm(S, mt, [("mt", k) for k in range(8)], 8, D, gm, ones_bf, sq_rot, psr, tmp, "x_tmp", rstd, "x_rstd", mn, mres, 256)
        wt, wres = wtmp_rot.next()
        dma(S, "pool", wt[:, :, :], wk[l].rearrange("(c p) n -> p c n", p=128), [], [wres])
        for c in range(8):
            pt, pres = psr.next()
            for k in range(8):
                mm(S, pt[:, :256], wt[:, k, c * 128:(c + 1) * 128], mn[:, k, :], k == 0, k == 7, [wres, mres[k]], [pres])
            cp(S, "act", KT[:, c, :], pt[:, :256], [pres], [("KT", c)])
        wt, wres = wtmp_rot.next()
        dma(S, "pool", wt[:, :, :], wv[l].rearrange("(c p) n -> p c n", p=128), [], [wres])
        for ms in range(2):
            for cb in range(2):
                pt, pres = psr.next()
                for k in range(8):
                    mm(S, pt[:, :], mn[:, k, ms * 128:(ms + 1) * 128], wt[:, k, cb * 512:(cb + 1) * 512], k == 0, k == 7, [wres, mres[k]], [pres])
                cp(S, "act", V[:, ms, cb * 512:(cb + 1) * 512], pt[:, :], [pres], [("V", ms, cb)])
        xin_v = xin.rearrange("(c p) n -> p c n", p=128)
        xout_v = xout.rearrange("(c p) n -> p c n", p=128)
        for t in range(ntiles):
            tok = slice(t * TT, (t + 1) * TT)
            xt, x0 = xt_rot.next()
            xres = [(x0, d) for d in range(8)]
            dma(S, "sp", xt[:, :, :], xin_v[:, :, tok], [("xin", t)], xres)
            hres = [("x_hT", k) for k in range(8)]
            rms_fm(S, xt, xres, 8, D, gx, ones_bf, sq_rot, psr, tmp, "x_tmp", rstd, "x_rstd", hT, hres, TT)
            for c in range(8):
                pt, pres = psr.next()
                for k in range(8):
                    mm(S, pt[:, :], wq_t[:, k, c * 128:(c + 1) * 128], hT[:, k, :], k == 0, k == 7, ["wq", hres[k]], [pres])
                cp(S, "act" if c % 2 else "dve", qT[:, c, :], pt[:, :], [pres], [("qT", c)])
            for h in range(4):
                pT, pTres = pT_rot.next()
                for ms in range(2):
                    pt, pres = psr.next()
                    for cc in range(2):
                        c = h * 2 + cc
                        mm(S, pt[:, :], KT[:, c, ms * 128:(ms + 1) * 128], qT[:, c, :], cc == 0, cc == 1, [("KT", c), ("qT", c)], [pres])
                    act(S, pT[:, ms, :], pt[:, :], AF.Exp, [pres], [(pTres, ms)], scale=1.0 / 16.0)
                pt, pres = psr.next()
                for ms in range(2):
                    mm(S, pt[:, :], ones_bf[:, :], pT[:, ms, :], ms == 0, ms == 1, ["ones", (pTres, ms)], [pres])
                S.add("dve", lambda e, pt=pt: e.reciprocal(out=rinv[:, :], in_=pt[:, :]), reads=[pres], writes=["rinv"])
                for cc in range(2):
                    c = h * 2 + cc
                    pt, pres = psr.next()
                    for ms in range(2):
                        mm(S, pt[:, :], V[:, ms, c * 128:(c + 1) * 128], pT[:, ms, :], ms == 0, ms == 1,
                           [("V", ms, c // 4), (pTres, ms)], [pres])
                    tt(S, "dve", oT[:, c, :], pt[:, :], rinv[:, :], ALU.mult, [pres, "rinv"], [("oT", c)])
            for d in range(8):
                pt, pres = psr.next()
                for c in range(8):
                    mm(S, pt[:, :], wo_t[:, c, d * 128:(d + 1) * 128], oT[:, c, :], c == 0, c == 7, ["wo", ("oT", c)], [pres])
                tt(S, "dve", xt[:, d, :], xt[:, d, :], pt[:, :], ALU.add, [pres, xres[d]], [xres[d]])
            dma(S, "sp", xout_v[:, :, tok], xt[:, :, :], xres, [("xout", t)])
    S.barrier()


def phase_merge(S, nc, l, xin, xout, hT_s, oaT_s, oT_s, wg_bf, w_branch, w_out, ntiles=NT):
    with ExitStack() as es:
        C = Ctx(nc, S, es, "g_")
        wba = C.sb("wba", [64, 4, 1024], BF16)
        wb = C.sb("wb", [128, 3, 2, 1024], BF16)
        wout = C.sb("wout", [128, 8, 1024], BF16)
        wg_rot = C.rot("wg", [128, 8, 1024], BF16, 2)
        xt_rot = C.rot("xt", [128, 8, TT], F32, 2)
        h_rot = C.rot("h", [128, 8, TT], BF16, 2)
        oa_rot = C.rot("oa", [64, 4, TT], BF16, 2)
        ob_rot = C.rot("ob", [128, 3, 2, TT], BF16, 2)
        sig_rot = C.rot("sig", [128, TT], BF16, 3)
        prod_rot = C.rot("prod", [128, TT], F32, 3)
        acc = C.sb("acc", [128, 8, TT])
        mT = C.sb("mT", [128, 8, TT], BF16)
        psr = C.rot("ps", [128, TT], F32, 7, psum=True)
        dma(S, "pool", wba[:, :, :], w_branch[l, 0].rearrange("(h p) n -> p h n", p=64), [], ["wba"])
        for n in range(3):
            dma(S, "pool", wb[:, n, :, :], w_branch[l, n + 1].rearrange("(c p) n -> p c n", p=128), [], [("wb", n)])
        dma(S, "pool", wout[:, :, :], w_out[l].rearrange("(c p) n -> p c n", p=128), [], ["wout"])
        xin_v = xin.rearrange("(c p) n -> p c n", p=128)
        xout_v = xout.rearrange("(c p) n -> p c n", p=128)
        hv = hT_s.rearrange("(c p) n -> p c n", p=128)
        win_v = wg_bf[l].rearrange("(c p) n -> p c n", p=128)
        for t in range(ntiles):
            tok = slice(t * TT, (t + 1) * TT)
            xt, x0 = xt_rot.next()
            xres = [(x0, d) for d in range(8)]
            dma(S, "sp", xt[:, :, :], xin_v[:, :, tok], [("xin", t)], xres)
            ht, hres = h_rot.next()
            dma(S, "sp", ht[:, :, :], hv[:, :, tok], [("hT_s", t)], [hres])
            oa, oares = oa_rot.next()
            dma(S, "sp", oa[:, :, :], oaT_s.rearrange("h p n -> p h n")[:, :, tok], [("oaT_s", t)], [oares])
            ob, obres = ob_rot.next()
            for n in range(3):
                dma(S, "sp", ob[:, n, :, :], oT_s[n].rearrange("(c p) n -> p c n", p=128)[:, :, tok], [("oT_s", n, t)], [(obres, n)])
            for n in range(4):
                wg, wgres = wg_rot.next()
                dma(S, "sp", wg[:, :, :], win_v[:, :, n * 1024:(n + 1) * 1024], [], [wgres])
                for d in range(8):
                    pt, pres = psr.next()
                    for k in range(8):
                        mm(S, pt[:, :], wg[:, k, d * 128:(d + 1) * 128], ht[:, k, :], k == 0, k == 7, [wgres, hres], [pres])
                    sg, sgres = sig_rot.next()
                    act(S, sg[:, :], pt[:, :], AF.Sigmoid, [pres], [sgres])
                    pt2, pres2 = psr.next()
                    if n == 0:
                        for h in range(4):
                            mm(S, pt2[:, :], wba[:, h, d * 128:(d + 1) * 128], oa[:, h, :], h == 0, h == 3, ["wba", oares], [pres2])
                    else:
                        for c in range(2):
                            mm(S, pt2[:, :], wb[:, n - 1, c, d * 128:(d + 1) * 128], ob[:, n - 1, c, :], c == 0, c == 1,
                               [("wb", n - 1), (obres, n - 1)], [pres2])
                    if n == 0:
                        tt(S, "dve", acc[:, d, :], pt2[:, :], sg[:, :], ALU.mult, [pres2, sgres], [("acc", d)])
                    else:
                        pr, prres = prod_rot.next()
                        tt(S, "dve", pr[:, :], pt2[:, :], sg[:, :], ALU.mult, [pres2, sgres], [prres])
                        if n < 3:
                            tt(S, "pool", acc[:, d, :], acc[:, d, :], pr[:, :], ALU.add, [("acc", d), prres], [("acc", d)])
                        else:
                            tt(S, "pool", mT[:, d, :], acc[:, d, :], pr[:, :], ALU.add, [("acc", d), prres], [("mT", d)])
            for d in range(8):
                pt, pres = psr.next()
                for c in range(8):
                    mm(S, pt[:, :], wout[:, c, d * 128:(d + 1) * 128], mT[:, c, :], c == 0, c == 7, ["wout", ("mT", c)], [pres])
                tt(S, "dve", xt[:, d, :], xt[:, d, :], pt[:, :], ALU.add, [pres, xres[d]], [xres[d]])
            dma(S, "sp", xout_v[:, :, tok], xt[:, :, :], xres, [("xout", t)])
    S.barrier()


def phase_mla(S, nc, qT_s, kT_s, v_s, oaT_s, cmask_d, nq=NT):
    with ExitStack() as es:
        C = Ctx(nc, S, es, "a_")
        KT = C.sb("KT", [96, SEQ], BF16)
        V = C.sb("V", [128, SEQ // 128, 65], BF16)
        q_rot = C.rot("q", [96, TT], BF16, 2)
        pT_rot = C.rot("pT", [128, TT], BF16, 4)
        cm = C.sb("cm", [128, 128], BF16)
        ones1 = C.sb("ones1", [65, 64])
        oS = C.sb("oS", [65, TT])
        rinv = C.sb("rinv", [65, TT])
        o_rot = C.rot("o", [64, TT], BF16, 2)
        psr = C.rot("ps", [128, TT], F32, 4, psum=True)
        pso = C.rot("pso", [128, TT], F32, 2, psum=True)
        psb = C.rot("psb", [128, TT], F32, 1, psum=True)
        dma(S, "pool", cm[:, :], cmask_d, [], ["cm"])
        memset(S, "pool", ones1[:, :], 1.0, ["ones1"])
        scale = float(96 ** -0.5)
        for h in range(4):
            dma(S, "sp", KT[:, :], kT_s[h], [("kT_s",)], ["KT"])
            dma(S, "sp", V[:, :, :], v_s.rearrange("(n p) (h e) -> p n h e", p=128, e=65)[:, :, h, :], [("v_s",)], ["V"], slow=True)
            for qt in range(nq):
                q, qres = q_rot.next()
                dma(S, "sp", q[:, :], qT_s[h][:, qt * TT:(qt + 1) * TT], [("qT_s",)], [qres])
                po, pores = pso.next()
                nkb = 4 * qt + 4
                for kb in range(nkb):
                    r = kb - 4 * qt
                    q0 = max(r, 0) * 128
                    pt, pres = psr.next()
                    mm(S, pt[:, q0:], KT[:, kb * 128:(kb + 1) * 128], q[:, q0:], True, True, ["KT", qres], [pres])
                    pT, pTres = pT_rot.next()
                    act(S, pT[:, q0:], pt[:, q0:], AF.Exp, [pres], [pTres], scale=scale)
                    if r >= 0:
                        tt(S, "pool", pT[:, q0:q0 + 128], pT[:, q0:q0 + 128], cm[:, :], ALU.mult, [pTres, "cm"], [pTres])
                    mm(S, po[:65, q0:], V[:, kb, :], pT[:, q0:], kb == 0, kb == nkb - 1, ["V", pTres], [pores])
                cp(S, "act", oS[:, :], po[:65, :], [pores], ["oS"])
                S.add("dve", lambda e: e.reciprocal(out=rinv[64:65, :], in_=oS[64:65, :]), reads=["oS"], writes=["rinv"])
                pb, pbres = psb.next()
                mm(S, pb[:64, :], ones1[64:65, :], rinv[64:65, :], True, True, ["ones1", "rinv"], [pbres])
                o, ores = o_rot.next()
                tt(S, "dve", o[:, :], pb[:64, :], oS[:64, :], ALU.mult, [pbres, "oS"], [ores])
                dma(S, "sp", oaT_s[h][:, qt * TT:(qt + 1) * TT], o[:, :], [ores], [("oaT_s", qt)])
    S.barrier()


def phase_precast(S, nc, P, sc):
    with ExitStack() as es:
        C = Ctx(nc, S, es, "pc_")
        st_rot = C.rot("st", [128, 8, 512], F32, 3)
        bf_rot = C.rot("bf", [128, 8, 512], BF16, 3)
        n = 0
        for l in range(NL):
            jobs = []
            for jb in range(8):
                jobs.append((P["w_up"][l].rearrange("(c p) n -> p c n", p=128)[:, :, jb * 512:(jb + 1) * 512],
                             sc["wup_bf"][l].rearrange("(c p) n -> p c n", p=128)[:, :, jb * 512:(jb + 1) * 512]))
                jobs.append((P["w_in"][l].rearrange("(c p) n -> p c n", p=128)[:, :, 2744 + jb * 512:2744 + (jb + 1) * 512],
                             sc["wg_bf"][l].rearrange("(c p) n -> p c n", p=128)[:, :, jb * 512:(jb + 1) * 512]))
            for cb in range(4):
                for nb in range(2):
                    jobs.append((P["w_down"][l].rearrange("(c p) n -> p c n", p=128)[:, cb * 8:(cb + 1) * 8, nb * 512:(nb + 1) * 512],
                                 sc["wdn_bf"][l].rearrange("(c p) n -> p c n", p=128)[:, cb * 8:(cb + 1) * 8, nb * 512:(nb + 1) * 512]))
            for src, dst in jobs:
                st, stres = st_rot.next()
                bf, bfres = bf_rot.next()
                dma(S, "sp", st[:, :, :], src, [], [stres])
                eng = ("act", "dve", "pool")[n % 3]
                n += 1
                cp(S, eng, bf[:, :, :], st[:, :, :], [stres], [bfres])
                dma(S, "sp", dst, bf[:, :, :], [bfres], [("pc_out", n)])
    S.barrier()

def phase_rope(S, nc, posrep, cconst, cos_s, sin_s):
    with ExitStack() as es:
        C = Ctx(nc, S, es, "r_")
        pi_ = C.sb("pi", [96, TT], I32)
        pf = C.sb("pf", [96, TT])
        ang = C.sb("ang", [96, TT])
        ni = C.sb("ni", [96, TT], I32)
        nf = C.sb("nf", [96, TT])
        y = C.sb("y", [96, TT])
        cc = C.sb("cc", [96, 2])
        dma(S, "sp", cc[:, :], cconst, [], ["cc"])
        for t in range(NT):
            tok = slice(t * TT, (t + 1) * TT)
            dma(S, "sp", pi_[:, :], posrep[:, tok], [], ["pi"])
            cp(S, "dve", pf[:, :], pi_[:, :], ["pi"], ["pf"])
            for which in range(2):
                off = 0.0 if which == 0 else float(np.pi / 2)
                ts(S, "dve", ang[:, :], pf[:, :], cc[:, 0:1], ALU.mult, ["pf", "cc"], ["ang"], s2=off, op1=ALU.add)
                ts(S, "dve", ni[:, :], ang[:, :], float(1 / (2 * np.pi)), ALU.mult, ["ang"], ["ni"])
                cp(S, "dve", nf[:, :], ni[:, :], ["ni"], ["nf"])
                stt(S, y[:, :], nf[:, :], float(-2 * np.pi), ang[:, :], ALU.mult, ALU.add, ["nf", "ang"], ["y"])
                ts(S, "dve", nf[:, :], y[:, :], float(np.pi), ALU.is_gt, ["y"], ["nf2"], s2=float(-2 * np.pi), op1=ALU.mult)
                tt(S, "dve", y[:, :], y[:, :], nf[:, :], ALU.add, ["y", "nf2"], ["y2"])
                ts(S, "dve", y[:, :], y[:, :], float(np.pi), ALU.min, ["y2"], ["y3"], s2=float(-np.pi), op1=ALU.max)
                act(S, ang[:, :], y[:, :], AF.Sin, ["y3"], ["sv"])
                if which == 0:
                    ts(S, "dve", y[:, :], ang[:, :], cc[:, 1:2], ALU.mult, ["sv", "cc"], ["yo"])
                    dma(S, "sp", sin_s[:, tok], y[:, :], ["yo"], [("sin_s", t)])
                else:
                    dma(S, "sp", cos_s[:, tok], ang[:, :], ["sv"], [("cos_s", t)])
    S.barrier()


def bcast_row(S, C, dst, src_row, n, ones_row, psr, tag):
    st = C.sb("bst_" + tag, [1, n])
    dma(S, "sp", st[:, :], src_row, [], ["bst_" + tag])
    pt, pres = psr.next()
    mm(S, pt[:, :n], ones_row[0:1, :], st[0:1, :], True, True, ["ones_row", "bst_" + tag], [pres])
    cp(S, "act", dst, pt[:, :n], [pres], ["bc_" + tag])


def phase_A(S, nc, l, xin, P, K, sc, ntiles=None, do=("mla", "sg", "gla", "dn")):
    TA = 256
    NCH = TA // 64
    NSUB = TA // 128
    if ntiles is None:
        ntiles = SEQ // TA
    with ExitStack() as es:
        C = Ctx(nc, S, es, "A_")
        sb = C.sb
        ones_bf = sb("ones", [128, 128], BF16)
        ones_row = sb("ones_row", [1, 128])
        ident = sb("ident", [128, 128])
        memset(S, "pool", ones_bf[:, :], 1.0, ["ones"])
        memset(S, "pool", ones_row[:, :], 1.0, ["ones_row"])
        dma(S, "sp", ident[:, :], K["ident"], [], ["ident"])
        Win = sb("Win", [128, 8, 2744], BF16)
        win_v = P["w_in"][l].rearrange("(c p) n -> p c n", p=128)
        for cb in range(0, 2744, 512):
            ce = min(cb + 512, 2744)
            dma(S, "pool", Win[:, :, cb:ce], win_v[:, :, cb:ce], [], ["Win"])
        gmix = sb("gmix", [128, 8])
        dma(S, "sp", gmix[:, :], P["norm_mix"][l].rearrange("(c p) -> p c", p=128), [], ["gmix"], slow=True)
        xt_rot = C.rot("xt", [128, 8, TA], F32, 1)
        sq_rot = C.rot("sq", [128, TA], BF16, 2)
        hT = sb("hT", [128, 8, TA], BF16)
        tmp = sb("tmp", [128, TA])
        rstd = sb("rstd", [128, TA])
        psr = C.rot("ps", [128, 512], F32, 8, psum=True)
        xin_v = xin.rearrange("(c p) n -> p c n", p=128)
        hv = sc["hT_s"].rearrange("(c p) n -> p c n", p=128)
        hres = [("A_hT", k) for k in range(8)]

        def fproj(pt, pres, c0, n, rows=None):
            for k in range(8):
                mm(S, pt[:n, :TA], Win[:, k, c0:c0 + n], hT[:, k, :], k == 0, k == 7, ["Win", hres[k]], [pres])

        def tproj(pt, pres, s, c0, n, o0=0):
            for k in range(8):
                mm(S, pt[:, o0:o0 + n], hT[:, k, s * 128:(s + 1) * 128], Win[:, k, c0:c0 + n], k == 0, k == 7, ["Win", hres[k]], [pres])

        if "mla" in do:
            gq = sb("gq", [128, 2])
            gkv = sb("gkv", [128, 1])
            dma(S, "sp", gq[:, :], P["mla_norm_q"][l].rearrange("(c p) -> p c", p=128), [], ["gq"], slow=True)
            dma(S, "sp", gkv[:, :], P["mla_norm_kv"][l].rearrange("(c p) -> p c", p=128), [], ["gkv"], slow=True)
            Wkr = sb("Wkr", [128, 8, 96], BF16)
            Wkt = sb("Wkt", [128, 8, 96], BF16)
            memset(S, "pool", Wkr[:, :, :], 0.0, ["Wkr"])
            memset(S, "pool", Wkt[:, :, :], 0.0, ["Wkt"])
            dma(S, "pool", Wkr[:, :, 64:96], win_v[:, :, 384:416], [], ["Wkr"])
            dma(S, "pool", Wkt[:, :, 64:80], win_v[:, :, 400:416], [], ["Wkt"])
            dma(S, "pool", Wkt[:, :, 80:96], win_v[:, :, 384:400], [], ["Wkt"])
            Wuq = sb("Wuq", [128, 2, 384], BF16)
            dma(S, "pool", Wuq[:, :, :], P["mla_w_uq"][l].rearrange("(c p) n -> p c n", p=128), [], ["Wuq"])
            Wuqt = sb("Wuqt", [128, 2, 4, 96], BF16)
            memset(S, "pool", Wuqt[:, :, :, :], 0.0, ["Wuqt"])
            uqv = P["mla_w_uq"][l].rearrange("(c p) (h e) -> p c h e", p=128, e=96)
            for c in range(2):
                dma(S, "pool", Wuqt[:, c, :, 64:80], uqv[:, c, :, 80:96], [], ["Wuqt"])
                dma(S, "pool", Wuqt[:, c, :, 80:96], uqv[:, c, :, 64:80], [], ["Wuqt"])
            Wkn = sb("Wkn", [128, 4, 96], BF16)
            memset(S, "pool", Wkn[:, :, :], 0.0, ["Wkn"])
            ukv = P["mla_w_ukv"][l].rearrange("k (h e) -> k h e", e=128)
            dma(S, "pool", Wkn[:, :, 0:64], ukv[:, :, 0:64], [], ["Wkn"])
            Wv = sb("Wv", [128, 4, 64], BF16)
            dma(S, "pool", Wv[:, :, :], ukv[:, :, 64:128], [], ["Wv"])
            cqs = sb("cqs", [128, 2, TA])
            cqn = sb("cqn", [128, 2, TA], BF16)
            ckvs = sb("ckvs", [128, 1, TA])
            ckvn = sb("ckvn", [128, 1, TA], BF16)
            cosT = sb("cosT", [96, TA])
            sinT = sb("sinT", [96, TA])
            tmpK = sb("tmpK", [96, TA])
            t1_rot = C.rot("t1", [96, TA], F32, 1)
            t2_rot = C.rot("t2", [96, TA], F32, 1)
            qk_rot = C.rot("qk", [96, TA], BF16, 2)
            vt = sb("vt", [128, NSUB, 4, 65], BF16)
            memset(S, "pool", vt[:, :, :, :], 1.0, ["vt"])
        if "sg" in do:
            WmT = sb("WmT", [128, 4, 128])
            sg_t = sb("sg_t", [128, 512])
            sgw = sg_t[:, :].rearrange("p (g j) -> p g j", g=4)
            sgmask = sb("sgmask", [128, 128])
            dma(S, "sp", sgmask[:, :], K["trilT"], [], ["sgmask"])
            dma(S, "sp", sgw, P["sg_w"][l].rearrange("g i j -> i g j"), [], ["sg_t"])
            for g in range(4):
                pt, pres = psr.next()
                tr(S, pt[:, :128], sgw[:, g, :], ident[:, :], ["sg_t", "ident"], [pres])
                tt(S, "dve", WmT[:, g, :], pt[:, :128], sgmask[:, :], ALU.mult, [pres, "sgmask"], ["WmT"])
            sgb = sb("sgb", [128, 4])
            dma(S, "sp", sgb[:, :], P["sg_b"][l].rearrange("g i -> i g"), [], ["sgb"], slow=True)
            lng = sb("lng", [128, 256])
            lnb = sb("lnb", [128, 256])
            bcast_row(S, C, lng[:, :], P["sg_ln_g"][l:l + 1, :], 256, ones_row, psr, "lng")
            bcast_row(S, C, lnb[:, :], P["sg_ln_b"][l:l + 1, :], 256, ones_row, psr, "lnb")
            sg_i = sb("sg_i", [128, 512])
            sg_g = sb("sg_g", [128, 512])
            sg_s = sb("sg_s", [128, 8])
            sg_vc = sb("sg_vc", [128, 256])
            sg_junk = sb("sg_junk", [128, 256])
            sg_vn = sb("sg_vn", [128, 256])
            sg_o = sb("sg_o", [128, 256])
            obT = sb("obT", [128, 2, TA], BF16)
        if "gla" in do:
            wgate = sb("wgate", [16, 128])
            dma(S, "sp", wgate[:, :], P["gla_w_gate"][l], [], ["wgate"])
            nbg = sb("nbg", [128, 1])
            dma(S, "sp", nbg[:, :], P["gla_b_gate"][l].rearrange("(p o) -> p o", o=1), [], ["nbg0"], slow=True)
            ts(S, "dve", nbg[:, :], nbg[:, :], -1.0, ALU.mult, ["nbg0"], ["nbg"])
            rm = sb("rm", [128, TA])
            dma(S, "sp", rm[:, :], K["rm"][:, 0:TA], [], ["rm"])
            mg = sb("mg", [128, 128])
            dma(S, "sp", mg[:, :], K["mg"], [], ["mg"])
            hm = sb("hm", [128, 256])
            dma(S, "sp", hm[:, :], K["hm"], [], ["hm"])
            hcol = sb("hcol", [128, 4])
            dma(S, "sp", hcol[:, :], K["hcol"], [], ["hcol"])
            topbot = sb("topbot", [128, 2])
            dma(S, "sp", topbot[:, :], K["topbot"], [], ["topbot"])
            cmA = sb("cmA", [128, TA])
            dma(S, "sp", cmA[:, :], K["cmA"][:, 0:TA], [], ["cmA"])
            gnb = sb("gnb", [128, 256])
            for h in range(4):
                bcast_row(S, C, gnb[:, h * 64:(h + 1) * 64], P["gla_norm"][l:l + 1, :], 64, ones_row, psr, f"gn{h}")
            glr = sb("glr", [16, TA])
            g_l = sb("g_l", [128, TA])
            g_cum = sb("g_cum", [128, TA])
            g_ec = sb("g_ec", [128, TA])
            g_en = sb("g_en", [128, TA])
            g_qe = sb("g_qe", [128, TA])
            g_qA = sb("g_qA", [128, TA])
            g_qB = sb("g_qB", [128, TA])
            g_ke = sb("g_ke", [128, TA])
            g_kl = sb("g_kl", [128, TA])
            g_qp = sb("g_qp", [128, 4, TA])
            g_klA = sb("g_klA", [128, 128])
            g_klB = sb("g_klB", [128, 128])
            g_v = sb("g_v", [128, 256])
            g_og = sb("g_og", [128, 256])
            g_sc = C.rot("g_sc", [128, 128], F32, 4)
            g_S = sb("g_S", [128, 256])
            g_tmp = sb("g_tmp", [128, 256])
            g_ss = sb("g_ss", [128, 8])
            g_junk = sb("g_junk", [128, 64])
            g_o = sb("g_o", [128, 256])
            ocT = sb("ocT", [128, 2, TA], BF16)
            memset(S, "pool", g_S[:, :], 0.0, ["g_S"])
        if "dn" in do:
            if "gla" not in do:
                rm = sb("rm", [128, TA])
                dma(S, "sp", rm[:, :], K["rm"][:, 0:TA], [], ["rm"])
                topbot = sb("topbot", [128, 2])
                dma(S, "sp", topbot[:, :], K["topbot"], [], ["topbot"])
                cmA = sb("cmA", [128, TA])
                dma(S, "sp", cmA[:, :], K["cmA"][:, 0:TA], [], ["cmA"])
            ones64 = ones_bf
            cU = sb("cU", [128, 128]); dma(S, "sp", cU[:, :], K["U"], [], ["cU"])
            cL = sb("cL", [128, 128]); dma(S, "sp", cL[:, :], K["L"], [], ["cL"])
            cst = sb("cst", [128, 128]); dma(S, "sp", cst[:, :], K["strict"], [], ["cst"])
            sel4 = sb("sel4", [4, 4, 128]); dma(S, "sp", sel4[:, :, :], K["sel4"], [], ["sel4"])
            zrow = sb("zrow", [1, 256]); memset(S, "pool", zrow[:, :], 0.0, ["zrow"])
            cw = sb("cw", [64, 12, 4])
            for kk in range(4):
                dma(S, "sp", cw[:, :, kk], P["dn_conv"][l, kk].rearrange("(i p) -> p i", p=64), [], ["cw"], slow=True)
            alog = sb("alog", [4, 1]); dma(S, "sp", alog[:, :], P["dn_a_log"][l].rearrange("(p o) -> p o", o=1), [], ["alog0"], slow=True)
            dtb = sb("dtb", [4, 1]); dma(S, "sp", dtb[:, :], P["dn_dt_bias"][l].rearrange("(p o) -> p o", o=1), [], ["dtb"], slow=True)
            negA = sb("negA", [4, 1])
            act(S, negA[:, :], alog[:, :], AF.Exp, ["alog0"], ["negA0"])
            ts(S, "dve", negA[:, :], negA[:, :], -1.0, ALU.mult, ["negA0"], ["negA"])
            dnb = sb("dnb", [128, 256])
            for h in range(4):
                bcast_row(S, C, dnb[:, h * 64:(h + 1) * 64], P["dn_norm"][l:l + 1, :], 64, ones_row, psr, f"dnn{h}")
            xc = sb("xc", [64, 12, 3 + TA])
            memset(S, "pool", xc[:, :, 0:3], 0.0, [("xc", i) for i in range(12)])
            d_y = sb("d_y", [64, 12, TA])
            d_sq = C.rot("d_sq", [64, TA], BF16, 2)
            d_t = sb("d_t", [64, TA])
            d_r = sb("d_r", [64, TA])
            d_a = sb("d_a", [4, TA]); d_e = sb("d_e", [4, TA]); d_g = sb("d_g", [4, TA]); d_gam = sb("d_gam", [4, TA])
            d_b = sb("d_b", [4, TA]); d_eg = sb("d_eg", [4, TA]); d_bk = sb("d_bk", [4, TA]); d_lg = sb("d_lg", [4, TA])
            d_elb = sb("d_elb", [64, 4, NCH])
            d_gbc = sb("d_gbc", [128, 4, TA])
            d_kp = sb("d_kp", [64, 4, TA])
            d_qA = sb("d_qA", [64, 4, TA]); d_qB = sb("d_qB", [64, 4, TA])
            d_cols = sb("d_cols", [128, 12])
            d_Vp = sb("d_Vp", [128, 256])
            d_kdA = sb("d_kdA", [128, 4, 64]); d_kdB = sb("d_kdB", [128, 4, 64])
            d_Dm = C.rot("d_Dm", [128, 128], F32, 2)
            d_d1 = C.rot("d_d1", [128, 128], F32, 2)
            d_at = sb("d_at", [128, 4, 128])
            d_Bp = [sb(f"d_Bp{h}", [128, 2, 128]) for h in range(4)]
            d_Bt = [sb(f"d_Bt{h}", [128, 2, 128]) for h in range(4)]
            d_R = [sb(f"d_R{h}", [128, 128]) for h in range(4)]
            d_rhs2 = sb("d_rhs2", [128, 256])
            d_u = sb("d_u", [128, 256])
            d_S = sb("d_S", [64, 4, 64])
            memset(S, "pool", d_S[:, :, :], 0.0, ["d_S"])
            memset(S, "pool", d_u[:, :], 0.0, ["d_u"])
            d_z = sb("d_z", [128, 256])
            d_ss = sb("d_ss", [128, 8])
            d_junk = sb("d_junk", [128, 64])
            d_junk2 = sb("d_junk2", [128, 64])
            d_o = sb("d_o", [128, 256])
            odT = sb("odT", [128, 2, TA], BF16)

        def post_norm_gate(o_ps, o_res, gate_sb, gate_res, ss, junk, o_out, tagp, oT_tile, s):
            for h in range(4):
                act(S, junk[:, :], o_ps[:, h * 64:(h + 1) * 64], AF.Square, [o_res], [tagp + "junk"], accum_out=ss[:, h:h + 1])
            S.stream
            act(S, ss[:, 4:8], ss[:, 0:4], AF.Sqrt, [tagp + "junk"], [tagp + "ss1"], scale=1.0 / 64, bias=EPS)
            S.add("dve", lambda e: e.reciprocal(out=ss[:, 0:4], in_=ss[:, 4:8]), reads=[tagp + "ss1"], writes=[tagp + "ss2"])
            for h in range(4):
                stt(S, o_out[:, h * 64:(h + 1) * 64], o_ps[:, h * 64:(h + 1) * 64], ss[:, h:h + 1], gate_sb[:, h * 64:(h + 1) * 64],
                    ALU.mult, ALU.mult, [o_res, tagp + "ss2", gate_res], [tagp + "oo"])
            for c in range(2):
                pt, pres = psr.next()
                tr(S, pt[:, :128], o_out[:, c * 128:(c + 1) * 128], ident[:, :], [tagp + "oo", "ident"], [pres])
                cp(S, "act", oT_tile[:, c, s * 128:(s + 1) * 128], pt[:, :128], [pres], [tagp + "oT"])

        for t in range(ntiles):
            tok = slice(t * TA, (t + 1) * TA)
            xt, x0 = xt_rot.next()
            xres = [(x0, d) for d in range(8)]
            dma(S, "sp", xt[:, :, :], xin_v[:, :, tok], [], xres)
            rms_fm(S, xt, xres, 8, D, gmix, ones_bf, sq_rot, psr, tmp, "A_tmp", rstd, "A_rstd", hT, hres, TA)
            dma(S, "sp", hv[:, :, tok], hT[:, :, :], hres, [("hT_s", t)])
            if "mla" in do:
                dma(S, "sp", cosT[:, :], sc["cos_s"][:, tok], [], ["cosT"])
                dma(S, "sp", sinT[:, :], sc["sin_s"][:, tok], [], ["sinT"])
                for c in range(2):
                    pt, pres = psr.next()
                    fproj(pt, pres, c * 128, 128)
                    cp(S, "act", cqs[:, c, :], pt[:, :TA], [pres], [("cqs", c)])
                rms_fm(S, cqs, [("cqs", 0), ("cqs", 1)], 2, 256, gq, ones_bf, sq_rot, psr, tmp, "A_tmp", rstd, "A_rstd",
                       cqn, [("cqn", 0), ("cqn", 1)], TA)
                pt, pres = psr.next()
                fproj(pt, pres, 256, 128)
                cp(S, "act", ckvs[:, 0, :], pt[:, :TA], [pres], [("ckvs", 0)])
                rms_fm(S, ckvs, [("ckvs", 0)], 1, 128, gkv, ones_bf, sq_rot, psr, tmp, "A_tmp", rstd, "A_rstd",
                       ckvn, [("ckvn", 0)], TA)
                pt, pres = psr.next()
                for k in range(8):
                    mm(S, pt[:96, :TA], Wkt[:, k, :], hT[:, k, :], k == 0, k == 7, ["Wkt", hres[k]], [pres])
                tt(S, "dve", tmpK[:, :], pt[:96, :TA], sinT[:, :], ALU.mult, [pres, "sinT"], ["tmpK"])
                for h in range(4):
                    pt, pres = psr.next()
                    for c in range(2):
                        mm(S, pt[:96, :TA], Wuq[:, c, h * 96:(h + 1) * 96], cqn[:, c, :], c == 0, c == 1, ["Wuq", ("cqn", c)], [pres])
                    pt2, pres2 = psr.next()
                    for c in range(2):
                        mm(S, pt2[:96, :TA], Wuqt[:, c, h, :], cqn[:, c, :], c == 0, c == 1, ["Wuqt", ("cqn", c)], [pres2])
                    t1, t1r = t1_rot.next()
                    t2, t2r = t2_rot.next()
                    tt(S, "dve", t1[:, :], pt[:96, :TA], cosT[:, :], ALU.mult, [pres, "cosT"], [t1r])
                    tt(S, "dve", t2[:, :], pt2[:96, :TA], sinT[:, :], ALU.mult, [pres2, "sinT"], [t2r])
                    qk, qkr = qk_rot.next()
                    tt(S, "pool", qk[:, :], t1[:, :], t2[:, :], ALU.add, [t1r, t2r], [qkr])
                    dma(S, "sp", sc["qT_s"][h][:, tok], qk[:, :], [qkr], [("qT_s", h, t)])
                    pt, pres = psr.next()
                    for k in range(8):
                        mm(S, pt[:96, :TA], Wkr[:, k, :], hT[:, k, :], k == 0, False, ["Wkr", hres[k]], [pres])
                    mm(S, pt[:96, :TA], Wkn[:, h, :], ckvn[:, 0, :], False, True, ["Wkn", ("ckvn", 0)], [pres])
                    t1, t1r = t1_rot.next()
                    tt(S, "dve", t1[:, :], pt[:96, :TA], cosT[:, :], ALU.mult, [pres, "cosT"], [t1r])
                    qk, qkr = qk_rot.next()
                    tt(S, "pool", qk[:, :], t1[:, :], tmpK[:, :], ALU.add, [t1r, "tmpK"], [qkr])
                    dma(S, "sp", sc["kT_s"][h][:, tok], qk[:, :], [qkr], [("kT_s", h, t)])
                for s in range(NSUB):
                    pt, pres = psr.next()
                    mm(S, pt[:, :256], ckvn[:, 0, s * 128:(s + 1) * 128], Wv[:, :, :].rearrange("p h e -> p (h e)"), True, True,
                       ["Wv", ("ckvn", 0)], [pres])
                    cp(S, "act", vt[:, s, :, 0:64], pt[:, :256].rearrange("p (h e) -> p h e", e=64), [pres], ["vt"])
                dma(S, "sp", sc["v_s"][tok, :].rearrange("(s p) n -> p s n", p=128), vt[:, :, :, :].rearrange("p s h e -> p s (h e)"),
                    ["vt"], [("v_s", t)])
            if "gla" in do:
                pt, pres = psr.next()
                fproj(pt, pres, 1440, 16)
                cp(S, "act", glr[:, :], pt[:16, :TA], [pres], ["glr"])
                pt, pres = psr.next()
                mm(S, pt[:, :TA], wgate[:, :], glr[:, :], True, True, ["wgate", "glr"], [pres])
                act(S, g_l[:, :], pt[:, :TA], AF.Exp, [pres], ["g_l0"], scale=-1.0, bias=nbg[:, 0:1])
                act(S, g_l[:, :], g_l[:, :], AF.Ln, ["g_l0", "nbg"], ["g_l"], bias=1.0)
                S.add("dve", lambda e: e.tensor_tensor_scan(out=g_cum[:, :], data0=rm[:, :], data1=g_l[:, :], initial=0.0,
                                                            op0=ALU.mult, op1=ALU.add), reads=["rm", "g_l"], writes=["g_cum"])
                act(S, g_ec[:, :], g_cum[:, :], AF.Exp, ["g_cum"], ["g_ec"], scale=-1.0 / 16)
                act(S, g_en[:, :], g_cum[:, :], AF.Exp, ["g_cum"], ["g_en"], scale=1.0 / 16)
                pt, pres = psr.next()
                fproj(pt, pres, 928, 128)
                stt(S, g_qe[:, :], pt[:, :TA], float(32 ** -0.5), g_ec[:, :], ALU.mult, ALU.mult, [pres, "g_ec"], ["g_qe"])
                pt, pres = psr.next()
                fproj(pt, pres, 1056, 128)
                tt(S, "dve", g_ke[:, :], pt[:, :TA], g_en[:, :], ALU.mult, [pres, "g_en"], ["g_ke"])
                for c in range(NCH):
                    ts(S, "dve", g_kl[:, c * 64:(c + 1) * 64], g_ke[:, c * 64:(c + 1) * 64], g_ec[:, c * 64 + 63:c * 64 + 64], ALU.mult,
                       ["g_ke", "g_ec"], [("g_kl", c // 2)])
                for h in range(4):
                    ts(S, "pool", g_qp[:, h, :], g_qe[:, :], hcol[:, h:h + 1], ALU.mult, ["g_qe", "hcol"], [("g_qp", h)])
                tt(S, "pool", g_qA[:, :], g_qe[:, :], cmA[:, :], ALU.mult, ["g_qe", "cmA"], ["g_qA"])
                tt(S, "pool", g_qB[:, :], g_qe[:, :], g_qA[:, :], ALU.subtract, ["g_qe", "g_qA"], ["g_qB"])
            if "dn" in do:
                for i in range(12):
                    pt, pres = psr.next()
                    fproj(pt, pres, 1712 + i * 64, 64)
                    cp(S, "act", xc[:, i, 3:3 + TA], pt[:64, :TA], [pres], [("xc", i)])
                    yi = d_y[:, i, :]
                    ts(S, "dve", yi, xc[:, i, 3:3 + TA], cw[:, i, 3:4], ALU.mult, [("xc", i), "cw"], [("d_y", i)])
                    for kk in range(3):
                        stt(S, yi, xc[:, i, kk:kk + TA], cw[:, i, kk:kk + 1], yi, ALU.mult, ALU.add, [("xc", i), ("d_y", i), "cw"], [("d_y", i)])
                    cp(S, "pool", xc[:, i, 0:3], xc[:, i, TA:TA + 3], [("xc", i)], [("xc", i)])
                    act(S, yi, yi, AF.Silu, [("d_y", i)], [("d_y", i)])
                    if i < 8:
                        sq, sqr = d_sq.next()
                        act(S, sq[:, :], yi, AF.Square, [("d_y", i)], [sqr])
                        pt, pres = psr.next()
                        mm(S, pt[:64, :TA], ones64[:64, :64], sq[:, :], True, True, ["ones", sqr], [pres])
                        act(S, d_t[:, :], pt[:64, :TA], AF.Sqrt, [pres], ["d_t"], bias=EPS)
                        S.add("dve", lambda e: e.reciprocal(out=d_r[:, :], in_=d_t[:, :]), reads=["d_t"], writes=["d_r"])
                        stt(S, yi, yi, 0.125 if i < 4 else 1.0, d_r[:, :], ALU.mult, ALU.mult, [("d_y", i), "d_r"], [("d_y", i)])
                pt, pres = psr.next()
                fproj(pt, pres, 2480, 4)
                act(S, d_e[:, :], pt[:4, :TA], AF.Exp, [pres, "dtb"], ["d_e0"], bias=dtb[:, 0:1])
                act(S, d_e[:, :], d_e[:, :], AF.Ln, ["d_e0"], ["d_e"], bias=1.0)
                ts(S, "dve", d_g[:, :], d_e[:, :], negA[:, 0:1], ALU.mult, ["d_e", "negA"], ["d_g"])
                S.add("dve", lambda e: e.tensor_tensor_scan(out=d_gam[:, :], data0=rm[0:4, :], data1=d_g[:, :], initial=0.0,
                                                            op0=ALU.mult, op1=ALU.add), reads=["rm", "d_g"], writes=["d_gam"])
                pt, pres = psr.next()
                fproj(pt, pres, 2484, 4)
                act(S, d_b[:, :], pt[:4, :TA], AF.Sigmoid, [pres], ["d_b"])
                act(S, d_eg[:, :], d_gam[:, :], AF.Exp, ["d_gam"], ["d_eg"])
                tt(S, "dve", d_bk[:, :], d_b[:, :], d_eg[:, :], ALU.mult, ["d_b", "d_eg"], ["d_bk"])
                for c in range(NCH):
                    ts(S, "dve", d_lg[:, c * 64:(c + 1) * 64], d_gam[:, c * 64:(c + 1) * 64], d_gam[:, c * 64 + 63:c * 64 + 64], ALU.subtract,
                       ["d_gam"], ["d_lg0"])
                act(S, d_lg[:, :], d_lg[:, :], AF.Exp, ["d_lg0"], ["d_lg"], scale=-1.0)
                pt, pres = psr.next()
                for h in range(4):
                    mm(S, pt[:64, h * NCH:(h + 1) * NCH], sel4[:, h, 0:64], d_eg[:, 63:TA:64], True, True, ["sel4", "d_eg"], [pres])
                cp(S, "act", d_elb[:, :, :], pt[:64, 0:4 * NCH].rearrange("p (h c) -> p h c", c=NCH), [pres], ["d_elb"])
                for h in range(4):
                    pt, pres = psr.next()
                    mm(S, pt[:, :TA], sel4[:, h, :], d_gam[:, :], True, True, ["sel4", "d_gam"], [pres])
                    cp(S, "act", d_gbc[:, h, :], pt[:, :TA], [pres], [("d_gbc", h)])
                    pt, pres = psr.next()
                    mm(S, pt[:64, :TA], sel4[:, h, 0:64], d_bk[:, :], True, True, ["sel4", "d_bk"], [pres])
                    tt(S, "dve", d_kp[:, h, :], pt[:64, :TA], d_y[:, 4 + h, :], ALU.mult, [pres, ("d_y", 4 + h)], [("d_kp", h)])
                    pt, pres = psr.next()
                    mm(S, pt[:64, :TA], sel4[:, h, 0:64], d_eg[:, :], True, True, ["sel4", "d_eg"], [pres])
                    tt(S, "dve", d_qB[:, h, :], pt[:64, :TA], d_y[:, h, :], ALU.mult, [pres, ("d_y", h)], [("d_qB0", h)])
                    tt(S, "pool", d_qA[:, h, :], d_qB[:, h, :], cmA[:64, :], ALU.mult, [("d_qB0", h), "cmA"], [("d_qA", h)])
                    tt(S, "pool", d_qB[:, h, :], d_qB[:, h, :], d_qA[:, h, :], ALU.subtract, [("d_qB0", h), ("d_qA", h)], [("d_qB", h)])
            for s in range(NSUB):
                ssl = slice(s * 128, (s + 1) * 128)
                if "sg" in do:
                    pu, pures = psr.next()
                    tproj(pu, pures, s, 416, 512)
                    act(S, sg_t[:, :], pu[:, :], AF.Square, [pures], ["sg_t"])
                    ts(S, "dve", sg_i[:, :], sg_t[:, :], 0.044715, ALU.mult, ["sg_t"], ["sg_i0"], s2=1.0, op1=ALU.add)
                    tt(S, "dve", sg_i[:, :], sg_i[:, :], pu[:, :], ALU.mult, ["sg_i0", pures], ["sg_i"])
                    act(S, sg_t[:, :], sg_i[:, :], AF.Sigmoid, ["sg_i"], ["sg_t2"], scale=1.5957691216057308)
                    tt(S, "dve", sg_g[:, :], sg_t[:, :], pu[:, :], ALU.mult, ["sg_t2", pures], ["sg_g"])
                    S.add("dve", lambda e: e.reduce_sum(out=sg_s[:, 0:1], in_=sg_g[:, 256:512], axis=mybir.AxisListType.X),
                          reads=["sg_g"], writes=["sg_s0"])
                    ts(S, "dve", sg_s[:, 1:2], sg_s[:, 0:1], -1.0 / 256, ALU.mult, ["sg_s0"], ["sg_s1"])
                    ts(S, "dve", sg_vc[:, :], sg_g[:, 256:512], sg_s[:, 1:2], ALU.add, ["sg_g", "sg_s1"], ["sg_vc"])
                    act(S, sg_junk[:, :], sg_vc[:, :], AF.Square, ["sg_vc"], ["sg_junk"], accum_out=sg_s[:, 2:3])
                    act(S, sg_s[:, 3:4], sg_s[:, 2:3], AF.Sqrt, ["sg_junk"], ["sg_s3"], scale=1.0 / 256, bias=EPS)
                    S.add("dve", lambda e: e.reciprocal(out=sg_s[:, 4:5], in_=sg_s[:, 3:4]), reads=["sg_s3"], writes=["sg_s4"])
                    stt(S, sg_vn[:, :], sg_vc[:, :], sg_s[:, 4:5], lng[:, :], ALU.mult, ALU.mult, ["sg_vc", "sg_s4", "bc_lng"], ["sg_vn0"])
                    tt(S, "dve", sg_vn[:, :], sg_vn[:, :], lnb[:, :], ALU.add, ["sg_vn0", "bc_lnb"], ["sg_vn"])
                    pt, pres = psr.next()
                    for g in range(4):
                        mm(S, pt[:, g * 64:(g + 1) * 64], WmT[:, g, :], sg_vn[:, g * 64:(g + 1) * 64], True, True, ["WmT", "sg_vn"], [pres])
                    for g in range(4):
                        stt(S, sg_o[:, g * 64:(g + 1) * 64], pt[:, g * 64:(g + 1) * 64], sgb[:, g:g + 1], sg_g[:, g * 64:(g + 1) * 64],
                            ALU.add, ALU.mult, [pres, "sgb", "sg_g"], ["sg_o"])
                    for c in range(2):
                        pt, pres = psr.next()
                        tr(S, pt[:, :128], sg_o[:, c * 128:(c + 1) * 128], ident[:, :], ["sg_o", "ident"], [pres])
                        cp(S, "act", obT[:, c, ssl], pt[:, :128], [pres], ["obT"])
                if "gla" in do:
                    pv, pvres = psr.next()
                    tproj(pv, pvres, s, 1184, 256, 0)
                    tproj(pv, pvres, s, 1456, 256, 256)
                    cp(S, "act", g_v[:, :], pv[:, 0:256], [pvres], ["g_v"])
                    act(S, g_og[:, :], pv[:, 256:512], AF.Silu, [pvres], ["g_og0"])
                    tt(S, "pool", g_og[:, :], g_og[:, :], gnb[:, :], ALU.mult, ["g_og0"] + [f"bc_gn{h}" for h in range(4)], ["g_og"])
                    pt, pres = psr.next()
                    tr(S, pt[:, :128], g_kl[:, ssl], ident[:, :], [("g_kl", s), "ident"], [pres])
                    ts(S, "dve", g_klA[:, :], pt[:, :128], topbot[:, 0:1], ALU.mult, [pres, "topbot"], ["g_klA"])
                    ts(S, "dve", g_klB[:, :], pt[:, :128], topbot[:, 1:2], ALU.mult, [pres, "topbot"], ["g_klB"])
                    scs = []
                    for h in range(4):
                        pt, pres = psr.next()
                        mm(S, pt[:, :128], g_ke[:, ssl], g_qp[:, h, ssl], True, True, ["g_ke", ("g_qp", h)], [pres])
                        sct, scr = g_sc.next()
                        tt(S, "dve", sct[:, :], pt[:, :128], mg[:, :], ALU.mult, [pres, "mg"], [scr])
                        scs.append((sct, scr))
                    po, pores = psr.next()
                    mm(S, po[:, :256], g_qA[:, ssl], g_S[:, :], True, False, ["g_qA", "g_S"], [pores])
                    for (klt, klr, isB) in ((g_klA, "g_klA", False), (g_klB, "g_klB", True)):
                        if isB:
                            mm(S, po[:, :256], g_qB[:, ssl], g_S[:, :], False, False, ["g_qB", "g_S"], [pores])
                        pt, pres = psr.next()
                        mm(S, pt[:, :256], klt[:, :], g_v[:, :], True, True, [klr, "g_v"], [pres])
                        tt(S, "dve", g_tmp[:, :], pt[:, :256], hm[:, :], ALU.mult, [pres, "hm"], ["g_tmp"])
                        cidx = s * 2 + (1 if isB else 0)
                        stt(S, g_S[:, :], g_S[:, :], g_ec[:, cidx * 64 + 63:cidx * 64 + 64], g_tmp[:, :], ALU.mult, ALU.add,
                            ["g_S", "g_ec", "g_tmp"], ["g_S"])
                    for h in range(4):
                        mm(S, po[:, h * 64:(h + 1) * 64], scs[h][0][:, :], g_v[:, h * 64:(h + 1) * 64], False, h == 3, [scs[h][1], "g_v"], [pores])
                    post_norm_gate(po, pores, g_og, "g_og", g_ss, g_junk, g_o, "g_", ocT, s)
                if "dn" in do and DNSTAGE >= 1:
                    pz, pzres = psr.next()
                    tproj(pz, pzres, s, 2488, 256, 0)
                    act(S, d_z[:, :], pz[:, 0:256], AF.Silu, [pzres], ["d_z0"])
                    tt(S, "pool", d_z[:, :], d_z[:, :], dnb[:, :], ALU.mult, ["d_z0"] + [f"bc_dnn{h}" for h in range(4)], ["d_z"])
                    pt, pres = psr.next()
                    mm(S, pt[:, 0:4], d_gam[:, ssl], ident[0:4, 0:4], True, True, ["d_gam", "ident"], [pres])
                    mm(S, pt[:, 4:8], d_b[:, ssl], ident[0:4, 0:4], True, True, ["d_b", "ident"], [pres])
                    mm(S, pt[:, 8:12], d_lg[:, ssl], ident[0:4, 0:4], True, True, ["d_lg", "ident"], [pres])
                    cp(S, "act", d_cols[:, :], pt[:, 0:12], [pres], ["d_cols"])
                    for h in range(4 if DNSTAGE >= 1.2 else 0):
                        kTh = d_y[:, 4 + h, ssl]
                        qTh = d_y[:, h, ssl]
                        vTh = d_y[:, 8 + h, ssl]
                        pt, pres = psr.next()
                        mm(S, pt[:, 0:64], kTh, ident[0:64, 0:64], True, True, [("d_y", 4 + h), "ident"], [pres])
                        mm(S, pt[:, 64:128], vTh, ident[0:64, 0:64], True, True, [("d_y", 8 + h), "ident"], [pres])
                        ts(S, "dve", d_Vp[:, h * 64:(h + 1) * 64], pt[:, 64:128], d_cols[:, 4 + h:5 + h], ALU.mult, [pres, "d_cols"], [("d_Vp", h)])
                        ts(S, "dve", d_junk[:, :], pt[:, 0:64], d_cols[:, 8 + h:9 + h], ALU.mult, [pres, "d_cols"], ["d_kdf"])
                        ts(S, "pool", d_kdA[:, h, :], d_junk[:, :], topbot[:, 0:1], ALU.mult, ["d_kdf", "topbot"], [("d_kdA", h)])
                        ts(S, "pool", d_kdB[:, h, :], d_junk[:, :], topbot[:, 1:2], ALU.mult, ["d_kdf", "topbot"], [("d_kdB", h)])
                        if DNSTAGE < 1.3:
                            continue
                        pkk, pkkres = psr.next()
                        mm(S, pkk[:, 0:128], kTh, kTh, True, True, [("d_y", 4 + h)], [pkkres])
                        mm(S, pkk[:, 128:256], kTh, qTh, True, True, [("d_y", 4 + h), ("d_y", h)], [pkkres])
                        d1, d1r = d_d1.next()
                        stt(S, d1[:, :], d_gbc[:, h, ssl], d_cols[:, h:h + 1], cU[:, :], ALU.subtract, ALU.max, [("d_gbc", h), "d_cols", "cU"], [d1r])
                        Dm, Dmr = d_Dm.next()
                        act(S, Dm[:, :], d1[:, :], AF.Exp, [d1r], [Dmr], scale=-1.0)
                        Bp, Bt, Rr = d_Bp[h], d_Bt[h], d_R[h]
                        stt(S, Bp[:, 0, :], pkk[:, 0:128], d_cols[:, 4 + h:5 + h], Dm[:, :], ALU.mult, ALU.mult, [pkkres, "d_cols", Dmr], [("Bp", h, 0)])
                        tt(S, "pool", Bp[:, 0, :], Bp[:, 0, :], cst[:, :], ALU.mult, [("Bp", h, 0), "cst"], [("Bp", h, 0)])
                        d2, d2r = d_d1.next()
                        stt(S, d2[:, :], d_gbc[:, h, ssl], d_cols[:, h:h + 1], cL[:, :], ALU.subtract, ALU.min, [("d_gbc", h), "d_cols", "cL"], [d2r])
                        DmT, DmTr = d_Dm.next()
                        act(S, DmT[:, :], d2[:, :], AF.Exp, [d2r], [DmTr])
                        tt(S, "dve", d_at[:, h, :], pkk[:, 128:256], DmT[:, :], ALU.mult, [pkkres, DmTr], [("d_at", h)])
                        if DNSTAGE < 1.4:
                            continue
                        pt, pres = psr.next()
                        mm(S, pt[:, :128], Bp[:, 0, :], ident[:, :], True, True, [("Bp", h, 0), "ident"], [pres])
                        if DNV != "a":
                            cp(S, "act", Bt[:, 0, :], pt[:, :128], [pres], [("Bt", h, 0)])
                        tt(S, "pool", Rr[:, :], ident[:, :], Bt[:, 0, :], ALU.subtract, [("Bt", h, 0), "ident"], [("R", h)])
                    for lev in range(1, 6 if DNSTAGE >= 2 else 1):
                        a, b = (lev - 1) % 2, lev % 2
                        for h in range(4):
                            Bp, Bt, Rr = d_Bp[h], d_Bt[h], d_R[h]
                            pt, pres = psr.next()
                            mm(S, pt[:, 0:128], Bt[:, a, :], Bp[:, a, :], True, True, [("Bt", h, a), ("Bp", h, a)], [pres])
                            if lev < 5:
                                mm(S, pt[:, 128:256], Bp[:, a, :], Bt[:, a, :], True, True, [("Bt", h, a), ("Bp", h, a)], [pres])
                            cp(S, "act", Bp[:, b, :], pt[:, 0:128], [pres], [("Bp", h, b)])
                            if lev < 5:
                                cp(S, "act", Bt[:, b, :], pt[:, 128:256], [pres], [("Bt", h, b)])
                            pt2, pres2 = psr.next()
                            mm(S, pt2[:, 0:128], Bp[:, b, :], Rr[:, :], True, True, [("Bp", h, b), ("R", h)], [pres2])
                            tt(S, "dve", Rr[:, :], Rr[:, :], pt2[:, 0:128], ALU.add, [pres2, ("R", h)], [("R", h)])
                    if DNSTAGE < 1.5:
                        continue
                    po, pores = psr.next()
                    mm(S, po[:, :256], zrow[0:1, 0:128], zrow[0:1, :], True, False, ["zrow"], [pores])
                    for ci, (kd, kdn, qd, qdn) in enumerate(((d_kdA, "d_kdA", d_qA, "d_qA"), (d_kdB, "d_kdB", d_qB, "d_qB")) if DNSTAGE >= 3 else ()):
                        rows = slice(ci * 64, (ci + 1) * 64)
                        pks, pksres = psr.next()
                        for h in range(4):
                            mm(S, pks[:, h * 64:(h + 1) * 64], d_kp[:, h, ssl], d_S[:, h, :], True, True, [("d_kp", h), "d_S"], [pksres])
                            mm(S, po[:, h * 64:(h + 1) * 64], qd[:, h, ssl], d_S[:, h, :], False, False, [(qdn, h), "d_S"], [pores])
                        tt(S, "dve", d_rhs2[:, :], d_Vp[:, :], pks[:, :256], ALU.subtract, [pksres] + [("d_Vp", h) for h in range(4)], ["d_rhs2"])
                        pu, pures = psr.next()
                        for h in range(4):
                            mm(S, pu[:, h * 64:(h + 1) * 64], d_R[h][:, :], d_rhs2[:, h * 64:(h + 1) * 64], True, True, [("R", h), "d_rhs2"], [pures])
                        cp(S, "act", d_u[rows, :], pu[rows, :256], [pures], ["d_u"])
                        psu, psures = psr.next()
                        for h in range(4):
                            mm(S, psu[:64, h * 64:(h + 1) * 64], kd[:, h, :], d_u[:, h * 64:(h + 1) * 64], True, True, [(kdn, h), "d_u"], [psures])
                        cidx = s * 2 + ci
                        for h in range(4):
                            stt(S, d_S[:, h, :], d_S[:, h, :], d_elb[:, h, cidx:cidx + 1], psu[:64, h * 64:(h + 1) * 64], ALU.mult, ALU.add,
                                ["d_S", "d_elb", psures], ["d_S"])
                    for h in range(4):
                        mm(S, po[:, h * 64:(h + 1) * 64], d_at[:, h, :], d_u[:, h * 64:(h + 1) * 64], False, h == 3, [("d_at", h), "d_u"], [pores])
                    post_norm_gate(po, pores, d_z, "d_z", d_ss, d_junk2, d_o, "d_", odT, s)
            if "sg" in do:
                dma(S, "sp", sc["oT_s"][0].rearrange("(c p) n -> p c n", p=128)[:, :, tok], obT[:, :, :], ["obT"], [("oT_s", 0, t)])
            if "gla" in do:
                dma(S, "sp", sc["oT_s"][1].rearrange("(c p) n -> p c n", p=128)[:, :, tok], ocT[:, :, :], ["g_oT"], [("oT_s", 1, t)])
            if "dn" in do:
                dma(S, "sp", sc["oT_s"][2].rearrange("(c p) n -> p c n", p=128)[:, :, tok], odT[:, :, :], ["d_oT"], [("oT_s", 2, t)])
    S.barrier()

PARAM_SHAPES = {
    "norm_mix": [NL, D], "w_in": [NL, D, INW], "mla_norm_q": [NL, 256], "mla_norm_kv": [NL, 128],
    "mla_w_uq": [NL, 256, 384], "mla_w_ukv": [NL, 128, 512], "sg_ln_g": [NL, 256], "sg_ln_b": [NL, 256],
    "sg_w": [NL, 4, 128, 128], "sg_b": [NL, 4, 128], "gla_w_gate": [NL, 16, 128], "gla_b_gate": [NL, 128],
    "gla_norm": [NL, 64], "dn_conv": [NL, 4, 768], "dn_a_log": [NL, 4], "dn_dt_bias": [NL, 4], "dn_norm": [NL, 64],
    "w_branch": [NL, 4, 256, D], "w_out": [NL, D, D], "norm_xattn": [NL, D], "norm_mem": [NL, D],
    "xattn_wq": [NL, D, D], "xattn_wk": [NL, D, D], "xattn_wv": [NL, D, D], "xattn_wo": [NL, D, D],
    "norm_mlp": [NL, D], "w_up": [NL, D, HID], "w_down": [NL, HID, D], "norm_final": [D],
}


def make_consts():
    p = np.arange(128)
    same = (p[:, None] // 64) == (p[None, :] // 64)
    c = {}
    c["ident"] = np.eye(128, dtype=np.float32)
    c["trilT"] = (p[:, None] <= p[None, :]).astype(np.float32)
    t = np.arange(TT)
    c["rm"] = np.broadcast_to((t % 64 != 0).astype(np.float32), (128, TT)).copy()
    c["mg"] = (same & (p[:, None] <= p[None, :])).astype(np.float32)
    c["hm"] = ((p[:, None] // 32) == (np.arange(256)[None, :] // 64)).astype(np.float32)
    c["hcol"] = ((p[:, None] // 32) == np.arange(4)[None, :]).astype(np.float32)
    c["topbot"] = np.stack([(p < 64), (p >= 64)], axis=1).astype(np.float32)
    c["cmA"] = np.broadcast_to(((t % 128) < 64).astype(np.float32), (128, TT)).copy()
    c["U"] = np.where(same & (p[None, :] <= p[:, None]), 0.0, 80.0).astype(np.float32)
    c["L"] = np.where(same & (p[None, :] >= p[:, None]), 0.0, -80.0).astype(np.float32)
    c["strict"] = (same & (p[None, :] < p[:, None])).astype(np.float32)
    sel = np.zeros((4, 4, 128), np.float32)
    for h in range(4):
        sel[h, h, :] = 1.0
    c["sel4"] = sel
    cc = np.zeros((96, 2), np.float32)
    f = (10000.0 ** (-np.arange(0, 32, 2, dtype=np.float32) / 32)).astype(np.float32)
    cc[64:80, 0] = f
    cc[80:96, 0] = f
    cc[64:80, 1] = -1.0
    cc[80:96, 1] = 1.0
    c["cconst"] = cc
    return c


CONST_SHAPES = {k: list(v.shape) for k, v in make_consts().items()}


def build(nl=NL, ntiles=NT, phases=("rope", "A", "mla", "merge", "xattn", "mlp", "final"), dbg=(), doA=("mla", "sg", "gla", "dn")):
    nc = bass.Bass("TRN2", target_bir_lowering=False)

    def dt(name, shape, dtype, kind):
        return nc.dram_tensor(name, shape, dtype, kind=kind).ap()

    xT = dt("xT", [D, SEQ], F32, "ExternalInput")
    memT = dt("memT", [D, 256], F32, "ExternalInput")
    posrep = dt("posrep", [96, SEQ], I32, "ExternalInput")
    P = {k: dt(k, s, F32, "ExternalInput") for k, s in PARAM_SHAPES.items()}
    K = {k: dt("c_" + k, s, F32, "ExternalInput") for k, s in CONST_SHAPES.items()}
    outT = dt("outT", [D, SEQ], F32, "ExternalOutput")

    def scr(name, shape, dtype):
        return dt(name, shape, dtype, "ExternalOutput" if name in dbg else "Internal")

    sc = {
        "hT_s": scr("hT_s", [D, SEQ], BF16), "qT_s": scr("qT_s", [4, 96, SEQ], BF16), "kT_s": scr("kT_s", [4, 96, SEQ], BF16),
        "v_s": scr("v_s", [SEQ, 260], BF16), "oaT_s": scr("oaT_s", [4, 64, SEQ], BF16), "oT_s": scr("oT_s", [3, 256, SEQ], BF16),
        "cos_s": scr("cos_s", [96, SEQ], F32), "sin_s": scr("sin_s", [96, SEQ], F32),
        "wup_bf": scr("wup_bf", [NL, D, HID], BF16), "wdn_bf": scr("wdn_bf", [NL, HID, D], BF16), "wg_bf": scr("wg_bf", [NL, D, 4096], BF16),
        "xa": scr("xa", [D, SEQ], F32), "xb": scr("xb", [D, SEQ], F32), "xc": scr("xc", [D, SEQ], F32),
    }
    S = Sched(nc)
    phase_precast(S, nc, P, sc)
    if "rope" in phases:
        phase_rope(S, nc, posrep, K["cconst"], sc["cos_s"], sc["sin_s"])
    cur = xT
    for l in range(nl):
        if "A" in phases:
            phase_A(S, nc, l, cur, P, K, sc, ntiles=(None if ntiles == NT else 2 * ntiles), do=doA)
        if "mla" in phases:
            phase_mla(S, nc, sc["qT_s"], sc["kT_s"], sc["v_s"], sc["oaT_s"], K["trilT"], nq=ntiles)
        if "merge" in phases:
            phase_merge(S, nc, l, cur, sc["xa"], sc["hT_s"], sc["oaT_s"], sc["oT_s"], sc["wg_bf"], P["w_branch"], P["w_out"], ntiles=ntiles)
            cur = sc["xa"]
        if "xattn" in phases:
            phase_xattn(S, nc, l, cur, sc["xb"], memT, P["norm_xattn"], P["norm_mem"], P["xattn_wq"], P["xattn_wk"], P["xattn_wv"], P["xattn_wo"], ntiles=ntiles)
            cur = sc["xb"]
        if "mlp" in phases:
            phase_mlp(S, nc, l, cur, sc["xc"], P["norm_mlp"], sc["wup_bf"], sc["wdn_bf"], ntiles=ntiles)
            cur = sc["xc"]
    if "final" in phases:
        phase_final(S, nc, cur, outT, P["norm_final"], ntiles=ntiles)
    S.emit()
    return nc


def make_in_maps(inputs, cores):
    consts = make_consts()
    x = np.asarray(inputs["x"], dtype=np.float32)
    mem = np.asarray(inputs["mem"], dtype=np.float32)
    pos = np.asarray(inputs["positions"]).astype(np.int32)
    shared = {k: np.ascontiguousarray(np.asarray(inputs[k], dtype=np.float32)) for k in PARAM_SHAPES}
    for k, v in consts.items():
        shared["c_" + k] = v
    in_maps = []
    for b in cores:
        m = dict(shared)
        m["xT"] = np.ascontiguousarray(x[b].T)
        m["memT"] = np.ascontiguousarray(mem[b].T)
        m["posrep"] = np.ascontiguousarray(np.broadcast_to(pos[b][None, :], (96, SEQ)))
        in_maps.append(m)
    return in_maps


def kernel(**inputs):
    B = np.asarray(inputs["x"]).shape[0]
    nc = build()
    in_maps = make_in_maps(inputs, list(range(B)))
    res = run_bass_kernel_spmd(nc, in_maps, core_ids=list(range(B)))
    out = np.stack([np.ascontiguousarray(np.asarray(r["outT"]).T) for r in res.results], axis=0)
    return out.astype(np.float32)
```

```python
from contextlib import ExitStack
import numpy as np
import concourse.bass as bass
import concourse.mybir as mybir
from concourse.bass_utils import run_bass_kernel_spmd

F32 = mybir.dt.float32
BF16 = mybir.dt.bfloat16
I32 = mybir.dt.int32
AF = mybir.ActivationFunctionType
ALU = mybir.AluOpType

D = 1024
SEQ = 4096
NL = 2
TT = 512
NT = SEQ // TT
EPS = 1e-6
HID = 4096
INW = 6840
DBG = 9
DNSTAGE = 9
DNV = ''


class Sched:
    ENGS = ("pe", "act", "dve", "pool", "sp")
    EPOCH = 12000
    RING = 8

    def __init__(self, nc):
        self.nc = nc
        self.stream = {e: [] for e in self.ENGS}
        self.vt = {e: 0 for e in self.ENGS}
        self.known = {e: {} for e in self.ENGS}
        self.need_sig = {e: set() for e in self.ENGS}
        self.last_write = {}
        self.readers = {}
        self.op_sig = []
        self.op_clock = []
        self.op_eng = []
        self.op_dma = []
        self.dma_n = {e: 0 for e in self.ENGS}
        self.last_sig = {}

    def _emit_wait(self, eng, key, val):
        self.stream[eng].append(("wait", key, val))
        if key[0] == "eng":
            self.need_sig[key[1]].add(val)

    def _wait(self, eng, dep):
        key, val = self.op_sig[dep]
        kn = self.known[eng]
        if kn.get(key, 0) >= val:
            return
        self._emit_wait(eng, key, val)
        for k, v in self.op_clock[dep].items():
            if kn.get(k, 0) < v:
                kn[k] = v
        kn[key] = max(kn.get(key, 0), val)

    def add(self, eng, fn, reads=(), writes=(), signal=True, dma=False):
        deps = set()
        for r in reads:
            w = self.last_write.get(r)
            if w is not None:
                deps.add(w)
        for w_ in writes:
            w = self.last_write.get(w_)
            if w is not None:
                deps.add(w)
            for r in self.readers.get(w_, ()):
                deps.add(r)
        opid = len(self.op_sig)
        for d in sorted(deps):
            if (not dma) and (not self.op_dma[d]) and self.op_eng[d] == eng:
                if eng == "pe":
                    continue
                israw = any(self.last_write.get(r) == d for r in reads)
                if not israw:
                    continue
            self._wait(eng, d)
        if dma:
            n = self.dma_n[eng]
            self.dma_n[eng] += 1
            slot = n % self.RING
            key = ("dma", eng, slot)
            val = 16 * (n // self.RING + 1)
            if val > 16:
                kn = self.known[eng]
                if kn.get(key, 0) < val - 16:
                    self._emit_wait(eng, key, val - 16)
                    kn[key] = val - 16
            self.stream[eng].append(("op", fn, None, key))
        else:
            self.vt[eng] += 1
            key = ("eng", eng)
            val = self.vt[eng]
            self.stream[eng].append(("op", fn, val, None))
        clock = dict(self.known[eng])
        clock[key] = val
        self.op_sig.append((key, val))
        self.op_clock.append(clock)
        self.op_eng.append(eng)
        self.op_dma.append(dma)
        self.last_sig[key] = max(self.last_sig.get(key, 0), val)
        for r in reads:
            self.readers.setdefault(r, []).append(opid)
        for w_ in writes:
            self.last_write[w_] = opid
            self.readers[w_] = []
        return opid

    def barrier(self):
        for eng in self.ENGS:
            kn = self.known[eng]
            for key, val in list(self.last_sig.items()):
                if kn.get(key, 0) < val:
                    self._emit_wait(eng, key, val)
                    kn[key] = val
        self.last_write = {}
        self.readers = {}

    def emit(self):
        nc = self.nc
        vmap = {}
        semkeys = set()
        for e in self.ENGS:
            cnt = 0
            ep = 0
            for vt in sorted(self.need_sig[e]):
                if cnt >= self.EPOCH:
                    ep += 1
                    cnt = 0
                cnt += 1
                vmap[(e, vt)] = (("eng", e, ep), cnt)
                semkeys.add(("eng", e, ep))
            for it in self.stream[e]:
                if it[0] == "op" and it[3] is not None:
                    semkeys.add(it[3])
        with ExitStack() as es:
            sems = {}
            for key in sorted(semkeys, key=str):
                sems[key] = es.enter_context(nc.semaphore("s_" + "_".join(str(k) for k in key)))
            block = es.enter_context(nc.Block())
            engmap = {"pe": block.tensor, "act": block.scalar, "dve": block.vector,
                      "pool": block.gpsimd, "sp": block.sync}
            for ename in self.ENGS:
                items = self.stream[ename]

                def body(eng, items=items, ename=ename):
                    for it in items:
                        if it[0] == "wait":
                            key, val = it[1], it[2]
                            if key[0] == "eng":
                                key, val = vmap[(key[1], val)]
                            eng.wait_ge(sems[key], val)
                        else:
                            ins = it[1](eng)
                            if it[3] is not None:
                                ins.then_inc(sems[it[3]], 16)
                            elif (ename, it[2]) in vmap:
                                ins.then_inc(sems[vmap[(ename, it[2])][0]], 1)
                engmap[ename](body)


class Rot:
    def __init__(self, es, alloc, name, shape, dtype, n):
        self.tiles = [es.enter_context(alloc(f"{name}{i}", shape, dtype)) for i in range(n)]
        self.name = name
        self.i = 0

    def next(self):
        k = self.i % len(self.tiles)
        self.i += 1
        return self.tiles[k], (self.name, k)


def mm(S, out, lhsT, rhs, start, stop, r, w):
    S.add("pe", lambda e: e.matmul(out, lhsT, rhs, start=start, stop=stop), reads=r, writes=w)


def tr(S, out, in_, ident, r, w):
    S.add("pe", lambda e: e.transpose(out, in_, ident), reads=r, writes=w)


def act(S, out, in_, func, r, w, **kw):
    S.add("act", lambda e: e.activation(out=out, in_=in_, func=func, **kw), reads=r, writes=w)


def tt(S, eng, out, a, b, op, r, w):
    S.add(eng, lambda e: e.tensor_tensor(out=out, in0=a, in1=b, op=op), reads=r, writes=w)


def ts(S, eng, out, a, s1, op0, r, w, s2=None, op1=None):
    if op1 is None:
        S.add(eng, lambda e: e.tensor_scalar(out=out, in0=a, scalar1=s1, scalar2=None, op0=op0), reads=r, writes=w)
    else:
        S.add(eng, lambda e: e.tensor_scalar(out=out, in0=a, scalar1=s1, scalar2=s2, op0=op0, op1=op1), reads=r, writes=w)


def stt(S, out, a, scalar, b, op0, op1, r, w):
    S.add("dve", lambda e: e.scalar_tensor_tensor(out=out, in0=a, scalar=scalar, in1=b, op0=op0, op1=op1), reads=r, writes=w)


def cp(S, eng, out, in_, r, w):
    if eng == "act":
        S.add("act", lambda e: e.activation(out=out, in_=in_, func=AF.Copy), reads=r, writes=w)
    else:
        S.add(eng, lambda e: e.tensor_copy(out=out, in_=in_), reads=r, writes=w)


def dma(S, q, out, in_, r, w, slow=False):
    if slow:
        S.add(q, lambda e: e.dma_start(out=out, in_=in_, allow_slow_non_contiguous=True), reads=r, writes=w, dma=True)
    else:
        S.add(q, lambda e: e.dma_start(out=out, in_=in_), reads=r, writes=w, dma=True)


def memset(S, eng, ap, val, w, r=()):
    S.add(eng, lambda e: e.memset(ap, val), reads=r, writes=w)


class Ctx:
    count = 0

    def __init__(self, nc, S, es, prefix):
        Ctx.count += 1
        self.nc, self.S, self.es, self.p = nc, S, es, f"{prefix}{Ctx.count}_"
        self.n = 0

    def sb(self, name, shape, dtype=F32):
        return self.es.enter_context(self.nc.sbuf_tensor(self.p + name, shape, dtype))

    def ps(self, name, shape, dtype=F32):
        return self.es.enter_context(self.nc.psum_tensor(self.p + name, shape, dtype))

    def rot(self, name, shape, dtype, n, psum=False):
        alloc = (lambda a, b, c: self.nc.psum_tensor(a, b, c)) if psum else (lambda a, b, c: self.nc.sbuf_tensor(a, b, c))
        return Rot(self.es, alloc, self.p + name, shape, dtype, n)


def rms_fm(S, src, src_res, nk, nfeat, gcol, ones_bf, sq_rot, psr, tmp, tmp_res, rstd, rstd_res, dst, dst_res, width, pn=128):
    pt, pres = psr.next()
    for k in range(nk):
        sq, sqr = sq_rot.next()
        act(S, sq[:pn, :width], src[:pn, k, :width], AF.Square, [src_res[k]], [sqr])
        mm(S, pt[:pn, :width], ones_bf[:pn, :pn], sq[:pn, :width], k == 0, k == nk - 1, [sqr, "ones"], [pres])
    act(S, tmp[:pn, :width], pt[:pn, :width], AF.Sqrt, [pres], [tmp_res], scale=1.0 / nfeat, bias=EPS)
    S.add("dve", lambda e: e.reciprocal(out=rstd[:pn, :width], in_=tmp[:pn, :width]), reads=[tmp_res], writes=[rstd_res])
    for k in range(nk):
        stt(S, dst[:pn, k, :width], src[:pn, k, :width], gcol[:pn, k:k + 1], rstd[:pn, :width], ALU.mult, ALU.mult,
            [src_res[k], rstd_res], [dst_res[k]])


def phase_mlp(S, nc, l, xin, xout, norm_mlp, w_up, w_down, ntiles=NT, wq_="sp"):
    with ExitStack() as es:
        C = Ctx(nc, S, es, "m_")
        ones_bf = C.sb("ones", [128, 128], BF16)
        gcol = C.sb("g", [128, 8])
        xt_rot = C.rot("xt", [128, 8, TT], F32, 2)
        sq_rot = C.rot("sq", [128, TT], BF16, 2)
        hT = C.sb("hT", [128, 8, TT], BF16)
        tmp = C.sb("tmp", [128, TT])
        rstd = C.sb("rstd", [128, TT])
        r_rot = C.rot("r", [128, TT], BF16, 3)
        aT = C.sb("aT", [128, 32, TT], BF16)
        wup_rot = C.rot("wup", [128, 8, 512], BF16, 2)
        wdn_rot = C.rot("wdn", [128, 32, 256], BF16, 2)
        psr = C.rot("ps", [128, TT], F32, 7, psum=True)
        memset(S, "pool", ones_bf[:, :], 1.0, ["ones"])
        dma(S, "sp", gcol[:, :], norm_mlp[l].rearrange("(c p) -> p c", p=128), [], ["g"], slow=True)
        xin_v = xin.rearrange("(c p) n -> p c n", p=128)
        xout_v = xout.rearrange("(c p) n -> p c n", p=128)
        wup_v = w_up[l].rearrange("(c p) n -> p c n", p=128)
        wdn_v = w_down[l].rearrange("(c p) n -> p c n", p=128)
        for t in range(ntiles):
            tok = slice(t * TT, (t + 1) * TT)
            xt, x0 = xt_rot.next()
            xres = [(x0, d) for d in range(8)]
            dma(S, "sp", xt[:, :, :], xin_v[:, :, tok], [("xin", t)], xres)
            hres = [("m_hT", k) for k in range(8)]
            rms_fm(S, xt, xres, 8, D, gcol, ones_bf, sq_rot, psr, tmp, "m_tmp", rstd, "m_rstd", hT, hres, TT)
            for jb in range(8):
                wu, wures = wup_rot.next()
                dma(S, wq_, wu[:, :, :], wup_v[:, :, jb * 512:(jb + 1) * 512], [], [wures])
                for jj in range(4):
                    j = jb * 4 + jj
                    pt, pres = psr.next()
                    for k in range(8):
                        mm(S, pt[:, :], wu[:, k, jj * 128:(jj + 1) * 128], hT[:, k, :], k == 0, k == 7, [wures, hres[k]], [pres])
                    r, rres = r_rot.next()
                    act(S, r[:, :], pt[:, :], AF.Relu, [pres], [rres])
                    tt(S, "dve", aT[:, j, :], r[:, :], r[:, :], ALU.mult, [rres], [("m_aT", j)])
            for db in range(4):
                wd, wdres = wdn_rot.next()
                dma(S, wq_, wd[:, :, :], wdn_v[:, :, db * 256:(db + 1) * 256], [], [wdres])
                for dd in range(2):
                    d = db * 2 + dd
                    pt, pres = psr.next()
                    for j in range(32):
                        mm(S, pt[:, :], wd[:, j, dd * 128:(dd + 1) * 128], aT[:, j, :], j == 0, j == 31, [wdres, ("m_aT", j)], [pres])
                    tt(S, "dve", xt[:, d, :], xt[:, d, :], pt[:, :], ALU.add, [pres, xres[d]], [xres[d]])
            dma(S, "sp", xout_v[:, :, tok], xt[:, :, :], xres, [("xout", t)])
    S.barrier()


def phase_final(S, nc, xin, xout, norm_final, ntiles=NT):
    with ExitStack() as es:
        C = Ctx(nc, S, es, "f_")
        ones_bf = C.sb("ones", [128, 128], BF16)
        gcol = C.sb("g", [128, 8])
        xt_rot = C.rot("xt", [128, 8, TT], F32, 2)
        ot_rot = C.rot("ot", [128, 8, TT], F32, 2)
        sq_rot = C.rot("sq", [128, TT], BF16, 2)
        tmp = C.sb("tmp", [128, TT])
        rstd = C.sb("rstd", [128, TT])
        psr = C.rot("ps", [128, TT], F32, 2, psum=True)
        memset(S, "pool", ones_bf[:, :], 1.0, ["ones"])
        dma(S, "sp", gcol[:, :], norm_final.rearrange("(c p) -> p c", p=128), [], ["g"], slow=True)
        xin_v = xin.rearrange("(c p) n -> p c n", p=128)
        xout_v = xout.rearrange("(c p) n -> p c n", p=128)
        for t in range(ntiles):
            tok = slice(t * TT, (t + 1) * TT)
            xt, x0 = xt_rot.next()
            xres = [(x0, d) for d in range(8)]
            dma(S, "sp", xt[:, :, :], xin_v[:, :, tok], [("xin", t)], xres)
            ot, o0 = ot_rot.next()
            ores = [(o0, d) for d in range(8)]
            rms_fm(S, xt, xres, 8, D, gcol, ones_bf, sq_rot, psr, tmp, "f_tmp", rstd, "f_rstd", ot, ores, TT)
            dma(S, "sp", xout_v[:, :, tok], ot[:, :, :], ores, [("xout", t)])
    S.barrier()


def phase_xattn(S, nc, l, xin, xout, memT, norm_x, norm_mem, wq, wk, wv, wo, ntiles=NT):
    with ExitStack() as es:
        C = Ctx(nc, S, es, "x_")
        ones_bf = C.sb("ones", [128, 128], BF16)
        gx = C.sb("gx", [128, 8])
        gm = C.sb("gm", [128, 8])
        mt = C.sb("mt", [128, 8, 256])
        mn = C.sb("mn", [128, 8, 256], BF16)
        wq_t = C.sb("wq", [128, 8, 1024], BF16)
        wo_t = C.sb("wo", [128, 8, 1024], BF16)
        wtmp_rot = C.rot("wtmp", [128, 8, 1024], BF16, 1)
        KT = C.sb("KT", [128, 8, 256], BF16)
        V = C.sb("V", [128, 2, 1024], BF16)
        xt_rot = C.rot("xt", [128, 8, TT], F32, 2)
        sq_rot = C.rot("sq", [128, TT], BF16, 2)
        hT = C.sb("hT", [128, 8, TT], BF16)
        qT = C.sb("qT", [128, 8, TT], BF16)
        oT = C.sb("oT", [128, 8, TT], BF16)
        pT_rot = C.rot("pT", [128, 2, TT], BF16, 2)
        tmp = C.sb("tmp", [128, TT])
        rstd = C.sb("rstd", [128, TT])
        rinv = C.sb("rinv", [128, TT])
        psr = C.rot("ps", [128, TT], F32, 7, psum=True)
        memset(S, "pool", ones_bf[:, :], 1.0, ["ones"])
        dma(S, "sp", gx[:, :], norm_x[l].rearrange("(c p) -> p c", p=128), [], ["gx"], slow=True)
        dma(S, "sp", gm[:, :], norm_mem[l].rearrange("(c p) -> p c", p=128), [], ["gm"], slow=True)
        dma(S, "sp", mt[:, :, :], memT.rearrange("(c p) n -> p c n", p=128), [], [("mt", k) for k in range(8)])
        dma(S, "pool", wq_t[:, :, :], wq[l].rearrange("(c p) n -> p c n", p=128), [], ["wq"])
        dma(S, "pool", wo_t[:, :, :], wo[l].rearrange("(c p) n -> p c n", p=128), [], ["wo"])
        mres = [("mn", k) for k in range(8)]
        rms_fm(S, mt, [("mt", k) for k in range(8)], 8, D, gm, ones_bf, sq_rot, psr, tmp, "x_tmp", rstd, "x_rstd", mn, mres, 256)
        wt, wres = wtmp_rot.next()
        dma(S, "pool", wt[:, :, :], wk[l].rearrange("(c p) n -> p c n", p=128), [], [wres])
        for c in range(8):
            pt, pres = psr.next()
            for k in range(8):
                mm(S, pt[:, :256], wt[:, k, c * 128:(c + 1) * 128], mn[:, k, :], k == 0, k == 7, [wres, mres[k]], [pres])
            cp(S, "act", KT[:, c, :], pt[:, :256], [pres], [("KT", c)])
        wt, wres = wtmp_rot.next()
        dma(S, "pool", wt[:, :, :], wv[l].rearrange("(c p) n -> p c n", p=128), [], [wres])
        for ms in range(2):
            for cb in range(2):
                pt, pres = psr.next()
                for k in range(8):
                    mm(S, pt[:, :], mn[:, k, ms * 128:(ms + 1) * 128], wt[:, k, cb * 512:(cb + 1) * 512], k == 0, k == 7, [wres, mres[k]], [pres])
                cp(S, "act", V[:, ms, cb * 512:(cb + 1) * 512], pt[:, :], [pres], [("V", ms, cb)])
        xin_v = xin.rearrange("(c p) n -> p c n", p=128)
        xout_v = xout.rearrange("(c p) n -> p c n", p=128)
        for t in range(ntiles):
            tok = slice(t * TT, (t + 1) * TT)
            xt, x0 = xt_rot.next()
            xres = [(x0, d) for d in range(8)]
            dma(S, "sp", xt[:, :, :], xin_v[:, :, tok], [("xin", t)], xres)
            hres = [("x_hT", k) for k in range(8)]
            rms_fm(S, xt, xres, 8, D, gx, ones_bf, sq_rot, psr, tmp, "x_tmp", rstd, "x_rstd", hT, hres, TT)
            for c in range(8):
                pt, pres = psr.next()
                for k in range(8):
                    mm(S, pt[:, :], wq_t[:, k, c * 128:(c + 1) * 128], hT[:, k, :], k == 0, k == 7, ["wq", hres[k]], [pres])
                cp(S, "act" if c % 2 else "dve", qT[:, c, :], pt[:, :], [pres], [("qT", c)])
            for h in range(4):
                pT, pTres = pT_rot.next()
                for ms in range(2):
                    pt, pres = psr.next()
                    for cc in range(2):
                        c = h * 2 + cc
                        mm(S, pt[:, :], KT[:, c, ms * 128:(ms + 1) * 128], qT[:, c, :], cc == 0, cc == 1, [("KT", c), ("qT", c)], [pres])
                    act(S, pT[:, ms, :], pt[:, :], AF.Exp, [pres], [(pTres, ms)], scale=1.0 / 16.0)
                pt, pres = psr.next()
                for ms in range(2):
                    mm(S, pt[:, :], ones_bf[:, :], pT[:, ms, :], ms == 0, ms == 1, ["ones", (pTres, ms)], [pres])
                S.add("dve", lambda e, pt=pt: e.reciprocal(out=rinv[:, :], in_=pt[:, :]), reads=[pres], writes=["rinv"])
                for cc in range(2):
                    c = h * 2 + cc
                    pt, pres = psr.next()
                    for ms in range(2):
                        mm(S, pt[:, :], V[:, ms, c * 128:(c + 1) * 128], pT[:, ms, :], ms == 0, ms == 1,
                           [("V", ms, c // 4), (pTres, ms)], [pres])
                    tt(S, "dve", oT[:, c, :], pt[:, :], rinv[:, :], ALU.mult, [pres, "rinv"], [("oT", c)])
            for d in range(8):
                pt, pres = psr.next()
                for c in range(8):
                    mm(S, pt[:, :], wo_t[:, c, d * 128:(d + 1) * 128], oT[:, c, :], c == 0, c == 7, ["wo", ("oT", c)], [pres])
                tt(S, "dve", xt[:, d, :], xt[:, d, :], pt[:, :], ALU.add, [pres, xres[d]], [xres[d]])
            dma(S, "sp", xout_v[:, :, tok], xt[:, :, :], xres, [("xout", t)])
    S.barrier()


def phase_merge(S, nc, l, xin, xout, hT_s, oaT_s, oT_s, wg_bf, w_branch, w_out, ntiles=NT):
    with ExitStack() as es:
        C = Ctx(nc, S, es, "g_")
        wba = C.sb("wba", [64, 4, 1024], BF16)
        wb = C.sb("wb", [128, 3, 2, 1024], BF16)
        wout = C.sb("wout", [128, 8, 1024], BF16)
        wg_rot = C.rot("wg", [128, 8, 1024], BF16, 2)
        xt_rot = C.rot("xt", [128, 8, TT], F32, 2)
        h_rot = C.rot("h", [128, 8, TT], BF16, 2)
        oa_rot = C.rot("oa", [64, 4, TT], BF16, 2)
        ob_rot = C.rot("ob", [128, 3, 2, TT], BF16, 2)
        sig_rot = C.rot("sig", [128, TT], BF16, 3)
        prod_rot = C.rot("prod", [128, TT], F32, 3)
        acc = C.sb("acc", [128, 8, TT])
        mT = C.sb("mT", [128, 8, TT], BF16)
        psr = C.rot("ps", [128, TT], F32, 7, psum=True)
        dma(S, "pool", wba[:, :, :], w_branch[l, 0].rearrange("(h p) n -> p h n", p=64), [], ["wba"])
        for n in range(3):
            dma(S, "pool", wb[:, n, :, :], w_branch[l, n + 1].rearrange("(c p) n -> p c n", p=128), [], [("wb", n)])
        dma(S, "pool", wout[:, :, :], w_out[l].rearrange("(c p) n -> p c n", p=128), [], ["wout"])
        xin_v = xin.rearrange("(c p) n -> p c n", p=128)
        xout_v = xout.rearrange("(c p) n -> p c n", p=128)
        hv = hT_s.rearrange("(c p) n -> p c n", p=128)
        win_v = wg_bf[l].rearrange("(c p) n -> p c n", p=128)
        for t in range(ntiles):
            tok = slice(t * TT, (t + 1) * TT)
            xt, x0 = xt_rot.next()
            xres = [(x0, d) for d in range(8)]
            dma(S, "sp", xt[:, :, :], xin_v[:, :, tok], [("xin", t)], xres)
            ht, hres = h_rot.next()
            dma(S, "sp", ht[:, :, :], hv[:, :, tok], [("hT_s", t)], [hres])
            oa, oares = oa_rot.next()
            dma(S, "sp", oa[:, :, :], oaT_s.rearrange("h p n -> p h n")[:, :, tok], [("oaT_s", t)], [oares])
            ob, obres = ob_rot.next()
            for n in range(3):
                dma(S, "sp", ob[:, n, :, :], oT_s[n].rearrange("(c p) n -> p c n", p=128)[:, :, tok], [("oT_s", n, t)], [(obres, n)])
            for n in range(4):
                wg, wgres = wg_rot.next()
                dma(S, "sp", wg[:, :, :], win_v[:, :, n * 1024:(n + 1) * 1024], [], [wgres])
                for d in range(8):
                    pt, pres = psr.next()
                    for k in range(8):
                        mm(S, pt[:, :], wg[:, k, d * 128:(d + 1) * 128], ht[:, k, :], k == 0, k == 7, [wgres, hres], [pres])
                    sg, sgres = sig_rot.next()
                    act(S, sg[:, :], pt[:, :], AF.Sigmoid, [pres], [sgres])
                    pt2, pres2 = psr.next()
                    if n == 0:
                        for h in range(4):
                            mm(S, pt2[:, :], wba[:, h, d * 128:(d + 1) * 128], oa[:, h, :], h == 0, h == 3, ["wba", oares], [pres2])
                    else:
                        for c in range(2):
                            mm(S, pt2[:, :], wb[:, n - 1, c, d * 128:(d + 1) * 128], ob[:, n - 1, c, :], c == 0, c == 1,
                               [("wb", n - 1), (obres, n - 1)], [pres2])
                    if n == 0:
                        tt(S, "dve", acc[:, d, :], pt2[:, :], sg[:, :], ALU.mult, [pres2, sgres], [("acc", d)])
                    else:
                        pr, prres = prod_rot.next()
                        tt(S, "dve", pr[:, :], pt2[:, :], sg[:, :], ALU.mult, [pres2, sgres], [prres])
                        if n < 3:
                            tt(S, "pool", acc[:, d, :], acc[:, d, :], pr[:, :], ALU.add, [("acc", d), prres], [("acc", d)])
                        else:
                            tt(S, "pool", mT[:, d, :], acc[:, d, :], pr[:, :], ALU.add, [("acc", d), prres], [("mT", d)])
            for d in range(8):
                pt, pres = psr.next()
                for c in range(8):
                    mm(S, pt[:, :], wout[:, c, d * 128:(d + 1) * 128], mT[:, c, :], c == 0, c == 7, ["wout", ("mT", c)], [pres])
                tt(S, "dve", xt[:, d, :], xt[:, d, :], pt[:, :], ALU.add, [pres, xres[d]], [xres[d]])
            dma(S, "sp", xout_v[:, :, tok], xt[:, :, :], xres, [("xout", t)])
    S.barrier()


def phase_mla(S, nc, qT_s, kT_s, v_s, oaT_s, cmask_d, nq=NT):
    with ExitStack() as es:
        C = Ctx(nc, S, es, "a_")
        KT = C.sb("KT", [96, SEQ], BF16)
        V = C.sb("V", [128, SEQ // 128, 65], BF16)
        q_rot = C.rot("q", [96, TT], BF16, 2)
        pT_rot = C.rot("pT", [128, TT], BF16, 4)
        cm = C.sb("cm", [128, 128], BF16)
        ones1 = C.sb("ones1", [65, 64])
        oS = C.sb("oS", [65, TT])
        rinv = C.sb("rinv", [65, TT])
        o_rot = C.rot("o", [64, TT], BF16, 2)
        psr = C.rot("ps", [128, TT], F32, 4, psum=True)
        pso = C.rot("pso", [128, TT], F32, 2, psum=True)
        psb = C.rot("psb", [128, TT], F32, 1, psum=True)
        dma(S, "pool", cm[:, :], cmask_d, [], ["cm"])
        memset(S, "pool", ones1[:, :], 1.0, ["ones1"])
        scale = float(96 ** -0.5)
        for h in range(4):
            dma(S, "sp", KT[:, :], kT_s[h], [("kT_s",)], ["KT"])
            dma(S, "sp", V[:, :, :], v_s.rearrange("(n p) (h e) -> p n h e", p=128, e=65)[:, :, h, :], [("v_s",)], ["V"], slow=True)
            for qt in range(nq):
                q, qres = q_rot.next()
                dma(S, "sp", q[:, :], qT_s[h][:, qt * TT:(qt + 1) * TT], [("qT_s",)], [qres])
                po, pores = pso.next()
                nkb = 4 * qt + 4
                for kb in range(nkb):
                    r = kb - 4 * qt
                    q0 = max(r, 0) * 128
                    pt, pres = psr.next()
                    mm(S, pt[:, q0:], KT[:, kb * 128:(kb + 1) * 128], q[:, q0:], True, True, ["KT", qres], [pres])
                    pT, pTres = pT_rot.next()
                    act(S, pT[:, q0:], pt[:, q0:], AF.Exp, [pres], [pTres], scale=scale)
                    if r >= 0:
                        tt(S, "pool", pT[:, q0:q0 + 128], pT[:, q0:q0 + 128], cm[:, :], ALU.mult, [pTres, "cm"], [pTres])
                    mm(S, po[:65, q0:], V[:, kb, :], pT[:, q0:], kb == 0, kb == nkb - 1, ["V", pTres], [pores])
                cp(S, "act", oS[:, :], po[:65, :], [pores], ["oS"])
                S.add("dve", lambda e: e.reciprocal(out=rinv[64:65, :], in_=oS[64:65, :]), reads=["oS"], writes=["rinv"])
                pb, pbres = psb.next()
                mm(S, pb[:64, :], ones1[64:65, :], rinv[64:65, :], True, True, ["ones1", "rinv"], [pbres])
                o, ores = o_rot.next()
                tt(S, "dve", o[:, :], pb[:64, :], oS[:64, :], ALU.mult, [pbres, "oS"], [ores])
                dma(S, "sp", oaT_s[h][:, qt * TT:(qt + 1) * TT], o[:, :], [ores], [("oaT_s", qt)])
    S.barrier()


def phase_precast(S, nc, P, sc):
    with ExitStack() as es:
        C = Ctx(nc, S, es, "pc_")
        st_rot = C.rot("st", [128, 8, 512], F32, 3)
        bf_rot = C.rot("bf", [128, 8, 512], BF16, 3)
        n = 0
        for l in range(NL):
            jobs = []
            for jb in range(8):
                jobs.append((P["w_up"][l].rearrange("(c p) n -> p c n", p=128)[:, :, jb * 512:(jb + 1) * 512],
                             sc["wup_bf"][l].rearrange("(c p) n -> p c n", p=128)[:, :, jb * 512:(jb + 1) * 512]))
                jobs.append((P["w_in"][l].rearrange("(c p) n -> p c n", p=128)[:, :, 2744 + jb * 512:2744 + (jb + 1) * 512],
                             sc["wg_bf"][l].rearrange("(c p) n -> p c n", p=128)[:, :, jb * 512:(jb + 1) * 512]))
            for cb in range(4):
                for nb in range(2):
                    jobs.append((P["w_down"][l].rearrange("(c p) n -> p c n", p=128)[:, cb * 8:(cb + 1) * 8, nb * 512:(nb + 1) * 512],
                                 sc["wdn_bf"][l].rearrange("(c p) n -> p c n", p=128)[:, cb * 8:(cb + 1) * 8, nb * 512:(nb + 1) * 512]))
            for src, dst in jobs:
                st, stres = st_rot.next()
                bf, bfres = bf_rot.next()
                dma(S, "sp", st[:, :, :], src, [], [stres])
                eng = ("act", "dve", "pool")[n % 3]
                n += 1
                cp(S, eng, bf[:, :, :], st[:, :, :], [stres], [bfres])
                dma(S, "sp", dst, bf[:, :, :], [bfres], [("pc_out", n)])
    S.barrier()

def phase_rope(S, nc, posrep, cconst, cos_s, sin_s):
    with ExitStack() as es:
        C = Ctx(nc, S, es, "r_")
        pi_ = C.sb("pi", [96, TT], I32)
        pf = C.sb("pf", [96, TT])
        ang = C.sb("ang", [96, TT])
        ni = C.sb("ni", [96, TT], I32)
        nf = C.sb("nf", [96, TT])
        y = C.sb("y", [96, TT])
        cc = C.sb("cc", [96, 2])
        dma(S, "sp", cc[:, :], cconst, [], ["cc"])
        for t in range(NT):
            tok = slice(t * TT, (t + 1) * TT)
            dma(S, "sp", pi_[:, :], posrep[:, tok], [], ["pi"])
            cp(S, "dve", pf[:, :], pi_[:, :], ["pi"], ["pf"])
            for which in range(2):
                off = 0.0 if which == 0 else float(np.pi / 2)
                ts(S, "dve", ang[:, :], pf[:, :], cc[:, 0:1], ALU.mult, ["pf", "cc"], ["ang"], s2=off, op1=ALU.add)
                ts(S, "dve", ni[:, :], ang[:, :], float(1 / (2 * np.pi)), ALU.mult, ["ang"], ["ni"])
                cp(S, "dve", nf[:, :], ni[:, :], ["ni"], ["nf"])
                stt(S, y[:, :], nf[:, :], float(-2 * np.pi), ang[:, :], ALU.mult, ALU.add, ["nf", "ang"], ["y"])
                ts(S, "dve", nf[:, :], y[:, :], float(np.pi), ALU.is_gt, ["y"], ["nf2"], s2=float(-2 * np.pi), op1=ALU.mult)
                tt(S, "dve", y[:, :], y[:, :], nf[:, :], ALU.add, ["y", "nf2"], ["y2"])
                ts(S, "dve", y[:, :], y[:, :], float(np.pi), ALU.min, ["y2"], ["y3"], s2=float(-np.pi), op1=ALU.max)
                act(S, ang[:, :], y[:, :], AF.Sin, ["y3"], ["sv"])
                if which == 0:
                    ts(S, "dve", y[:, :], ang[:, :], cc[:, 1:2], ALU.mult, ["sv", "cc"], ["yo"])
                    dma(S, "sp", sin_s[:, tok], y[:, :], ["yo"], [("sin_s", t)])
                else:
                    dma(S, "sp", cos_s[:, tok], ang[:, :], ["sv"], [("cos_s", t)])
    S.barrier()


def bcast_row(S, C, dst, src_row, n, ones_row, psr, tag):
    st = C.sb("bst_" + tag, [1, n])
    dma(S, "sp", st[:, :], src_row, [], ["bst_" + tag])
    pt, pres = psr.next()
    mm(S, pt[:, :n], ones_row[0:1, :], st[0:1, :], True, True, ["ones_row", "bst_" + tag], [pres])
    cp(S, "act", dst, pt[:, :n], [pres], ["bc_" + tag])


def phase_A(S, nc, l, xin, P, K, sc, ntiles=None, do=("mla", "sg", "gla", "dn")):
    TA = 256
    NCH = TA // 64
    NSUB = TA // 128
    if ntiles is None:
        ntiles = SEQ // TA
    with ExitStack() as es:
        C = Ctx(nc, S, es, "A_")
        sb = C.sb
        ones_bf = sb("ones", [128, 128], BF16)
        ones_row = sb("ones_row", [1, 128])
        ident = sb("ident", [128, 128])
        memset(S, "pool", ones_bf[:, :], 1.0, ["ones"])
        memset(S, "pool", ones_row[:, :], 1.0, ["ones_row"])
        dma(S, "sp", ident[:, :], K["ident"], [], ["ident"])
        Win = sb("Win", [128, 8, 2744], BF16)
        win_v = P["w_in"][l].rearrange("(c p) n -> p c n", p=128)
        for cb in range(0, 2744, 512):
            ce = min(cb + 512, 2744)
            dma(S, "pool", Win[:, :, cb:ce], win_v[:, :, cb:ce], [], ["Win"])
        gmix = sb("gmix", [128, 8])
        dma(S, "sp", gmix[:, :], P["norm_mix"][l].rearrange("(c p) -> p c", p=128), [], ["gmix"], slow=True)
        xt_rot = C.rot("xt", [128, 8, TA], F32, 1)
        sq_rot = C.rot("sq", [128, TA], BF16, 2)
        hT = sb("hT", [128, 8, TA], BF16)
        tmp = sb("tmp", [128, TA])
        rstd = sb("rstd", [128, TA])
        psr = C.rot("ps", [128, 512], F32, 8, psum=True)
        xin_v = xin.rearrange("(c p) n -> p c n", p=128)
        hv = sc["hT_s"].rearrange("(c p) n -> p c n", p=128)
        hres = [("A_hT", k) for k in range(8)]

        def fproj(pt, pres, c0, n, rows=None):
            for k in range(8):
                mm(S, pt[:n, :TA], Win[:, k, c0:c0 + n], hT[:, k, :], k == 0, k == 7, ["Win", hres[k]], [pres])

        def tproj(pt, pres, s, c0, n, o0=0):
            for k in range(8):
                mm(S, pt[:, o0:o0 + n], hT[:, k, s * 128:(s + 1) * 128], Win[:, k, c0:c0 + n], k == 0, k == 7, ["Win", hres[k]], [pres])

        if "mla" in do:
            gq = sb("gq", [128, 2])
            gkv = sb("gkv", [128, 1])
            dma(S, "sp", gq[:, :], P["mla_norm_q"][l].rearrange("(c p) -> p c", p=128), [], ["gq"], slow=True)
            dma(S, "sp", gkv[:, :], P["mla_norm_kv"][l].rearrange("(c p) -> p c", p=128), [], ["gkv"], slow=True)
            Wkr = sb("Wkr", [128, 8, 96], BF16)
            Wkt = sb("Wkt", [128, 8, 96], BF16)
            memset(S, "pool", Wkr[:, :, :], 0.0, ["Wkr"])
            memset(S, "pool", Wkt[:, :, :], 0.0, ["Wkt"])
            dma(S, "pool", Wkr[:, :, 64:96], win_v[:, :, 384:416], [], ["Wkr"])
            dma(S, "pool", Wkt[:, :, 64:80], win_v[:, :, 400:416], [], ["Wkt"])
            dma(S, "pool", Wkt[:, :, 80:96], win_v[:, :, 384:400], [], ["Wkt"])
            Wuq = sb("Wuq", [128, 2, 384], BF16)
            dma(S, "pool", Wuq[:, :, :], P["mla_w_uq"][l].rearrange("(c p) n -> p c n", p=128), [], ["Wuq"])
            Wuqt = sb("Wuqt", [128, 2, 4, 96], BF16)
            memset(S, "pool", Wuqt[:, :, :, :], 0.0, ["Wuqt"])
            uqv = P["mla_w_uq"][l].rearrange("(c p) (h e) -> p c h e", p=128, e=96)
            for c in range(2):
                dma(S, "pool", Wuqt[:, c, :, 64:80], uqv[:, c, :, 80:96], [], ["Wuqt"])
                dma(S, "pool", Wuqt[:, c, :, 80:96], uqv[:, c, :, 64:80], [], ["Wuqt"])
            Wkn = sb("Wkn", [128, 4, 96], BF16)
            memset(S, "pool", Wkn[:, :, :], 0.0, ["Wkn"])
            ukv = P["mla_w_ukv"][l].rearrange("k (h e) -> k h e", e=128)
            dma(S, "pool", Wkn[:, :, 0:64], ukv[:, :, 0:64], [], ["Wkn"])
            Wv = sb("Wv", [128, 4, 64], BF16)
            dma(S, "pool", Wv[:, :, :], ukv[:, :, 64:128], [], ["Wv"])
            cqs = sb("cqs", [128, 2, TA])
            cqn = sb("cqn", [128, 2, TA], BF16)
            ckvs = sb("ckvs", [128, 1, TA])
            ckvn = sb("ckvn", [128, 1, TA], BF16)
            cosT = sb("cosT", [96, TA])
            sinT = sb("sinT", [96, TA])
            tmpK = sb("tmpK", [96, TA])
            t1_rot = C.rot("t1", [96, TA], F32, 1)
            t2_rot = C.rot("t2", [96, TA], F32, 1)
            qk_rot = C.rot("qk", [96, TA], BF16, 2)
            vt = sb("vt", [128, NSUB, 4, 65], BF16)
            memset(S, "pool", vt[:, :, :, :], 1.0, ["vt"])
        if "sg" in do:
            WmT = sb("WmT", [128, 4, 128])
            sg_t = sb("sg_t", [128, 512])
            sgw = sg_t[:, :].rearrange("p (g j) -> p g j", g=4)
            sgmask = sb("sgmask", [128, 128])
            dma(S, "sp", sgmask[:, :], K["trilT"], [], ["sgmask"])
            dma(S, "sp", sgw, P["sg_w"][l].rearrange("g i j -> i g j"), [], ["sg_t"])
            for g in range(4):
                pt, pres = psr.next()
                tr(S, pt[:, :128], sgw[:, g, :], ident[:, :], ["sg_t", "ident"], [pres])
                tt(S, "dve", WmT[:, g, :], pt[:, :128], sgmask[:, :], ALU.mult, [pres, "sgmask"], ["WmT"])
            sgb = sb("sgb", [128, 4])
            dma(S, "sp", sgb[:, :], P["sg_b"][l].rearrange("g i -> i g"), [], ["sgb"], slow=True)
            lng = sb("lng", [128, 256])
            lnb = sb("lnb", [128, 256])
            bcast_row(S, C, lng[:, :], P["sg_ln_g"][l:l + 1, :], 256, ones_row, psr, "lng")
            bcast_row(S, C, lnb[:, :], P["sg_ln_b"][l:l + 1, :], 256, ones_row, psr, "lnb")
            sg_i = sb("sg_i", [128, 512])
            sg_g = sb("sg_g", [128, 512])
            sg_s = sb("sg_s", [128, 8])
            sg_vc = sb("sg_vc", [128, 256])
            sg_junk = sb("sg_junk", [128, 256])
            sg_vn = sb("sg_vn", [128, 256])
            sg_o = sb("sg_o", [128, 256])
            obT = sb("obT", [128, 2, TA], BF16)
        if "gla" in do:
            wgate = sb("wgate", [16, 128])
            dma(S, "sp", wgate[:, :], P["gla_w_gate"][l], [], ["wgate"])
            nbg = sb("nbg", [128, 1])
            dma(S, "sp", nbg[:, :], P["gla_b_gate"][l].rearrange("(p o) -> p o", o=1), [], ["nbg0"], slow=True)
            ts(S, "dve", nbg[:, :], nbg[:, :], -1.0, ALU.mult, ["nbg0"], ["nbg"])
            rm = sb("rm", [128, TA])
            dma(S, "sp", rm[:, :], K["rm"][:, 0:TA], [], ["rm"])
            mg = sb("mg", [128, 128])
            dma(S, "sp", mg[:, :], K["mg"], [], ["mg"])
            hm = sb("hm", [128, 256])
            dma(S, "sp", hm[:, :], K["hm"], [], ["hm"])
            hcol = sb("hcol", [128, 4])
            dma(S, "sp", hcol[:, :], K["hcol"], [], ["hcol"])
            topbot = sb("topbot", [128, 2])
            dma(S, "sp", topbot[:, :], K["topbot"], [], ["topbot"])
            cmA = sb("cmA", [128, TA])
            dma(S, "sp", cmA[:, :], K["cmA"][:, 0:TA], [], ["cmA"])
            gnb = sb("gnb", [128, 256])
            for h in range(4):
                bcast_row(S, C, gnb[:, h * 64:(h + 1) * 64], P["gla_norm"][l:l + 1, :], 64, ones_row, psr, f"gn{h}")
            glr = sb("glr", [16, TA])
            g_l = sb("g_l", [128, TA])
            g_cum = sb("g_cum", [128, TA])
            g_ec = sb("g_ec", [128, TA])
            g_en = sb("g_en", [128, TA])
            g_qe = sb("g_qe", [128, TA])
            g_qA = sb("g_qA", [128, TA])
            g_qB = sb("g_qB", [128, TA])
            g_ke = sb("g_ke", [128, TA])
            g_kl = sb("g_kl", [128, TA])
            g_qp = sb("g_qp", [128, 4, TA])
            g_klA = sb("g_klA", [128, 128])
            g_klB = sb("g_klB", [128, 128])
            g_v = sb("g_v", [128, 256])
            g_og = sb("g_og", [128, 256])
            g_sc = C.rot("g_sc", [128, 128], F32, 4)
            g_S = sb("g_S", [128, 256])
            g_tmp = sb("g_tmp", [128, 256])
            g_ss = sb("g_ss", [128, 8])
            g_junk = sb("g_junk", [128, 64])
            g_o = sb("g_o", [128, 256])
            ocT = sb("ocT", [128, 2, TA], BF16)
            memset(S, "pool", g_S[:, :], 0.0, ["g_S"])
        if "dn" in do:
            if "gla" not in do:
                rm = sb("rm", [128, TA])
                dma(S, "sp", rm[:, :], K["rm"][:, 0:TA], [], ["rm"])
                topbot = sb("topbot", [128, 2])
                dma(S, "sp", topbot[:, :], K["topbot"], [], ["topbot"])
                cmA = sb("cmA", [128, TA])
                dma(S, "sp", cmA[:, :], K["cmA"][:, 0:TA], [], ["cmA"])
            ones64 = ones_bf
            cU = sb("cU", [128, 128]); dma(S, "sp", cU[:, :], K["U"], [], ["cU"])
            cL = sb("cL", [128, 128]); dma(S, "sp", cL[:, :], K["L"], [], ["cL"])
            cst = sb("cst", [128, 128]); dma(S, "sp", cst[:, :], K["strict"], [], ["cst"])
            sel4 = sb("sel4", [4, 4, 128]); dma(S, "sp", sel4[:, :, :], K["sel4"], [], ["sel4"])
            zrow = sb("zrow", [1, 256]); memset(S, "pool", zrow[:, :], 0.0, ["zrow"])
            cw = sb("cw", [64, 12, 4])
            for kk in range(4):
                dma(S, "sp", cw[:, :, kk], P["dn_conv"][l, kk].rearrange("(i p) -> p i", p=64), [], ["cw"], slow=True)
            alog = sb("alog", [4, 1]); dma(S, "sp", alog[:, :], P["dn_a_log"][l].rearrange("(p o) -> p o", o=1), [], ["alog0"], slow=True)
            dtb = sb("dtb", [4, 1]); dma(S, "sp", dtb[:, :], P["dn_dt_bias"][l].rearrange("(p o) -> p o", o=1), [], ["dtb"], slow=True)
            negA = sb("negA", [4, 1])
            act(S, negA[:, :], alog[:, :], AF.Exp, ["alog0"], ["negA0"])
            ts(S, "dve", negA[:, :], negA[:, :], -1.0, ALU.mult, ["negA0"], ["negA"])
            dnb = sb("dnb", [128, 256])
            for h in range(4):
                bcast_row(S, C, dnb[:, h * 64:(h + 1) * 64], P["dn_norm"][l:l + 1, :], 64, ones_row, psr, f"dnn{h}")
            xc = sb("xc", [64, 12, 3 + TA])
            memset(S, "pool", xc[:, :, 0:3], 0.0, [("xc", i) for i in range(12)])
            d_y = sb("d_y", [64, 12, TA])
            d_sq = C.rot("d_sq", [64, TA], BF16, 2)
            d_t = sb("d_t", [64, TA])
            d_r = sb("d_r", [64, TA])
            d_a = sb("d_a", [4, TA]); d_e = sb("d_e", [4, TA]); d_g = sb("d_g", [4, TA]); d_gam = sb("d_gam", [4, TA])
            d_b = sb("d_b", [4, TA]); d_eg = sb("d_eg", [4, TA]); d_bk = sb("d_bk", [4, TA]); d_lg = sb("d_lg", [4, TA])
            d_elb = sb("d_elb", [64, 4, NCH])
            d_gbc = sb("d_gbc", [128, 4, TA])
            d_kp = sb("d_kp", [64, 4, TA])
            d_qA = sb("d_qA", [64, 4, TA]); d_qB = sb("d_qB", [64, 4, TA])
            d_cols = sb("d_cols", [128, 12])
            d_Vp = sb("d_Vp", [128, 256])
            d_kdA = sb("d_kdA", [128, 4, 64]); d_kdB = sb("d_kdB", [128, 4, 64])
            d_Dm = C.rot("d_Dm", [128, 128], F32, 2)
            d_d1 = C.rot("d_d1", [128, 128], F32, 2)
            d_at = sb("d_at", [128, 4, 128])
            d_BpT = sb("d_BpT", [128, 2, 4, 128])
            d_BtT = sb("d_BtT", [128, 2, 4, 128])
            d_RT = sb("d_RT", [128, 4, 128])

            class _V:
                def __init__(self, t, h):
                    self.t, self.h = t, h

                def __getitem__(self, idx):
                    return self.t[idx[0], idx[1], self.h, idx[2]]

            class _VR:
                def __init__(self, t, h):
                    self.t, self.h = t, h

                def __getitem__(self, idx):
                    return self.t[idx[0], self.h, idx[1]]
            d_Bp = [_V(d_BpT, h) for h in range(4)]
            d_Bt = [_V(d_BtT, h) for h in range(4)]
            d_R = [_VR(d_RT, h) for h in range(4)]
            d_rhs2 = sb("d_rhs2", [128, 256])
            d_u = sb("d_u", [128, 256])
            d_S = sb("d_S", [64, 4, 64])
            memset(S, "pool", d_S[:, :, :], 0.0, ["d_S"])
            memset(S, "pool", d_u[:, :], 0.0, ["d_u"])
            d_z = sb("d_z", [128, 256])
            d_ss = sb("d_ss", [128, 8])
            d_junk = sb("d_junk", [128, 64])
            d_junk2 = sb("d_junk2", [128, 64])
            d_o = sb("d_o", [128, 256])
            odT = sb("odT", [128, 2, TA], BF16)

        def post_norm_gate(o_ps, o_res, gate_sb, gate_res, ss, junk, o_out, tagp, oT_tile, s):
            for h in range(4):
                act(S, junk[:, :], o_ps[:, h * 64:(h + 1) * 64], AF.Square, [o_res], [tagp + "junk"], accum_out=ss[:, h:h + 1])
            S.stream
            act(S, ss[:, 4:8], ss[:, 0:4], AF.Sqrt, [tagp + "junk"], [tagp + "ss1"], scale=1.0 / 64, bias=EPS)
            S.add("dve", lambda e: e.reciprocal(out=ss[:, 0:4], in_=ss[:, 4:8]), reads=[tagp + "ss1"], writes=[tagp + "ss2"])
            for h in range(4):
                stt(S, o_out[:, h * 64:(h + 1) * 64], o_ps[:, h * 64:(h + 1) * 64], ss[:, h:h + 1], gate_sb[:, h * 64:(h + 1) * 64],
                    ALU.mult, ALU.mult, [o_res, tagp + "ss2", gate_res], [tagp + "oo"])
            for c in range(2):
                pt, pres = psr.next()
                tr(S, pt[:, :128], o_out[:, c * 128:(c + 1) * 128], ident[:, :], [tagp + "oo", "ident"], [pres])
                cp(S, "act", oT_tile[:, c, s * 128:(s + 1) * 128], pt[:, :128], [pres], [tagp + "oT"])

        for t in range(ntiles):
            tok = slice(t * TA, (t + 1) * TA)
            xt, x0 = xt_rot.next()
            xres = [(x0, d) for d in range(8)]
            dma(S, "sp", xt[:, :, :], xin_v[:, :, tok], [], xres)
            rms_fm(S, xt, xres, 8, D, gmix, ones_bf, sq_rot, psr, tmp, "A_tmp", rstd, "A_rstd", hT, hres, TA)
            dma(S, "sp", hv[:, :, tok], hT[:, :, :], hres, [("hT_s", t)])
            if "mla" in do:
                dma(S, "sp", cosT[:, :], sc["cos_s"][:, tok], [], ["cosT"])
                dma(S, "sp", sinT[:, :], sc["sin_s"][:, tok], [], ["sinT"])
                for c in range(2):
                    pt, pres = psr.next()
                    fproj(pt, pres, c * 128, 128)
                    cp(S, "act", cqs[:, c, :], pt[:, :TA], [pres], [("cqs", c)])
                rms_fm(S, cqs, [("cqs", 0), ("cqs", 1)], 2, 256, gq, ones_bf, sq_rot, psr, tmp, "A_tmp", rstd, "A_rstd",
                       cqn, [("cqn", 0), ("cqn", 1)], TA)
                pt, pres = psr.next()
                fproj(pt, pres, 256, 128)
                cp(S, "act", ckvs[:, 0, :], pt[:, :TA], [pres], [("ckvs", 0)])
                rms_fm(S, ckvs, [("ckvs", 0)], 1, 128, gkv, ones_bf, sq_rot, psr, tmp, "A_tmp", rstd, "A_rstd",
                       ckvn, [("ckvn", 0)], TA)
                pt, pres = psr.next()
                for k in range(8):
                    mm(S, pt[:96, :TA], Wkt[:, k, :], hT[:, k, :], k == 0, k == 7, ["Wkt", hres[k]], [pres])
                tt(S, "dve", tmpK[:, :], pt[:96, :TA], sinT[:, :], ALU.mult, [pres, "sinT"], ["tmpK"])
                for h in range(4):
                    pt, pres = psr.next()
                    for c in range(2):
                        mm(S, pt[:96, :TA], Wuq[:, c, h * 96:(h + 1) * 96], cqn[:, c, :], c == 0, c == 1, ["Wuq", ("cqn", c)], [pres])
                    pt2, pres2 = psr.next()
                    for c in range(2):
                        mm(S, pt2[:96, :TA], Wuqt[:, c, h, :], cqn[:, c, :], c == 0, c == 1, ["Wuqt", ("cqn", c)], [pres2])
                    t1, t1r = t1_rot.next()
                    t2, t2r = t2_rot.next()
                    tt(S, "dve", t1[:, :], pt[:96, :TA], cosT[:, :], ALU.mult, [pres, "cosT"], [t1r])
                    tt(S, "dve", t2[:, :], pt2[:96, :TA], sinT[:, :], ALU.mult, [pres2, "sinT"], [t2r])
                    qk, qkr = qk_rot.next()
                    tt(S, "pool", qk[:, :], t1[:, :], t2[:, :], ALU.add, [t1r, t2r], [qkr])
                    dma(S, "sp", sc["qT_s"][h][:, tok], qk[:, :], [qkr], [("qT_s", h, t)])
                    pt, pres = psr.next()
                    for k in range(8):
                        mm(S, pt[:96, :TA], Wkr[:, k, :], hT[:, k, :], k == 0, False, ["Wkr", hres[k]], [pres])
                    mm(S, pt[:96, :TA], Wkn[:, h, :], ckvn[:, 0, :], False, True, ["Wkn", ("ckvn", 0)], [pres])
                    t1, t1r = t1_rot.next()
                    tt(S, "dve", t1[:, :], pt[:96, :TA], cosT[:, :], ALU.mult, [pres, "cosT"], [t1r])
                    qk, qkr = qk_rot.next()
                    tt(S, "pool", qk[:, :], t1[:, :], tmpK[:, :], ALU.add, [t1r, "tmpK"], [qkr])
                    dma(S, "sp", sc["kT_s"][h][:, tok], qk[:, :], [qkr], [("kT_s", h, t)])
                for s in range(NSUB):
                    pt, pres = psr.next()
                    mm(S, pt[:, :256], ckvn[:, 0, s * 128:(s + 1) * 128], Wv[:, :, :].rearrange("p h e -> p (h e)"), True, True,
                       ["Wv", ("ckvn", 0)], [pres])
                    cp(S, "act", vt[:, s, :, 0:64], pt[:, :256].rearrange("p (h e) -> p h e", e=64), [pres], ["vt"])
                dma(S, "sp", sc["v_s"][tok, :].rearrange("(s p) n -> p s n", p=128), vt[:, :, :, :].rearrange("p s h e -> p s (h e)"),
                    ["vt"], [("v_s", t)])
            if "gla" in do:
                pt, pres = psr.next()
                fproj(pt, pres, 1440, 16)
                cp(S, "act", glr[:, :], pt[:16, :TA], [pres], ["glr"])
                pt, pres = psr.next()
                mm(S, pt[:, :TA], wgate[:, :], glr[:, :], True, True, ["wgate", "glr"], [pres])
                act(S, g_l[:, :], pt[:, :TA], AF.Exp, [pres], ["g_l0"], scale=-1.0, bias=nbg[:, 0:1])
                act(S, g_l[:, :], g_l[:, :], AF.Ln, ["g_l0", "nbg"], ["g_l"], bias=1.0)
                S.add("dve", lambda e: e.tensor_tensor_scan(out=g_cum[:, :], data0=rm[:, :], data1=g_l[:, :], initial=0.0,
                                                            op0=ALU.mult, op1=ALU.add), reads=["rm", "g_l"], writes=["g_cum"])
                act(S, g_ec[:, :], g_cum[:, :], AF.Exp, ["g_cum"], ["g_ec"], scale=-1.0 / 16)
                act(S, g_en[:, :], g_cum[:, :], AF.Exp, ["g_cum"], ["g_en"], scale=1.0 / 16)
                pt, pres = psr.next()
                fproj(pt, pres, 928, 128)
                stt(S, g_qe[:, :], pt[:, :TA], float(32 ** -0.5), g_ec[:, :], ALU.mult, ALU.mult, [pres, "g_ec"], ["g_qe"])
                pt, pres = psr.next()
                fproj(pt, pres, 1056, 128)
                tt(S, "dve", g_ke[:, :], pt[:, :TA], g_en[:, :], ALU.mult, [pres, "g_en"], ["g_ke"])
                for c in range(NCH):
                    ts(S, "dve", g_kl[:, c * 64:(c + 1) * 64], g_ke[:, c * 64:(c + 1) * 64], g_ec[:, c * 64 + 63:c * 64 + 64], ALU.mult,
                       ["g_ke", "g_ec"], [("g_kl", c // 2)])
                for h in range(4):
                    ts(S, "pool", g_qp[:, h, :], g_qe[:, :], hcol[:, h:h + 1], ALU.mult, ["g_qe", "hcol"], [("g_qp", h)])
                tt(S, "pool", g_qA[:, :], g_qe[:, :], cmA[:, :], ALU.mult, ["g_qe", "cmA"], ["g_qA"])
                tt(S, "pool", g_qB[:, :], g_qe[:, :], g_qA[:, :], ALU.subtract, ["g_qe", "g_qA"], ["g_qB"])
            if "dn" in do:
                for i in range(12):
                    pt, pres = psr.next()
                    fproj(pt, pres, 1712 + i * 64, 64)
                    cp(S, "act", xc[:, i, 3:3 + TA], pt[:64, :TA], [pres], [("xc", i)])
                    yi = d_y[:, i, :]
                    ts(S, "dve", yi, xc[:, i, 3:3 + TA], cw[:, i, 3:4], ALU.mult, [("xc", i), "cw"], [("d_y", i)])
                    for kk in range(3):
                        stt(S, yi, xc[:, i, kk:kk + TA], cw[:, i, kk:kk + 1], yi, ALU.mult, ALU.add, [("xc", i), ("d_y", i), "cw"], [("d_y", i)])
                    cp(S, "pool", xc[:, i, 0:3], xc[:, i, TA:TA + 3], [("xc", i)], [("xc", i)])
                    act(S, yi, yi, AF.Silu, [("d_y", i)], [("d_y", i)])
                    if i < 8:
                        sq, sqr = d_sq.next()
                        act(S, sq[:, :], yi, AF.Square, [("d_y", i)], [sqr])
                        pt, pres = psr.next()
                        mm(S, pt[:64, :TA], ones64[:64, :64], sq[:, :], True, True, ["ones", sqr], [pres])
                        act(S, d_t[:, :], pt[:64, :TA], AF.Sqrt, [pres], ["d_t"], bias=EPS)
                        S.add("dve", lambda e: e.reciprocal(out=d_r[:, :], in_=d_t[:, :]), reads=["d_t"], writes=["d_r"])
                        stt(S, yi, yi, 0.125 if i < 4 else 1.0, d_r[:, :], ALU.mult, ALU.mult, [("d_y", i), "d_r"], [("d_y", i)])
                pt, pres = psr.next()
                fproj(pt, pres, 2480, 4)
                act(S, d_e[:, :], pt[:4, :TA], AF.Exp, [pres, "dtb"], ["d_e0"], bias=dtb[:, 0:1])
                act(S, d_e[:, :], d_e[:, :], AF.Ln, ["d_e0"], ["d_e"], bias=1.0)
                ts(S, "dve", d_g[:, :], d_e[:, :], negA[:, 0:1], ALU.mult, ["d_e", "negA"], ["d_g"])
                S.add("dve", lambda e: e.tensor_tensor_scan(out=d_gam[:, :], data0=rm[0:4, :], data1=d_g[:, :], initial=0.0,
                                                            op0=ALU.mult, op1=ALU.add), reads=["rm", "d_g"], writes=["d_gam"])
                pt, pres = psr.next()
                fproj(pt, pres, 2484, 4)
                act(S, d_b[:, :], pt[:4, :TA], AF.Sigmoid, [pres], ["d_b"])
                act(S, d_eg[:, :], d_gam[:, :], AF.Exp, ["d_gam"], ["d_eg"])
                tt(S, "dve", d_bk[:, :], d_b[:, :], d_eg[:, :], ALU.mult, ["d_b", "d_eg"], ["d_bk"])
                for c in range(NCH):
                    ts(S, "dve", d_lg[:, c * 64:(c + 1) * 64], d_gam[:, c * 64:(c + 1) * 64], d_gam[:, c * 64 + 63:c * 64 + 64], ALU.subtract,
                       ["d_gam"], ["d_lg0"])
                act(S, d_lg[:, :], d_lg[:, :], AF.Exp, ["d_lg0"], ["d_lg"], scale=-1.0)
                pt, pres = psr.next()
                for h in range(4):
                    mm(S, pt[:64, h * NCH:(h + 1) * NCH], sel4[:, h, 0:64], d_eg[:, 63:TA:64], True, True, ["sel4", "d_eg"], [pres])
                cp(S, "act", d_elb[:, :, :], pt[:64, 0:4 * NCH].rearrange("p (h c) -> p h c", c=NCH), [pres], ["d_elb"])
                for h in range(4):
                    pt, pres = psr.next()
                    mm(S, pt[:, :TA], sel4[:, h, :], d_gam[:, :], True, True, ["sel4", "d_gam"], [pres])
                    cp(S, "act", d_gbc[:, h, :], pt[:, :TA], [pres], [("d_gbc", h)])
                    pt, pres = psr.next()
                    mm(S, pt[:64, :TA], sel4[:, h, 0:64], d_bk[:, :], True, True, ["sel4", "d_bk"], [pres])
                    tt(S, "dve", d_kp[:, h, :], pt[:64, :TA], d_y[:, 4 + h, :], ALU.mult, [pres, ("d_y", 4 + h)], [("d_kp", h)])
                    pt, pres = psr.next()
                    mm(S, pt[:64, :TA], sel4[:, h, 0:64], d_eg[:, :], True, True, ["sel4", "d_eg"], [pres])
                    tt(S, "dve", d_qB[:, h, :], pt[:64, :TA], d_y[:, h, :], ALU.mult, [pres, ("d_y", h)], [("d_qB0", h)])
                    tt(S, "pool", d_qA[:, h, :], d_qB[:, h, :], cmA[:64, :], ALU.mult, [("d_qB0", h), "cmA"], [("d_qA", h)])
                    tt(S, "pool", d_qB[:, h, :], d_qB[:, h, :], d_qA[:, h, :], ALU.subtract, [("d_qB0", h), ("d_qA", h)], [("d_qB", h)])
            for s in range(NSUB):
                ssl = slice(s * 128, (s + 1) * 128)
                if "sg" in do:
                    pu, pures = psr.next()
                    tproj(pu, pures, s, 416, 512)
                    act(S, sg_t[:, :], pu[:, :], AF.Square, [pures], ["sg_t"])
                    ts(S, "dve", sg_i[:, :], sg_t[:, :], 0.044715, ALU.mult, ["sg_t"], ["sg_i0"], s2=1.0, op1=ALU.add)
                    tt(S, "dve", sg_i[:, :], sg_i[:, :], pu[:, :], ALU.mult, ["sg_i0", pures], ["sg_i"])
                    act(S, sg_t[:, :], sg_i[:, :], AF.Sigmoid, ["sg_i"], ["sg_t2"], scale=1.5957691216057308)
                    tt(S, "dve", sg_g[:, :], sg_t[:, :], pu[:, :], ALU.mult, ["sg_t2", pures], ["sg_g"])
                    S.add("dve", lambda e: e.reduce_sum(out=sg_s[:, 0:1], in_=sg_g[:, 256:512], axis=mybir.AxisListType.X),
                          reads=["sg_g"], writes=["sg_s0"])
                    ts(S, "dve", sg_s[:, 1:2], sg_s[:, 0:1], -1.0 / 256, ALU.mult, ["sg_s0"], ["sg_s1"])
                    ts(S, "dve", sg_vc[:, :], sg_g[:, 256:512], sg_s[:, 1:2], ALU.add, ["sg_g", "sg_s1"], ["sg_vc"])
                    act(S, sg_junk[:, :], sg_vc[:, :], AF.Square, ["sg_vc"], ["sg_junk"], accum_out=sg_s[:, 2:3])
                    act(S, sg_s[:, 3:4], sg_s[:, 2:3], AF.Sqrt, ["sg_junk"], ["sg_s3"], scale=1.0 / 256, bias=EPS)
                    S.add("dve", lambda e: e.reciprocal(out=sg_s[:, 4:5], in_=sg_s[:, 3:4]), reads=["sg_s3"], writes=["sg_s4"])
                    stt(S, sg_vn[:, :], sg_vc[:, :], sg_s[:, 4:5], lng[:, :], ALU.mult, ALU.mult, ["sg_vc", "sg_s4", "bc_lng"], ["sg_vn0"])
                    tt(S, "dve", sg_vn[:, :], sg_vn[:, :], lnb[:, :], ALU.add, ["sg_vn0", "bc_lnb"], ["sg_vn"])
                    pt, pres = psr.next()
                    for g in range(4):
                        mm(S, pt[:, g * 64:(g + 1) * 64], WmT[:, g, :], sg_vn[:, g * 64:(g + 1) * 64], True, True, ["WmT", "sg_vn"], [pres])
                    for g in range(4):
                        stt(S, sg_o[:, g * 64:(g + 1) * 64], pt[:, g * 64:(g + 1) * 64], sgb[:, g:g + 1], sg_g[:, g * 64:(g + 1) * 64],
                            ALU.add, ALU.mult, [pres, "sgb", "sg_g"], ["sg_o"])
                    for c in range(2):
                        pt, pres = psr.next()
                        tr(S, pt[:, :128], sg_o[:, c * 128:(c + 1) * 128], ident[:, :], ["sg_o", "ident"], [pres])
                        cp(S, "act", obT[:, c, ssl], pt[:, :128], [pres], ["obT"])
                if "gla" in do:
                    pv, pvres = psr.next()
                    tproj(pv, pvres, s, 1184, 256, 0)
                    tproj(pv, pvres, s, 1456, 256, 256)
                    cp(S, "act", g_v[:, :], pv[:, 0:256], [pvres], ["g_v"])
                    act(S, g_og[:, :], pv[:, 256:512], AF.Silu, [pvres], ["g_og0"])
                    tt(S, "pool", g_og[:, :], g_og[:, :], gnb[:, :], ALU.mult, ["g_og0"] + [f"bc_gn{h}" for h in range(4)], ["g_og"])
                    pt, pres = psr.next()
                    tr(S, pt[:, :128], g_kl[:, ssl], ident[:, :], [("g_kl", s), "ident"], [pres])
                    ts(S, "dve", g_klA[:, :], pt[:, :128], topbot[:, 0:1], ALU.mult, [pres, "topbot"], ["g_klA"])
                    ts(S, "dve", g_klB[:, :], pt[:, :128], topbot[:, 1:2], ALU.mult, [pres, "topbot"], ["g_klB"])
                    scs = []
                    for h in range(4):
                        pt, pres = psr.next()
                        mm(S, pt[:, :128], g_ke[:, ssl], g_qp[:, h, ssl], True, True, ["g_ke", ("g_qp", h)], [pres])
                        sct, scr = g_sc.next()
                        tt(S, "dve", sct[:, :], pt[:, :128], mg[:, :], ALU.mult, [pres, "mg"], [scr])
                        scs.append((sct, scr))
                    po, pores = psr.next()
                    mm(S, po[:, :256], g_qA[:, ssl], g_S[:, :], True, False, ["g_qA", "g_S"], [pores])
                    for (klt, klr, isB) in ((g_klA, "g_klA", False), (g_klB, "g_klB", True)):
                        if isB:
                            mm(S, po[:, :256], g_qB[:, ssl], g_S[:, :], False, False, ["g_qB", "g_S"], [pores])
                        pt, pres = psr.next()
                        mm(S, pt[:, :256], klt[:, :], g_v[:, :], True, True, [klr, "g_v"], [pres])
                        tt(S, "dve", g_tmp[:, :], pt[:, :256], hm[:, :], ALU.mult, [pres, "hm"], ["g_tmp"])
                        cidx = s * 2 + (1 if isB else 0)
                        stt(S, g_S[:, :], g_S[:, :], g_ec[:, cidx * 64 + 63:cidx * 64 + 64], g_tmp[:, :], ALU.mult, ALU.add,
                            ["g_S", "g_ec", "g_tmp"], ["g_S"])
                    for h in range(4):
                        mm(S, po[:, h * 64:(h + 1) * 64], scs[h][0][:, :], g_v[:, h * 64:(h + 1) * 64], False, h == 3, [scs[h][1], "g_v"], [pores])
                    post_norm_gate(po, pores, g_og, "g_og", g_ss, g_junk, g_o, "g_", ocT, s)
                if "dn" in do and DNSTAGE >= 1:
                    pz, pzres = psr.next()
                    tproj(pz, pzres, s, 2488, 256, 0)
                    act(S, d_z[:, :], pz[:, 0:256], AF.Silu, [pzres], ["d_z0"])
                    tt(S, "pool", d_z[:, :], d_z[:, :], dnb[:, :], ALU.mult, ["d_z0"] + [f"bc_dnn{h}" for h in range(4)], ["d_z"])
                    pt, pres = psr.next()
                    mm(S, pt[:, 0:4], d_gam[:, ssl], ident[0:4, 0:4], True, True, ["d_gam", "ident"], [pres])
                    mm(S, pt[:, 4:8], d_b[:, ssl], ident[0:4, 0:4], True, True, ["d_b", "ident"], [pres])
                    mm(S, pt[:, 8:12], d_lg[:, ssl], ident[0:4, 0:4], True, True, ["d_lg", "ident"], [pres])
                    cp(S, "act", d_cols[:, :], pt[:, 0:12], [pres], ["d_cols"])
                    for h in range(4 if DNSTAGE >= 1.2 else 0):
                        kTh = d_y[:, 4 + h, ssl]
                        qTh = d_y[:, h, ssl]
                        vTh = d_y[:, 8 + h, ssl]
                        pt, pres = psr.next()
                        mm(S, pt[:, 0:64], kTh, ident[0:64, 0:64], True, True, [("d_y", 4 + h), "ident"], [pres])
                        mm(S, pt[:, 64:128], vTh, ident[0:64, 0:64], True, True, [("d_y", 8 + h), "ident"], [pres])
                        ts(S, "dve", d_Vp[:, h * 64:(h + 1) * 64], pt[:, 64:128], d_cols[:, 4 + h:5 + h], ALU.mult, [pres, "d_cols"], [("d_Vp", h)])
                        ts(S, "dve", d_junk[:, :], pt[:, 0:64], d_cols[:, 8 + h:9 + h], ALU.mult, [pres, "d_cols"], ["d_kdf"])
                        ts(S, "pool", d_kdA[:, h, :], d_junk[:, :], topbot[:, 0:1], ALU.mult, ["d_kdf", "topbot"], [("d_kdA", h)])
                        ts(S, "pool", d_kdB[:, h, :], d_junk[:, :], topbot[:, 1:2], ALU.mult, ["d_kdf", "topbot"], [("d_kdB", h)])
                        if DNSTAGE < 1.3:
                            continue
                        pkk, pkkres = psr.next()
                        mm(S, pkk[:, 0:128], kTh, kTh, True, True, [("d_y", 4 + h)], [pkkres])
                        mm(S, pkk[:, 128:256], kTh, qTh, True, True, [("d_y", 4 + h), ("d_y", h)], [pkkres])
                        d1, d1r = d_d1.next()
                        stt(S, d1[:, :], d_gbc[:, h, ssl], d_cols[:, h:h + 1], cU[:, :], ALU.subtract, ALU.max, [("d_gbc", h), "d_cols", "cU"], [d1r])
                        Dm, Dmr = d_Dm.next()
                        act(S, Dm[:, :], d1[:, :], AF.Exp, [d1r], [Dmr], scale=-1.0)
                        Bp, Bt, Rr = d_Bp[h], d_Bt[h], d_R[h]
                        stt(S, Bp[:, 0, :], pkk[:, 0:128], d_cols[:, 4 + h:5 + h], Dm[:, :], ALU.mult, ALU.mult, [pkkres, "d_cols", Dmr], [("Bp", h, 0)])
                        tt(S, "pool", Bp[:, 0, :], Bp[:, 0, :], cst[:, :], ALU.mult, [("Bp", h, 0), "cst"], [("Bp", h, 0)])
                        d2, d2r = d_d1.next()
                        stt(S, d2[:, :], d_gbc[:, h, ssl], d_cols[:, h:h + 1], cL[:, :], ALU.subtract, ALU.min, [("d_gbc", h), "d_cols", "cL"], [d2r])
                        DmT, DmTr = d_Dm.next()
                        act(S, DmT[:, :], d2[:, :], AF.Exp, [d2r], [DmTr])
                        tt(S, "dve", d_at[:, h, :], pkk[:, 128:256], DmT[:, :], ALU.mult, [pkkres, DmTr], [("d_at", h)])
                        if DNSTAGE < 1.4:
                            continue
                        pt, pres = psr.next()
                        mm(S, pt[:, :128], Bp[:, 0, :], ident[:, :], True, True, [("Bp", h, 0), "ident"], [pres])
                        if DNV != "a":
                            cp(S, "act", Bt[:, 0, :], pt[:, :128], [pres], [("Bt", h, 0)])
                        tt(S, "pool", Rr[:, :], ident[:, :], Bt[:, 0, :], ALU.subtract, [("Bt", h, 0), "ident"], [("R", h)])
                    for lev in range(1, 6 if DNSTAGE >= 2 else 1):
                        a, b = (lev - 1) % 2, lev % 2
                        ptA, ptAres = psr.next()
                        if lev < 5:
                            ptT, ptTres = psr.next()
                        for h in range(4):
                            mm(S, ptA[:, h * 128:(h + 1) * 128], d_Bt[h][:, a, :], d_Bp[h][:, a, :], True, True, [("Bt", h, a), ("Bp", h, a)], [ptAres])
                            if lev < 5:
                                mm(S, ptT[:, h * 128:(h + 1) * 128], d_Bp[h][:, a, :], d_Bt[h][:, a, :], True, True, [("Bt", h, a), ("Bp", h, a)], [ptTres])
                        cp(S, "act", d_BpT[:, b, :, :].rearrange("p h c -> p (h c)"), ptA[:, :512], [ptAres], [("Bp", h, b) for h in range(4)])
                        if lev < 5:
                            cp(S, "dve", d_BtT[:, b, :, :].rearrange("p h c -> p (h c)"), ptT[:, :512], [ptTres], [("Bt", h, b) for h in range(4)])
                        pt2, pres2 = psr.next()
                        for h in range(4):
                            mm(S, pt2[:, h * 128:(h + 1) * 128], d_Bp[h][:, b, :], d_R[h][:, :], True, True, [("Bp", h, b), ("R", h)], [pres2])
                        tt(S, "dve", d_RT[:, :, :].rearrange("p h c -> p (h c)"), d_RT[:, :, :].rearrange("p h c -> p (h c)"), pt2[:, :512], ALU.add,
                           [pres2] + [("R", h) for h in range(4)], [("R", h) for h in range(4)])
                    if DNSTAGE < 1.5:
                        continue
                    po, pores = psr.next()
                    mm(S, po[:, :256], zrow[0:1, 0:128], zrow[0:1, :], True, False, ["zrow"], [pores])
                    for ci, (kd, kdn, qd, qdn) in enumerate(((d_kdA, "d_kdA", d_qA, "d_qA"), (d_kdB, "d_kdB", d_qB, "d_qB")) if DNSTAGE >= 3 else ()):
                        rows = slice(ci * 64, (ci + 1) * 64)
                        pks, pksres = psr.next()
                        for h in range(4):
                            mm(S, pks[:, h * 64:(h + 1) * 64], d_kp[:, h, ssl], d_S[:, h, :], True, True, [("d_kp", h), "d_S"], [pksres])
                            mm(S, po[:, h * 64:(h + 1) * 64], qd[:, h, ssl], d_S[:, h, :], False, False, [(qdn, h), "d_S"], [pores])
                        tt(S, "dve", d_rhs2[:, :], d_Vp[:, :], pks[:, :256], ALU.subtract, [pksres] + [("d_Vp", h) for h in range(4)], ["d_rhs2"])
                        pu, pures = psr.next()
                        for h in range(4):
                            mm(S, pu[:, h * 64:(h + 1) * 64], d_R[h][:, :], d_rhs2[:, h * 64:(h + 1) * 64], True, True, [("R", h), "d_rhs2"], [pures])
                        cp(S, "act", d_u[rows, :], pu[rows, :256], [pures], ["d_u"])
                        psu, psures = psr.next()
                        for h in range(4):
                            mm(S, psu[:64, h * 64:(h + 1) * 64], kd[:, h, :], d_u[:, h * 64:(h + 1) * 64], True, True, [(kdn, h), "d_u"], [psures])
                        cidx = s * 2 + ci
                        for h in range(4):
                            stt(S, d_S[:, h, :], d_S[:, h, :], d_elb[:, h, cidx:cidx + 1], psu[:64, h * 64:(h + 1) * 64], ALU.mult, ALU.add,
                                ["d_S", "d_elb", psures], ["d_S"])
                    for h in range(4):
                        mm(S, po[:, h * 64:(h + 1) * 64], d_at[:, h, :], d_u[:, h * 64:(h + 1) * 64], False, h == 3, [("d_at", h), "d_u"], [pores])
                    post_norm_gate(po, pores, d_z, "d_z", d_ss, d_junk2, d_o, "d_", odT, s)
            if "sg" in do:
                dma(S, "sp", sc["oT_s"][0].rearrange("(c p) n -> p c n", p=128)[:, :, tok], obT[:, :, :], ["obT"], [("oT_s", 0, t)])
            if "gla" in do:
                dma(S, "sp", sc["oT_s"][1].rearrange("(c p) n -> p c n", p=128)[:, :, tok], ocT[:, :, :], ["g_oT"], [("oT_s", 1, t)])
            if "dn" in do:
                dma(S, "sp", sc["oT_s"][2].rearrange("(c p) n -> p c n", p=128)[:, :, tok], odT[:, :, :], ["d_oT"], [("oT_s", 2, t)])
    S.barrier()

PARAM_SHAPES = {
    "norm_mix": [NL, D], "w_in": [NL, D, INW], "mla_norm_q": [NL, 256], "mla_norm_kv": [NL, 128],
    "mla_w_uq": [NL, 256, 384], "mla_w_ukv": [NL, 128, 512], "sg_ln_g": [NL, 256], "sg_ln_b": [NL, 256],
    "sg_w": [NL, 4, 128, 128], "sg_b": [NL, 4, 128], "gla_w_gate": [NL, 16, 128], "gla_b_gate": [NL, 128],
    "gla_norm": [NL, 64], "dn_conv": [NL, 4, 768], "dn_a_log": [NL, 4], "dn_dt_bias": [NL, 4], "dn_norm": [NL, 64],
    "w_branch": [NL, 4, 256, D], "w_out": [NL, D, D], "norm_xattn": [NL, D], "norm_mem": [NL, D],
    "xattn_wq": [NL, D, D], "xattn_wk": [NL, D, D], "xattn_wv": [NL, D, D], "xattn_wo": [NL, D, D],
    "norm_mlp": [NL, D], "w_up": [NL, D, HID], "w_down": [NL, HID, D], "norm_final": [D],
}


def make_consts():
    p = np.arange(128)
    same = (p[:, None] // 64) == (p[None, :] // 64)
    c = {}
    c["ident"] = np.eye(128, dtype=np.float32)
    c["trilT"] = (p[:, None] <= p[None, :]).astype(np.float32)
    t = np.arange(TT)
    c["rm"] = np.broadcast_to((t % 64 != 0).astype(np.float32), (128, TT)).copy()
    c["mg"] = (same & (p[:, None] <= p[None, :])).astype(np.float32)
    c["hm"] = ((p[:, None] // 32) == (np.arange(256)[None, :] // 64)).astype(np.float32)
    c["hcol"] = ((p[:, None] // 32) == np.arange(4)[None, :]).astype(np.float32)
    c["topbot"] = np.stack([(p < 64), (p >= 64)], axis=1).astype(np.float32)
    c["cmA"] = np.broadcast_to(((t % 128) < 64).astype(np.float32), (128, TT)).copy()
    c["U"] = np.where(same & (p[None, :] <= p[:, None]), 0.0, 80.0).astype(np.float32)
    c["L"] = np.where(same & (p[None, :] >= p[:, None]), 0.0, -80.0).astype(np.float32)
    c["strict"] = (same & (p[None, :] < p[:, None])).astype(np.float32)
    sel = np.zeros((4, 4, 128), np.float32)
    for h in range(4):
        sel[h, h, :] = 1.0
    c["sel4"] = sel
    cc = np.zeros((96, 2), np.float32)
    f = (10000.0 ** (-np.arange(0, 32, 2, dtype=np.float32) / 32)).astype(np.float32)
    cc[64:80, 0] = f
    cc[80:96, 0] = f
    cc[64:80, 1] = -1.0
    cc[80:96, 1] = 1.0
    c["cconst"] = cc
    return c


CONST_SHAPES = {k: list(v.shape) for k, v in make_consts().items()}


def build(nl=NL, ntiles=NT, phases=("rope", "A", "mla", "merge", "xattn", "mlp", "final"), dbg=(), doA=("mla", "sg", "gla", "dn")):
    nc = bass.Bass("TRN2", target_bir_lowering=False)

    def dt(name, shape, dtype, kind):
        return nc.dram_tensor(name, shape, dtype, kind=kind).ap()

    xT = dt("xT", [D, SEQ], F32, "ExternalInput")
    memT = dt("memT", [D, 256], F32, "ExternalInput")
    posrep = dt("posrep", [96, SEQ], I32, "ExternalInput")
    P = {k: dt(k, s, F32, "ExternalInput") for k, s in PARAM_SHAPES.items()}
    K = {k: dt("c_" + k, s, F32, "ExternalInput") for k, s in CONST_SHAPES.items()}
    outT = dt("outT", [D, SEQ], F32, "ExternalOutput")

    def scr(name, shape, dtype):
        return dt(name, shape, dtype, "ExternalOutput" if name in dbg else "Internal")

    sc = {
        "hT_s": scr("hT_s", [D, SEQ], BF16), "qT_s": scr("qT_s", [4, 96, SEQ], BF16), "kT_s": scr("kT_s", [4, 96, SEQ], BF16),
        "v_s": scr("v_s", [SEQ, 260], BF16), "oaT_s": scr("oaT_s", [4, 64, SEQ], BF16), "oT_s": scr("oT_s", [3, 256, SEQ], BF16),
        "cos_s": scr("cos_s", [96, SEQ], F32), "sin_s": scr("sin_s", [96, SEQ], F32),
        "wup_bf": scr("wup_bf", [NL, D, HID], BF16), "wdn_bf": scr("wdn_bf", [NL, HID, D], BF16), "wg_bf": scr("wg_bf", [NL, D, 4096], BF16),
        "xa": scr("xa", [D, SEQ], F32), "xb": scr("xb", [D, SEQ], F32), "xc": scr("xc", [D, SEQ], F32),
    }
    S = Sched(nc)
    phase_precast(S, nc, P, sc)
    if "rope" in phases:
        phase_rope(S, nc, posrep, K["cconst"], sc["cos_s"], sc["sin_s"])
    cur = xT
    for l in range(nl):
        if "A" in phases:
            phase_A(S, nc, l, cur, P, K, sc, ntiles=(None if ntiles == NT else 2 * ntiles), do=doA)
        if "mla" in phases:
            phase_mla(S, nc, sc["qT_s"], sc["kT_s"], sc["v_s"], sc["oaT_s"], K["trilT"], nq=ntiles)
        if "merge" in phases:
            phase_merge(S, nc, l, cur, sc["xa"], sc["hT_s"], sc["oaT_s"], sc["oT_s"], sc["wg_bf"], P["w_branch"], P["w_out"], ntiles=ntiles)
            cur = sc["xa"]
        if "xattn" in phases:
            phase_xattn(S, nc, l, cur, sc["xb"], memT, P["norm_xattn"], P["norm_mem"], P["xattn_wq"], P["xattn_wk"], P["xattn_wv"], P["xattn_wo"], ntiles=ntiles)
            cur = sc["xb"]
        if "mlp" in phases:
            phase_mlp(S, nc, l, cur, sc["xc"], P["norm_mlp"], sc["wup_bf"], sc["wdn_bf"], ntiles=ntiles)
            cur = sc["xc"]
    if "final" in phases:
        phase_final(S, nc, cur, outT, P["norm_final"], ntiles=ntiles)
    S.emit()
    return nc


def make_in_maps(inputs, cores):
    consts = make_consts()
    x = np.asarray(inputs["x"], dtype=np.float32)
    mem = np.asarray(inputs["mem"], dtype=np.float32)
    pos = np.asarray(inputs["positions"]).astype(np.int32)
    shared = {k: np.ascontiguousarray(np.asarray(inputs[k], dtype=np.float32)) for k in PARAM_SHAPES}
    for k, v in consts.items():
        shared["c_" + k] = v
    in_maps = []
    for b in cores:
        m = dict(shared)
        m["xT"] = np.ascontiguousarray(x[b].T)
        m["memT"] = np.ascontiguousarray(mem[b].T)
        m["posrep"] = np.ascontiguousarray(np.broadcast_to(pos[b][None, :], (96, SEQ)))
        in_maps.append(m)
    return in_maps


def kernel(**inputs):
    B = np.asarray(inputs["x"]).shape[0]
    nc = build()
    in_maps = make_in_maps(inputs, list(range(B)))
    res = run_bass_kernel_spmd(nc, in_maps, core_ids=list(range(B)))
    out = np.stack([np.ascontiguousarray(np.asarray(r["outT"]).T) for r in res.results], axis=0)
    return out.astype(np.float32)
```

```python
from contextlib import ExitStack
import numpy as np
import concourse.bass as bass
import concourse.mybir as mybir
from concourse.bass_utils import run_bass_kernel_spmd

F32 = mybir.dt.float32
BF16 = mybir.dt.bfloat16
I32 = mybir.dt.int32
AF = mybir.ActivationFunctionType
ALU = mybir.AluOpType

D = 1024
SEQ = 4096
NL = 2
TT = 512
NT = SEQ // TT
EPS = 1e-6
HID = 4096
INW = 6840
DBG = 9
DNSTAGE = 9
DNV = ''


class Sched:
    ENGS = ("pe", "act", "dve", "pool", "sp")
    EPOCH = 12000
    RING = 8

    def __init__(self, nc):
        self.nc = nc
        self.stream = {e: [] for e in self.ENGS}
        self.vt = {e: 0 for e in self.ENGS}
        self.known = {e: {} for e in self.ENGS}
        self.need_sig = {e: set() for e in self.ENGS}
        self.last_write = {}
        self.readers = {}
        self.op_sig = []
        self.op_clock = []
        self.op_eng = []
        self.op_dma = []
        self.dma_n = {e: 0 for e in self.ENGS}
        self.last_sig = {}

    def _emit_wait(self, eng, key, val):
        self.stream[eng].append(("wait", key, val))
        if key[0] == "eng":
            self.need_sig[key[1]].add(val)

    def _wait(self, eng, dep):
        key, val = self.op_sig[dep]
        kn = self.known[eng]
        if kn.get(key, 0) >= val:
            return
        self._emit_wait(eng, key, val)
        for k, v in self.op_clock[dep].items():
            if kn.get(k, 0) < v:
                kn[k] = v
        kn[key] = max(kn.get(key, 0), val)

    def add(self, eng, fn, reads=(), writes=(), signal=True, dma=False):
        deps = set()
        for r in reads:
            w = self.last_write.get(r)
            if w is not None:
                deps.add(w)
        for w_ in writes:
            w = self.last_write.get(w_)
            if w is not None:
                deps.add(w)
            for r in self.readers.get(w_, ()):
                deps.add(r)
        opid = len(self.op_sig)
        for d in sorted(deps):
            if (not dma) and (not self.op_dma[d]) and self.op_eng[d] == eng:
                if eng == "pe":
                    continue
                israw = any(self.last_write.get(r) == d for r in reads)
                if not israw:
                    continue
            self._wait(eng, d)
        if dma:
            n = self.dma_n[eng]
            self.dma_n[eng] += 1
            slot = n % self.RING
            key = ("dma", eng, slot)
            val = 16 * (n // self.RING + 1)
            if val > 16:
                kn = self.known[eng]
                if kn.get(key, 0) < val - 16:
                    self._emit_wait(eng, key, val - 16)
                    kn[key] = val - 16
            self.stream[eng].append(("op", fn, None, key))
        else:
            self.vt[eng] += 1
            key = ("eng", eng)
            val = self.vt[eng]
            self.stream[eng].append(("op", fn, val, None))
        clock = dict(self.known[eng])
        clock[key] = val
        self.op_sig.append((key, val))
        self.op_clock.append(clock)
        self.op_eng.append(eng)
        self.op_dma.append(dma)
        self.last_sig[key] = max(self.last_sig.get(key, 0), val)
        for r in reads:
            self.readers.setdefault(r, []).append(opid)
        for w_ in writes:
            self.last_write[w_] = opid
            self.readers[w_] = []
        return opid

    def barrier(self):
        for eng in self.ENGS:
            kn = self.known[eng]
            for key, val in list(self.last_sig.items()):
                if kn.get(key, 0) < val:
                    self._emit_wait(eng, key, val)
                    kn[key] = val
        self.last_write = {}
        self.readers = {}

    def emit(self):
        nc = self.nc
        vmap = {}
        semkeys = set()
        for e in self.ENGS:
            cnt = 0
            ep = 0
            for vt in sorted(self.need_sig[e]):
                if cnt >= self.EPOCH:
                    ep += 1
                    cnt = 0
                cnt += 1
                vmap[(e, vt)] = (("eng", e, ep), cnt)
                semkeys.add(("eng", e, ep))
            for it in self.stream[e]:
                if it[0] == "op" and it[3] is not None:
                    semkeys.add(it[3])
        with ExitStack() as es:
            sems = {}
            for key in sorted(semkeys, key=str):
                sems[key] = es.enter_context(nc.semaphore("s_" + "_".join(str(k) for k in key)))
            block = es.enter_context(nc.Block())
            engmap = {"pe": block.tensor, "act": block.scalar, "dve": block.vector,
                      "pool": block.gpsimd, "sp": block.sync}
            for ename in self.ENGS:
                items = self.stream[ename]

                def body(eng, items=items, ename=ename):
                    for it in items:
                        if it[0] == "wait":
                            key, val = it[1], it[2]
                            if key[0] == "eng":
                                key, val = vmap[(key[1], val)]
                            eng.wait_ge(sems[key], val)
                        else:
                            ins = it[1](eng)
                            if it[3] is not None:
                                ins.then_inc(sems[it[3]], 16)
                            elif (ename, it[2]) in vmap:
                                ins.then_inc(sems[vmap[(ename, it[2])][0]], 1)
                engmap[ename](body)


class Rot:
    def __init__(self, es, alloc, name, shape, dtype, n):
        self.tiles = [es.enter_context(alloc(f"{name}{i}", shape, dtype)) for i in range(n)]
        self.name = name
        self.i = 0

    def next(self):
        k = self.i % len(self.tiles)
        self.i += 1
        return self.tiles[k], (self.name, k)


def mm(S, out, lhsT, rhs, start, stop, r, w):
    S.add("pe", lambda e: e.matmul(out, lhsT, rhs, start=start, stop=stop), reads=r, writes=w)


def tr(S, out, in_, ident, r, w):
    S.add("pe", lambda e: e.transpose(out, in_, ident), reads=r, writes=w)


def act(S, out, in_, func, r, w, **kw):
    S.add("act", lambda e: e.activation(out=out, in_=in_, func=func, **kw), reads=r, writes=w)


def tt(S, eng, out, a, b, op, r, w):
    S.add(eng, lambda e: e.tensor_tensor(out=out, in0=a, in1=b, op=op), reads=r, writes=w)


def ts(S, eng, out, a, s1, op0, r, w, s2=None, op1=None):
    if op1 is None:
        S.add(eng, lambda e: e.tensor_scalar(out=out, in0=a, scalar1=s1, scalar2=None, op0=op0), reads=r, writes=w)
    else:
        S.add(eng, lambda e: e.tensor_scalar(out=out, in0=a, scalar1=s1, scalar2=s2, op0=op0, op1=op1), reads=r, writes=w)


def stt(S, out, a, scalar, b, op0, op1, r, w):
    S.add("dve", lambda e: e.scalar_tensor_tensor(out=out, in0=a, scalar=scalar, in1=b, op0=op0, op1=op1), reads=r, writes=w)


def cp(S, eng, out, in_, r, w):
    if eng == "act":
        S.add("act", lambda e: e.activation(out=out, in_=in_, func=AF.Copy), reads=r, writes=w)
    else:
        S.add(eng, lambda e: e.tensor_copy(out=out, in_=in_), reads=r, writes=w)


def dma(S, q, out, in_, r, w, slow=False):
    if slow:
        S.add(q, lambda e: e.dma_start(out=out, in_=in_, allow_slow_non_contiguous=True), reads=r, writes=w, dma=True)
    else:
        S.add(q, lambda e: e.dma_start(out=out, in_=in_), reads=r, writes=w, dma=True)


def memset(S, eng, ap, val, w, r=()):
    S.add(eng, lambda e: e.memset(ap, val), reads=r, writes=w)


class Ctx:
    count = 0

    def __init__(self, nc, S, es, prefix):
        Ctx.count += 1
        self.nc, self.S, self.es, self.p = nc, S, es, f"{prefix}{Ctx.count}_"
        self.n = 0

    def sb(self, name, shape, dtype=F32):
        return self.es.enter_context(self.nc.sbuf_tensor(self.p + name, shape, dtype))

    def ps(self, name, shape, dtype=F32):
        return self.es.enter_context(self.nc.psum_tensor(self.p + name, shape, dtype))

    def rot(self, name, shape, dtype, n, psum=False):
        alloc = (lambda a, b, c: self.nc.psum_tensor(a, b, c)) if psum else (lambda a, b, c: self.nc.sbuf_tensor(a, b, c))
        return Rot(self.es, alloc, self.p + name, shape, dtype, n)


def rms_fm(S, src, src_res, nk, nfeat, gcol, ones_bf, sq_rot, psr, tmp, tmp_res, rstd, rstd_res, dst, dst_res, width, pn=128):
    pt, pres = psr.next()
    for k in range(nk):
        sq, sqr = sq_rot.next()
        act(S, sq[:pn, :width], src[:pn, k, :width], AF.Square, [src_res[k]], [sqr])
        mm(S, pt[:pn, :width], ones_bf[:pn, :pn], sq[:pn, :width], k == 0, k == nk - 1, [sqr, "ones"], [pres])
    act(S, tmp[:pn, :width], pt[:pn, :width], AF.Sqrt, [pres], [tmp_res], scale=1.0 / nfeat, bias=EPS)
    S.add("dve", lambda e: e.reciprocal(out=rstd[:pn, :width], in_=tmp[:pn, :width]), reads=[tmp_res], writes=[rstd_res])
    for k in range(nk):
        stt(S, dst[:pn, k, :width], src[:pn, k, :width], gcol[:pn, k:k + 1], rstd[:pn, :width], ALU.mult, ALU.mult,
            [src_res[k], rstd_res], [dst_res[k]])


def phase_mlp(S, nc, l, xin, xout, norm_mlp, w_up, w_down, ntiles=NT, wq_="sp"):
    with ExitStack() as es:
        C = Ctx(nc, S, es, "m_")
        ones_bf = C.sb("ones", [128, 128], BF16)
        gcol = C.sb("g", [128, 8])
        xt_rot = C.rot("xt", [128, 8, TT], F32, 2)
        sq_rot = C.rot("sq", [128, TT], BF16, 2)
        hT = C.sb("hT", [128, 8, TT], BF16)
        tmp = C.sb("tmp", [128, TT])
        rstd = C.sb("rstd", [128, TT])
        r_rot = C.rot("r", [128, TT], BF16, 3)
        aT = C.sb("aT", [128, 32, TT], BF16)
        wup_rot = C.rot("wup", [128, 8, 512], BF16, 2)
        wdn_rot = C.rot("wdn", [128, 32, 256], BF16, 2)
        psr = C.rot("ps", [128, TT], F32, 7, psum=True)
        memset(S, "pool", ones_bf[:, :], 1.0, ["ones"])
        dma(S, "sp", gcol[:, :], norm_mlp[l].rearrange("(c p) -> p c", p=128), [], ["g"], slow=True)
        xin_v = xin.rearrange("(c p) n -> p c n", p=128)
        xout_v = xout.rearrange("(c p) n -> p c n", p=128)
        wup_v = w_up[l].rearrange("(c p) n -> p c n", p=128)
        wdn_v = w_down[l].rearrange("(c p) n -> p c n", p=128)
        for t in range(ntiles):
            tok = slice(t * TT, (t + 1) * TT)
            xt, x0 = xt_rot.next()
            xres = [(x0, d) for d in range(8)]
            dma(S, "sp", xt[:, :, :], xin_v[:, :, tok], [("xin", t)], xres)
            hres = [("m_hT", k) for k in range(8)]
            rms_fm(S, xt, xres, 8, D, gcol, ones_bf, sq_rot, psr, tmp, "m_tmp", rstd, "m_rstd", hT, hres, TT)
            for jb in range(8):
                wu, wures = wup_rot.next()
                dma(S, wq_, wu[:, :, :], wup_v[:, :, jb * 512:(jb + 1) * 512], [], [wures])
                for jj in range(4):
                    j = jb * 4 + jj
                    pt, pres = psr.next()
                    for k in range(8):
                        mm(S, pt[:, :], wu[:, k, jj * 128:(jj + 1) * 128], hT[:, k, :], k == 0, k == 7, [wures, hres[k]], [pres])
                    r, rres = r_rot.next()
                    act(S, r[:, :], pt[:, :], AF.Relu, [pres], [rres])
                    tt(S, "dve", aT[:, j, :], r[:, :], r[:, :], ALU.mult, [rres], [("m_aT", j)])
            for db in range(4):
                wd, wdres = wdn_rot.next()
                dma(S, wq_, wd[:, :, :], wdn_v[:, :, db * 256:(db + 1) * 256], [], [wdres])
                for dd in range(2):
                    d = db * 2 + dd
                    pt, pres = psr.next()
                    for j in range(32):
                        mm(S, pt[:, :], wd[:, j, dd * 128:(dd + 1) * 128], aT[:, j, :], j == 0, j == 31, [wdres, ("m_aT", j)], [pres])
                    tt(S, "dve", xt[:, d, :], xt[:, d, :], pt[:, :], ALU.add, [pres, xres[d]], [xres[d]])
            dma(S, "sp", xout_v[:, :, tok], xt[:, :, :], xres, [("xout", t)])
    S.barrier()


def phase_final(S, nc, xin, xout, norm_final, ntiles=NT):
    with ExitStack() as es:
        C = Ctx(nc, S, es, "f_")
        ones_bf = C.sb("ones", [128, 128], BF16)
        gcol = C.sb("g", [128, 8])
        xt_rot = C.rot("xt", [128, 8, TT], F32, 2)
        ot_rot = C.rot("ot", [128, 8, TT], F32, 2)
        sq_rot = C.rot("sq", [128, TT], BF16, 2)
        tmp = C.sb("tmp", [128, TT])
        rstd = C.sb("rstd", [128, TT])
        psr = C.rot("ps", [128, TT], F32, 2, psum=True)
        memset(S, "pool", ones_bf[:, :], 1.0, ["ones"])
        dma(S, "sp", gcol[:, :], norm_final.rearrange("(c p) -> p c", p=128), [], ["g"], slow=True)
        xin_v = xin.rearrange("(c p) n -> p c n", p=128)
        xout_v = xout.rearrange("(c p) n -> p c n", p=128)
        for t in range(ntiles):
            tok = slice(t * TT, (t + 1) * TT)
            xt, x0 = xt_rot.next()
            xres = [(x0, d) for d in range(8)]
            dma(S, "sp", xt[:, :, :], xin_v[:, :, tok], [("xin", t)], xres)
            ot, o0 = ot_rot.next()
            ores = [(o0, d) for d in range(8)]
            rms_fm(S, xt, xres, 8, D, gcol, ones_bf, sq_rot, psr, tmp, "f_tmp", rstd, "f_rstd", ot, ores, TT)
            dma(S, "sp", xout_v[:, :, tok], ot[:, :, :], ores, [("xout", t)])
    S.barrier()


def phase_xattn(S, nc, l, xin, xout, memT, norm_x, norm_mem, wq, wk, wv, wo, ntiles=NT):
    with ExitStack() as es:
        C = Ctx(nc, S, es, "x_")
        ones_bf = C.sb("ones", [128, 128], BF16)
        gx = C.sb("gx", [128, 8])
        gm = C.sb("gm", [128, 8])
        mt = C.sb("mt", [128, 8, 256])
        mn = C.sb("mn", [128, 8, 256], BF16)
        wq_t = C.sb("wq", [128, 8, 1024], BF16)
        wo_t = C.sb("wo", [128, 8, 1024], BF16)
        wtmp_rot = C.rot("wtmp", [128, 8, 1024], BF16, 1)
        KT = C.sb("KT", [128, 8, 256], BF16)
        V = C.sb("V", [128, 2, 1024], BF16)
        xt_rot = C.rot("xt", [128, 8, TT], F32, 2)
        sq_rot = C.rot("sq", [128, TT], BF16, 2)
        hT = C.sb("hT", [128, 8, TT], BF16)
        qT = C.sb("qT", [128, 8, TT], BF16)
        oT = C.sb("oT", [128, 8, TT], BF16)
        pT_rot = C.rot("pT", [128, 2, TT], BF16, 2)
        tmp = C.sb("tmp", [128, TT])
        rstd = C.sb("rstd", [128, TT])
        rinv = C.sb("rinv", [128, TT])
        psr = C.rot("ps", [128, TT], F32, 7, psum=True)
        memset(S, "pool", ones_bf[:, :], 1.0, ["ones"])
        dma(S, "sp", gx[:, :], norm_x[l].rearrange("(c p) -> p c", p=128), [], ["gx"], slow=True)
        dma(S, "sp", gm[:, :], norm_mem[l].rearrange("(c p) -> p c", p=128), [], ["gm"], slow=True)
        dma(S, "sp", mt[:, :, :], memT.rearrange("(c p) n -> p c n", p=128), [], [("mt", k) for k in range(8)])
        dma(S, "pool", wq_t[:, :, :], wq[l].rearrange("(c p) n -> p c n", p=128), [], ["wq"])
        dma(S, "pool", wo_t[:, :, :], wo[l].rearrange("(c p) n -> p c n", p=128), [], ["wo"])
        mres = [("mn", k) for k in range(8)]
        rms_fm(S, mt, [("mt", k) for k in range(8)], 8, D, gm, ones_bf, sq_rot, psr, tmp, "x_tmp", rstd, "x_rstd", mn, mres, 256)
        wt, wres = wtmp_rot.next()
        dma(S, "pool", wt[:, :, :], wk[l].rearrange("(c p) n -> p c n", p=128), [], [wres])
        for c in range(8):
            pt, pres = psr.next()
            for k in range(8):
                mm(S, pt[:, :256], wt[:, k, c * 128:(c + 1) * 128], mn[:, k, :], k == 0, k == 7, [wres, mres[k]], [pres])
            cp(S, "act", KT[:, c, :], pt[:, :256], [pres], [("KT", c)])
        wt, wres = wtmp_rot.next()
        dma(S, "pool", wt[:, :, :], wv[l].rearrange("(c p) n -> p c n", p=128), [], [wres])
        for ms in range(2):
            for cb in range(2):
                pt, pres = psr.next()
                for k in range(8):
                    mm(S, pt[:, :], mn[:, k, ms * 128:(ms + 1) * 128], wt[:, k, cb * 512:(cb + 1) * 512], k == 0, k == 7, [wres, mres[k]], [pres])
                cp(S, "act", V[:, ms, cb * 512:(cb + 1) * 512], pt[:, :], [pres], [("V", ms, cb)])
        xin_v = xin.rearrange("(c p) n -> p c n", p=128)
        xout_v = xout.rearrange("(c p) n -> p c n", p=128)
        for t in range(ntiles):
            tok = slice(t * TT, (t + 1) * TT)
            xt, x0 = xt_rot.next()
            xres = [(x0, d) for d in range(8)]
            dma(S, "sp", xt[:, :, :], xin_v[:, :, tok], [("xin", t)], xres)
            hres = [("x_hT", k) for k in range(8)]
            rms_fm(S, xt, xres, 8, D, gx, ones_bf, sq_rot, psr, tmp, "x_tmp", rstd, "x_rstd", hT, hres, TT)
            for c in range(8):
                pt, pres = psr.next()
                for k in range(8):
                    mm(S, pt[:, :], wq_t[:, k, c * 128:(c + 1) * 128], hT[:, k, :], k == 0, k == 7, ["wq", hres[k]], [pres])
                cp(S, "act" if c % 2 else "dve", qT[:, c, :], pt[:, :], [pres], [("qT", c)])
            for h in range(4):
                pT, pTres = pT_rot.next()
                for ms in range(2):
                    pt, pres = psr.next()
                    for cc in range(2):
                        c = h * 2 + cc
                        mm(S, pt[:, :], KT[:, c, ms * 128:(ms + 1) * 128], qT[:, c, :], cc == 0, cc == 1, [("KT", c), ("qT", c)], [pres])
                    act(S, pT[:, ms, :], pt[:, :], AF.Exp, [pres], [(pTres, ms)], scale=1.0 / 16.0)
                pt, pres = psr.next()
                for ms in range(2):
                    mm(S, pt[:, :], ones_bf[:, :], pT[:, ms, :], ms == 0, ms == 1, ["ones", (pTres, ms)], [pres])
                S.add("dve", lambda e, pt=pt: e.reciprocal(out=rinv[:, :], in_=pt[:, :]), reads=[pres], writes=["rinv"])
                for cc in range(2):
                    c = h * 2 + cc
                    pt, pres = psr.next()
                    for ms in range(2):
                        mm(S, pt[:, :], V[:, ms, c * 128:(c + 1) * 128], pT[:, ms, :], ms == 0, ms == 1,
                           [("V", ms, c // 4), (pTres, ms)], [pres])
                    tt(S, "dve", oT[:, c, :], pt[:, :], rinv[:, :], ALU.mult, [pres, "rinv"], [("oT", c)])
            for d in range(8):
                pt, pres = psr.next()
                for c in range(8):
                    mm(S, pt[:, :], wo_t[:, c, d * 128:(d + 1) * 128], oT[:, c, :], c == 0, c == 7, ["wo", ("oT", c)], [pres])
                tt(S, "dve", xt[:, d, :], xt[:, d, :], pt[:, :], ALU.add, [pres, xres[d]], [xres[d]])
            dma(S, "sp", xout_v[:, :, tok], xt[:, :, :], xres, [("xout", t)])
    S.barrier()


def phase_merge(S, nc, l, xin, xout, hT_s, oaT_s, oT_s, wg_bf, w_branch, w_out, ntiles=NT):
    with ExitStack() as es:
        C = Ctx(nc, S, es, "g_")
        wba = C.sb("wba", [64, 4, 1024], BF16)
        wb = C.sb("wb", [128, 3, 2, 1024], BF16)
        wout = C.sb("wout", [128, 8, 1024], BF16)
        wg_rot = C.rot("wg", [128, 8, 1024], BF16, 2)
        xt_rot = C.rot("xt", [128, 8, TT], F32, 2)
        h_rot = C.rot("h", [128, 8, TT], BF16, 2)
        oa_rot = C.rot("oa", [64, 4, TT], BF16, 2)
        ob_rot = C.rot("ob", [128, 3, 2, TT], BF16, 2)
        sig_rot = C.rot("sig", [128, TT], BF16, 3)
        prod_rot = C.rot("prod", [128, TT], F32, 3)
        acc = C.sb("acc", [128, 8, TT])
        mT = C.sb("mT", [128, 8, TT], BF16)
        psr = C.rot("ps", [128, TT], F32, 7, psum=True)
        dma(S, "pool", wba[:, :, :], w_branch[l, 0].rearrange("(h p) n -> p h n", p=64), [], ["wba"])
        for n in range(3):
            dma(S, "pool", wb[:, n, :, :], w_branch[l, n + 1].rearrange("(c p) n -> p c n", p=128), [], [("wb", n)])
        dma(S, "pool", wout[:, :, :], w_out[l].rearrange("(c p) n -> p c n", p=128), [], ["wout"])
        xin_v = xin.rearrange("(c p) n -> p c n", p=128)
        xout_v = xout.rearrange("(c p) n -> p c n", p=128)
        hv = hT_s.rearrange("(c p) n -> p c n", p=128)
        win_v = wg_bf[l].rearrange("(c p) n -> p c n", p=128)
        for t in range(ntiles):
            tok = slice(t * TT, (t + 1) * TT)
            xt, x0 = xt_rot.next()
            xres = [(x0, d) for d in range(8)]
            dma(S, "sp", xt[:, :, :], xin_v[:, :, tok], [("xin", t)], xres)
            ht, hres = h_rot.next()
            dma(S, "sp", ht[:, :, :], hv[:, :, tok], [("hT_s", t)], [hres])
            oa, oares = oa_rot.next()
            dma(S, "sp", oa[:, :, :], oaT_s.rearrange("h p n -> p h n")[:, :, tok], [("oaT_s", t)], [oares])
            ob, obres = ob_rot.next()
            for n in range(3):
                dma(S, "sp", ob[:, n, :, :], oT_s[n].rearrange("(c p) n -> p c n", p=128)[:, :, tok], [("oT_s", n, t)], [(obres, n)])
            for n in range(4):
                wg, wgres = wg_rot.next()
                dma(S, "sp", wg[:, :, :], win_v[:, :, n * 1024:(n + 1) * 1024], [], [wgres])
                for d in range(8):
                    pt, pres = psr.next()
                    for k in range(8):
                        mm(S, pt[:, :], wg[:, k, d * 128:(d + 1) * 128], ht[:, k, :], k == 0, k == 7, [wgres, hres], [pres])
                    sg, sgres = sig_rot.next()
                    act(S, sg[:, :], pt[:, :], AF.Sigmoid, [pres], [sgres])
                    pt2, pres2 = psr.next()
                    if n == 0:
                        for h in range(4):
                            mm(S, pt2[:, :], wba[:, h, d * 128:(d + 1) * 128], oa[:, h, :], h == 0, h == 3, ["wba", oares], [pres2])
                    else:
                        for c in range(2):
                            mm(S, pt2[:, :], wb[:, n - 1, c, d * 128:(d + 1) * 128], ob[:, n - 1, c, :], c == 0, c == 1,
                               [("wb", n - 1), (obres, n - 1)], [pres2])
                    if n == 0:
                        tt(S, "dve", acc[:, d, :], pt2[:, :], sg[:, :], ALU.mult, [pres2, sgres], [("acc", d)])
                    else:
                        pr, prres = prod_rot.next()
                        tt(S, "dve", pr[:, :], pt2[:, :], sg[:, :], ALU.mult, [pres2, sgres], [prres])
                        if n < 3:
                            tt(S, "pool", acc[:, d, :], acc[:, d, :], pr[:, :], ALU.add, [("acc", d), prres], [("acc", d)])
                        else:
                            tt(S, "pool", mT[:, d, :], acc[:, d, :], pr[:, :], ALU.add, [("acc", d), prres], [("mT", d)])
            for d in range(8):
                pt, pres = psr.next()
                for c in range(8):
                    mm(S, pt[:, :], wout[:, c, d * 128:(d + 1) * 128], mT[:, c, :], c == 0, c == 7, ["wout", ("mT", c)], [pres])
                tt(S, "dve", xt[:, d, :], xt[:, d, :], pt[:, :], ALU.add, [pres, xres[d]], [xres[d]])
            dma(S, "sp", xout_v[:, :, tok], xt[:, :, :], xres, [("xout", t)])
    S.barrier()


def phase_mla(S, nc, qT_s, kT_s, v_s, oaT_s, cmask_d, nq=NT):
    with ExitStack() as es:
        C = Ctx(nc, S, es, "a_")
        KT = C.sb("KT", [96, SEQ], BF16)
        V = C.sb("V", [128, SEQ // 128, 65], BF16)
        q_rot = C.rot("q", [96, TT], BF16, 2)
        pT_rot = C.rot("pT", [128, TT], BF16, 4)
        cm = C.sb("cm", [128, 128], BF16)
        ones1 = C.sb("ones1", [65, 64])
        oS = C.sb("oS", [65, TT])
        rinv = C.sb("rinv", [65, TT])
        o_rot = C.rot("o", [64, TT], BF16, 2)
        psr = C.rot("ps", [128, TT], F32, 4, psum=True)
        pso = C.rot("pso", [128, TT], F32, 2, psum=True)
        psb = C.rot("psb", [128, TT], F32, 1, psum=True)
        dma(S, "pool", cm[:, :], cmask_d, [], ["cm"])
        memset(S, "pool", ones1[:, :], 1.0, ["ones1"])
        scale = float(96 ** -0.5)
        for h in range(4):
            dma(S, "sp", KT[:, :], kT_s[h], [("kT_s",)], ["KT"])
            dma(S, "sp", V[:, :, :], v_s.rearrange("(n p) (h e) -> p n h e", p=128, e=65)[:, :, h, :], [("v_s",)], ["V"], slow=True)
            for qt in range(nq):
                q, qres = q_rot.next()
                dma(S, "sp", q[:, :], qT_s[h][:, qt * TT:(qt + 1) * TT], [("qT_s",)], [qres])
                po, pores = pso.next()
                nkb = 4 * qt + 4
                for kb in range(nkb):
                    r = kb - 4 * qt
                    q0 = max(r, 0) * 128
                    pt, pres = psr.next()
                    mm(S, pt[:, q0:], KT[:, kb * 128:(kb + 1) * 128], q[:, q0:], True, True, ["KT", qres], [pres])
                    pT, pTres = pT_rot.next()
                    act(S, pT[:, q0:], pt[:, q0:], AF.Exp, [pres], [pTres], scale=scale)
                    if r >= 0:
                        tt(S, "pool", pT[:, q0:q0 + 128], pT[:, q0:q0 + 128], cm[:, :], ALU.mult, [pTres, "cm"], [pTres])
                    mm(S, po[:65, q0:], V[:, kb, :], pT[:, q0:], kb == 0, kb == nkb - 1, ["V", pTres], [pores])
                cp(S, "act", oS[:, :], po[:65, :], [pores], ["oS"])
                S.add("dve", lambda e: e.reciprocal(out=rinv[64:65, :], in_=oS[64:65, :]), reads=["oS"], writes=["rinv"])
                pb, pbres = psb.next()
                mm(S, pb[:64, :], ones1[64:65, :], rinv[64:65, :], True, True, ["ones1", "rinv"], [pbres])
                o, ores = o_rot.next()
                tt(S, "dve", o[:, :], pb[:64, :], oS[:64, :], ALU.mult, [pbres, "oS"], [ores])
                dma(S, "sp", oaT_s[h][:, qt * TT:(qt + 1) * TT], o[:, :], [ores], [("oaT_s", qt)])
    S.barrier()


def phase_precast(S, nc, P, sc):
    with ExitStack() as es:
        C = Ctx(nc, S, es, "pc_")
        st_rot = C.rot("st", [128, 8, 512], F32, 3)
        bf_rot = C.rot("bf", [128, 8, 512], BF16, 3)
        n = 0
        for l in range(NL):
            jobs = []
            for jb in range(8):
                jobs.append((P["w_up"][l].rearrange("(c p) n -> p c n", p=128)[:, :, jb * 512:(jb + 1) * 512],
                             sc["wup_bf"][l].rearrange("(c p) n -> p c n", p=128)[:, :, jb * 512:(jb + 1) * 512]))
                jobs.append((P["w_in"][l].rearrange("(c p) n -> p c n", p=128)[:, :, 2744 + jb * 512:2744 + (jb + 1) * 512],
                             sc["wg_bf"][l].rearrange("(c p) n -> p c n", p=128)[:, :, jb * 512:(jb + 1) * 512]))
            for cb in range(4):
                for nb in range(2):
                    jobs.append((P["w_down"][l].rearrange("(c p) n -> p c n", p=128)[:, cb * 8:(cb + 1) * 8, nb * 512:(nb + 1) * 512],
                                 sc["wdn_bf"][l].rearrange("(c p) n -> p c n", p=128)[:, cb * 8:(cb + 1) * 8, nb * 512:(nb + 1) * 512]))
            for src, dst in jobs:
                st, stres = st_rot.next()
                bf, bfres = bf_rot.next()
                dma(S, "sp", st[:, :, :], src, [], [stres])
                eng = ("act", "dve", "pool")[n % 3]
                n += 1
                cp(S, eng, bf[:, :, :], st[:, :, :], [stres], [bfres])
                dma(S, "sp", dst, bf[:, :, :], [bfres], [("pc_out", n)])
    S.barrier()

def phase_rope(S, nc, posrep, cconst, cos_s, sin_s):
    with ExitStack() as es:
        C = Ctx(nc, S, es, "r_")
        pi_ = C.sb("pi", [96, TT], I32)
        pf = C.sb("pf", [96, TT])
        ang = C.sb("ang", [96, TT])
        ni = C.sb("ni", [96, TT], I32)
        nf = C.sb("nf", [96, TT])
        y = C.sb("y", [96, TT])
        cc = C.sb("cc", [96, 2])
        dma(S, "sp", cc[:, :], cconst, [], ["cc"])
        for t in range(NT):
            tok = slice(t * TT, (t + 1) * TT)
            dma(S, "sp", pi_[:, :], posrep[:, tok], [], ["pi"])
            cp(S, "dve", pf[:, :], pi_[:, :], ["pi"], ["pf"])
            for which in range(2):
                off = 0.0 if which == 0 else float(np.pi / 2)
                ts(S, "dve", ang[:, :], pf[:, :], cc[:, 0:1], ALU.mult, ["pf", "cc"], ["ang"], s2=off, op1=ALU.add)
                ts(S, "dve", ni[:, :], ang[:, :], float(1 / (2 * np.pi)), ALU.mult, ["ang"], ["ni"])
                cp(S, "dve", nf[:, :], ni[:, :], ["ni"], ["nf"])
                stt(S, y[:, :], nf[:, :], float(-2 * np.pi), ang[:, :], ALU.mult, ALU.add, ["nf", "ang"], ["y"])
                ts(S, "dve", nf[:, :], y[:, :], float(np.pi), ALU.is_gt, ["y"], ["nf2"], s2=float(-2 * np.pi), op1=ALU.mult)
                tt(S, "dve", y[:, :], y[:, :], nf[:, :], ALU.add, ["y", "nf2"], ["y2"])
                ts(S, "dve", y[:, :], y[:, :], float(np.pi), ALU.min, ["y2"], ["y3"], s2=float(-np.pi), op1=ALU.max)
                act(S, ang[:, :], y[:, :], AF.Sin, ["y3"], ["sv"])
                if which == 0:
                    ts(S, "dve", y[:, :], ang[:, :], cc[:, 1:2], ALU.mult, ["sv", "cc"], ["yo"])
                    dma(S, "sp", sin_s[:, tok], y[:, :], ["yo"], [("sin_s", t)])
                else:
                    dma(S, "sp", cos_s[:, tok], ang[:, :], ["sv"], [("cos_s", t)])
    S.barrier()


def bcast_row(S, C, dst, src_row, n, ones_row, psr, tag):
    st = C.sb("bst_" + tag, [1, n])
    dma(S, "sp", st[:, :], src_row, [], ["bst_" + tag])
    pt, pres = psr.next()
    mm(S, pt[:, :n], ones_row[0:1, :], st[0:1, :], True, True, ["ones_row", "bst_" + tag], [pres])
    cp(S, "act", dst, pt[:, :n], [pres], ["bc_" + tag])


def phase_A(S, nc, l, xin, P, K, sc, ntiles=None, do=("mla", "sg", "gla", "dn")):
    TA = 256
    NCH = TA // 64
    NSUB = TA // 128
    if ntiles is None:
        ntiles = SEQ // TA
    with ExitStack() as es:
        C = Ctx(nc, S, es, "A_")
        sb = C.sb
        ones_bf = sb("ones", [128, 128], BF16)
        ones_row = sb("ones_row", [1, 128])
        ident = sb("ident", [128, 128])
        memset(S, "pool", ones_bf[:, :], 1.0, ["ones"])
        memset(S, "pool", ones_row[:, :], 1.0, ["ones_row"])
        dma(S, "sp", ident[:, :], K["ident"], [], ["ident"])
        Win = sb("Win", [128, 8, 2744], BF16)
        win_v = P["w_in"][l].rearrange("(c p) n -> p c n", p=128)
        for cb in range(0, 2744, 512):
            ce = min(cb + 512, 2744)
            dma(S, "pool", Win[:, :, cb:ce], win_v[:, :, cb:ce], [], ["Win"])
        gmix = sb("gmix", [128, 8])
        dma(S, "sp", gmix[:, :], P["norm_mix"][l].rearrange("(c p) -> p c", p=128), [], ["gmix"], slow=True)
        xt_rot = C.rot("xt", [128, 8, TA], F32, 1)
        sq_rot = C.rot("sq", [128, TA], BF16, 2)
        hT = sb("hT", [128, 8, TA], BF16)
        tmp = sb("tmp", [128, TA])
        rstd = sb("rstd", [128, TA])
        psr = C.rot("ps", [128, 512], F32, 8, psum=True)
        xin_v = xin.rearrange("(c p) n -> p c n", p=128)
        hv = sc["hT_s"].rearrange("(c p) n -> p c n", p=128)
        hres = [("A_hT", k) for k in range(8)]

        def fproj(pt, pres, c0, n, rows=None):
            for k in range(8):
                mm(S, pt[:n, :TA], Win[:, k, c0:c0 + n], hT[:, k, :], k == 0, k == 7, ["Win", hres[k]], [pres])

        def tproj(pt, pres, s, c0, n, o0=0):
            for k in range(8):
                mm(S, pt[:, o0:o0 + n], hT[:, k, s * 128:(s + 1) * 128], Win[:, k, c0:c0 + n], k == 0, k == 7, ["Win", hres[k]], [pres])

        if "mla" in do:
            gq = sb("gq", [128, 2])
            gkv = sb("gkv", [128, 1])
            dma(S, "sp", gq[:, :], P["mla_norm_q"][l].rearrange("(c p) -> p c", p=128), [], ["gq"], slow=True)
            dma(S, "sp", gkv[:, :], P["mla_norm_kv"][l].rearrange("(c p) -> p c", p=128), [], ["gkv"], slow=True)
            Wkr = sb("Wkr", [128, 8, 96], BF16)
            Wkt = sb("Wkt", [128, 8, 96], BF16)
            memset(S, "pool", Wkr[:, :, :], 0.0, ["Wkr"])
            memset(S, "pool", Wkt[:, :, :], 0.0, ["Wkt"])
            dma(S, "pool", Wkr[:, :, 64:96], win_v[:, :, 384:416], [], ["Wkr"])
            dma(S, "pool", Wkt[:, :, 64:80], win_v[:, :, 400:416], [], ["Wkt"])
            dma(S, "pool", Wkt[:, :, 80:96], win_v[:, :, 384:400], [], ["Wkt"])
            Wuq = sb("Wuq", [128, 2, 384], BF16)
            dma(S, "pool", Wuq[:, :, :], P["mla_w_uq"][l].rearrange("(c p) n -> p c n", p=128), [], ["Wuq"])
            Wuqt = sb("Wuqt", [128, 2, 4, 96], BF16)
            memset(S, "pool", Wuqt[:, :, :, :], 0.0, ["Wuqt"])
            uqv = P["mla_w_uq"][l].rearrange("(c p) (h e) -> p c h e", p=128, e=96)
            for c in range(2):
                dma(S, "pool", Wuqt[:, c, :, 64:80], uqv[:, c, :, 80:96], [], ["Wuqt"])
                dma(S, "pool", Wuqt[:, c, :, 80:96], uqv[:, c, :, 64:80], [], ["Wuqt"])
            Wkn = sb("Wkn", [128, 4, 96], BF16)
            memset(S, "pool", Wkn[:, :, :], 0.0, ["Wkn"])
            ukv = P["mla_w_ukv"][l].rearrange("k (h e) -> k h e", e=128)
            dma(S, "pool", Wkn[:, :, 0:64], ukv[:, :, 0:64], [], ["Wkn"])
            Wv = sb("Wv", [128, 4, 64], BF16)
            dma(S, "pool", Wv[:, :, :], ukv[:, :, 64:128], [], ["Wv"])
            cqs = sb("cqs", [128, 2, TA])
            cqn = sb("cqn", [128, 2, TA], BF16)
            ckvs = sb("ckvs", [128, 1, TA])
            ckvn = sb("ckvn", [128, 1, TA], BF16)
            cosT = sb("cosT", [96, TA])
            sinT = sb("sinT", [96, TA])
            tmpK = sb("tmpK", [96, TA])
            t1_rot = C.rot("t1", [96, TA], F32, 1)
            t2_rot = C.rot("t2", [96, TA], F32, 1)
            qk_rot = C.rot("qk", [96, TA], BF16, 2)
            vt = sb("vt", [128, NSUB, 4, 65], BF16)
            memset(S, "pool", vt[:, :, :, :], 1.0, ["vt"])
        if "sg" in do:
            WmT = sb("WmT", [128, 4, 128])
            sg_t = sb("sg_t", [128, 512])
            sgw = sg_t[:, :].rearrange("p (g j) -> p g j", g=4)
            sgmask = sb("sgmask", [128, 128])
            dma(S, "sp", sgmask[:, :], K["trilT"], [], ["sgmask"])
            dma(S, "sp", sgw, P["sg_w"][l].rearrange("g i j -> i g j"), [], ["sg_t"])
            for g in range(4):
                pt, pres = psr.next()
                tr(S, pt[:, :128], sgw[:, g, :], ident[:, :], ["sg_t", "ident"], [pres])
                tt(S, "dve", WmT[:, g, :], pt[:, :128], sgmask[:, :], ALU.mult, [pres, "sgmask"], ["WmT"])
            sgb = sb("sgb", [128, 4])
            dma(S, "sp", sgb[:, :], P["sg_b"][l].rearrange("g i -> i g"), [], ["sgb"], slow=True)
            lng = sb("lng", [128, 256])
            lnb = sb("lnb", [128, 256])
            bcast_row(S, C, lng[:, :], P["sg_ln_g"][l:l + 1, :], 256, ones_row, psr, "lng")
            bcast_row(S, C, lnb[:, :], P["sg_ln_b"][l:l + 1, :], 256, ones_row, psr, "lnb")
            sg_i = sb("sg_i", [128, 512])
            sg_g = sb("sg_g", [128, 512])
            sg_s = sb("sg_s", [128, 8])
            sg_vc = sb("sg_vc", [128, 256])
            sg_junk = sb("sg_junk", [128, 256])
            sg_vn = sb("sg_vn", [128, 256])
            sg_o = sb("sg_o", [128, 256])
            obT = sb("obT", [128, 2, TA], BF16)
        if "gla" in do:
            wgate = sb("wgate", [16, 128])
            dma(S, "sp", wgate[:, :], P["gla_w_gate"][l], [], ["wgate"])
            nbg = sb("nbg", [128, 1])
            dma(S, "sp", nbg[:, :], P["gla_b_gate"][l].rearrange("(p o) -> p o", o=1), [], ["nbg0"], slow=True)
            ts(S, "dve", nbg[:, :], nbg[:, :], -1.0, ALU.mult, ["nbg0"], ["nbg"])
            rm = sb("rm", [128, TA])
            dma(S, "sp", rm[:, :], K["rm"][:, 0:TA], [], ["rm"])
            mg = sb("mg", [128, 128])
            dma(S, "sp", mg[:, :], K["mg"], [], ["mg"])
            hm = sb("hm", [128, 256])
            dma(S, "sp", hm[:, :], K["hm"], [], ["hm"])
            hcol = sb("hcol", [128, 4])
            dma(S, "sp", hcol[:, :], K["hcol"], [], ["hcol"])
            topbot = sb("topbot", [128, 2])
            dma(S, "sp", topbot[:, :], K["topbot"], [], ["topbot"])
            cmA = sb("cmA", [128, TA])
            dma(S, "sp", cmA[:, :], K["cmA"][:, 0:TA], [], ["cmA"])
            gnb = sb("gnb", [128, 256])
            for h in range(4):
                bcast_row(S, C, gnb[:, h * 64:(h + 1) * 64], P["gla_norm"][l:l + 1, :], 64, ones_row, psr, f"gn{h}")
            glr = sb("glr", [16, TA])
            g_l = sb("g_l", [128, TA])
            g_cum = sb("g_cum", [128, TA])
            g_ec = sb("g_ec", [128, TA])
            g_en = sb("g_en", [128, TA])
            g_qe = sb("g_qe", [128, TA])
            g_qA = sb("g_qA", [128, TA])
            g_qB = sb("g_qB", [128, TA])
            g_ke = sb("g_ke", [128, TA])
            g_kl = sb("g_kl", [128, TA])
            g_qp = sb("g_qp", [128, 4, TA])
            g_klA = sb("g_klA", [128, 128])
            g_klB = sb("g_klB", [128, 128])
            g_v = sb("g_v", [128, 256])
            g_og = sb("g_og", [128, 256])
            g_sc = C.rot("g_sc", [128, 128], F32, 4)
            g_S = sb("g_S", [128, 256])
            g_tmp = sb("g_tmp", [128, 256])
            g_ss = sb("g_ss", [128, 8])
            g_junk = sb("g_junk", [128, 64])
            g_o = sb("g_o", [128, 256])
            ocT = sb("ocT", [128, 2, TA], BF16)
            memset(S, "pool", g_S[:, :], 0.0, ["g_S"])
        if "dn" in do:
            if "gla" not in do:
                rm = sb("rm", [128, TA])
                dma(S, "sp", rm[:, :], K["rm"][:, 0:TA], [], ["rm"])
                topbot = sb("topbot", [128, 2])
                dma(S, "sp", topbot[:, :], K["topbot"], [], ["topbot"])
                cmA = sb("cmA", [128, TA])
                dma(S, "sp", cmA[:, :], K["cmA"][:, 0:TA], [], ["cmA"])
            ones64 = ones_bf
            cU = sb("cU", [128, 128]); dma(S, "sp", cU[:, :], K["U"], [], ["cU"])
            cL = sb("cL", [128, 128]); dma(S, "sp", cL[:, :], K["L"], [], ["cL"])
            cst = sb("cst", [128, 128]); dma(S, "sp", cst[:, :], K["strict"], [], ["cst"])
            sel4 = sb("sel4", [4, 4, 128]); dma(S, "sp", sel4[:, :, :], K["sel4"], [], ["sel4"])
            zrow = sb("zrow", [1, 256]); memset(S, "pool", zrow[:, :], 0.0, ["zrow"])
            cw = sb("cw", [64, 12, 4])
            for kk in range(4):
                dma(S, "sp", cw[:, :, kk], P["dn_conv"][l, kk].rearrange("(i p) -> p i", p=64), [], ["cw"], slow=True)
            alog = sb("alog", [4, 1]); dma(S, "sp", alog[:, :], P["dn_a_log"][l].rearrange("(p o) -> p o", o=1), [], ["alog0"], slow=True)
            dtb = sb("dtb", [4, 1]); dma(S, "sp", dtb[:, :], P["dn_dt_bias"][l].rearrange("(p o) -> p o", o=1), [], ["dtb"], slow=True)
            negA = sb("negA", [4, 1])
            act(S, negA[:, :], alog[:, :], AF.Exp, ["alog0"], ["negA0"])
            ts(S, "dve", negA[:, :], negA[:, :], -1.0, ALU.mult, ["negA0"], ["negA"])
            dnb = sb("dnb", [128, 256])
            for h in range(4):
                bcast_row(S, C, dnb[:, h * 64:(h + 1) * 64], P["dn_norm"][l:l + 1, :], 64, ones_row, psr, f"dnn{h}")
            xc = sb("xc", [64, 12, 3 + TA])
            memset(S, "pool", xc[:, :, 0:3], 0.0, [("xc", i) for i in range(12)])
            d_y = sb("d_y", [64, 12, TA])
            d_sq = C.rot("d_sq", [64, TA], BF16, 2)
            d_t = sb("d_t", [64, TA])
            d_r = sb("d_r", [64, TA])
            d_a = sb("d_a", [4, TA]); d_e = sb("d_e", [4, TA]); d_g = sb("d_g", [4, TA]); d_gam = sb("d_gam", [4, TA])
            d_b = sb("d_b", [4, TA]); d_eg = sb("d_eg", [4, TA]); d_bk = sb("d_bk", [4, TA]); d_lg = sb("d_lg", [4, TA])
            d_elb = sb("d_elb", [64, 4, NCH])
            d_gbc = sb("d_gbc", [128, 4, TA])
            d_kp = sb("d_kp", [64, 4, TA])
            d_qA = sb("d_qA", [64, 4, TA]); d_qB = sb("d_qB", [64, 4, TA])
            d_cols = sb("d_cols", [128, 12])
            d_Vp = sb("d_Vp", [128, 256])
            d_kdA = sb("d_kdA", [128, 4, 64]); d_kdB = sb("d_kdB", [128, 4, 64])
            d_Dm = C.rot("d_Dm", [128, 128], F32, 2)
            d_d1 = C.rot("d_d1", [128, 128], F32, 2)
            d_at = sb("d_at", [128, 4, 128])
            d_BpT = sb("d_BpT", [128, 2, 4, 128])
            d_BtT = sb("d_BtT", [128, 2, 4, 128])
            d_RT = sb("d_RT", [128, 4, 128])

            class _V:
                def __init__(self, t, h):
                    self.t, self.h = t, h

                def __getitem__(self, idx):
                    return self.t[idx[0], idx[1], self.h, idx[2]]

            class _VR:
                def __init__(self, t, h):
                    self.t, self.h = t, h

                def __getitem__(self, idx):
                    return self.t[idx[0], self.h, idx[1]]
            d_Bp = [_V(d_BpT, h) for h in range(4)]
            d_Bt = [_V(d_BtT, h) for h in range(4)]
            d_R = [_VR(d_RT, h) for h in range(4)]
            d_rhs2 = sb("d_rhs2", [128, 256])
            d_u = sb("d_u", [128, 256])
            d_S = sb("d_S", [64, 4, 64])
            memset(S, "pool", d_S[:, :, :], 0.0, ["d_S"])
            memset(S, "pool", d_u[:, :], 0.0, ["d_u"])
            d_z = sb("d_z", [128, 256])
            d_ss = sb("d_ss", [128, 8])
            d_junk = sb("d_junk", [128, 64])
            d_junk2 = sb("d_junk2", [128, 64])
            d_o = sb("d_o", [128, 256])
            odT = sb("odT", [128, 2, TA], BF16)

        def post_norm_gate(o_ps, o_res, gate_sb, gate_res, ss, junk, o_out, tagp, oT_tile, s):
            for h in range(4):
                act(S, junk[:, :], o_ps[:, h * 64:(h + 1) * 64], AF.Square, [o_res], [tagp + "junk"], accum_out=ss[:, h:h + 1])
            S.stream
            act(S, ss[:, 4:8], ss[:, 0:4], AF.Sqrt, [tagp + "junk"], [tagp + "ss1"], scale=1.0 / 64, bias=EPS)
            S.add("dve", lambda e: e.reciprocal(out=ss[:, 0:4], in_=ss[:, 4:8]), reads=[tagp + "ss1"], writes=[tagp + "ss2"])
            for h in range(4):
                stt(S, o_out[:, h * 64:(h + 1) * 64], o_ps[:, h * 64:(h + 1) * 64], ss[:, h:h + 1], gate_sb[:, h * 64:(h + 1) * 64],
                    ALU.mult, ALU.mult, [o_res, tagp + "ss2", gate_res], [tagp + "oo"])
            pt, pres = psr.next()
            for c in range(2):
                tr(S, pt[:, c * 128:(c + 1) * 128], o_out[:, c * 128:(c + 1) * 128], ident[:, :], [tagp + "oo", "ident"], [pres])
            cp(S, "act", oT_tile[:, :, s * 128:(s + 1) * 128], pt[:, :256].rearrange("p (c t) -> p c t", c=2), [pres], [tagp + "oT"])

        for t in range(ntiles):
            tok = slice(t * TA, (t + 1) * TA)
            xt, x0 = xt_rot.next()
            xres = [(x0, d) for d in range(8)]
            dma(S, "sp", xt[:, :, :], xin_v[:, :, tok], [], xres)
            rms_fm(S, xt, xres, 8, D, gmix, ones_bf, sq_rot, psr, tmp, "A_tmp", rstd, "A_rstd", hT, hres, TA)
            dma(S, "sp", hv[:, :, tok], hT[:, :, :], hres, [("hT_s", t)])
            if "mla" in do:
                dma(S, "sp", cosT[:, :], sc["cos_s"][:, tok], [], ["cosT"])
                dma(S, "sp", sinT[:, :], sc["sin_s"][:, tok], [], ["sinT"])
                for c in range(2):
                    pt, pres = psr.next()
                    fproj(pt, pres, c * 128, 128)
                    cp(S, "act", cqs[:, c, :], pt[:, :TA], [pres], [("cqs", c)])
                rms_fm(S, cqs, [("cqs", 0), ("cqs", 1)], 2, 256, gq, ones_bf, sq_rot, psr, tmp, "A_tmp", rstd, "A_rstd",
                       cqn, [("cqn", 0), ("cqn", 1)], TA)
                pt, pres = psr.next()
                fproj(pt, pres, 256, 128)
                cp(S, "act", ckvs[:, 0, :], pt[:, :TA], [pres], [("ckvs", 0)])
                rms_fm(S, ckvs, [("ckvs", 0)], 1, 128, gkv, ones_bf, sq_rot, psr, tmp, "A_tmp", rstd, "A_rstd",
                       ckvn, [("ckvn", 0)], TA)
                pt, pres = psr.next()
                for k in range(8):
                    mm(S, pt[:96, :TA], Wkt[:, k, :], hT[:, k, :], k == 0, k == 7, ["Wkt", hres[k]], [pres])
                tt(S, "dve", tmpK[:, :], pt[:96, :TA], sinT[:, :], ALU.mult, [pres, "sinT"], ["tmpK"])
                for h in range(4):
                    pt, pres = psr.next()
                    for c in range(2):
                        mm(S, pt[:96, :TA], Wuq[:, c, h * 96:(h + 1) * 96], cqn[:, c, :], c == 0, c == 1, ["Wuq", ("cqn", c)], [pres])
                    pt2, pres2 = psr.next()
                    for c in range(2):
                        mm(S, pt2[:96, :TA], Wuqt[:, c, h, :], cqn[:, c, :], c == 0, c == 1, ["Wuqt", ("cqn", c)], [pres2])
                    t1, t1r = t1_rot.next()
                    t2, t2r = t2_rot.next()
                    tt(S, "dve", t1[:, :], pt[:96, :TA], cosT[:, :], ALU.mult, [pres, "cosT"], [t1r])
                    tt(S, "dve", t2[:, :], pt2[:96, :TA], sinT[:, :], ALU.mult, [pres2, "sinT"], [t2r])
                    qk, qkr = qk_rot.next()
                    tt(S, "pool", qk[:, :], t1[:, :], t2[:, :], ALU.add, [t1r, t2r], [qkr])
                    dma(S, "sp", sc["qT_s"][h][:, tok], qk[:, :], [qkr], [("qT_s", h, t)])
                    pt, pres = psr.next()
                    for k in range(8):
                        mm(S, pt[:96, :TA], Wkr[:, k, :], hT[:, k, :], k == 0, False, ["Wkr", hres[k]], [pres])
                    mm(S, pt[:96, :TA], Wkn[:, h, :], ckvn[:, 0, :], False, True, ["Wkn", ("ckvn", 0)], [pres])
                    t1, t1r = t1_rot.next()
                    tt(S, "dve", t1[:, :], pt[:96, :TA], cosT[:, :], ALU.mult, [pres, "cosT"], [t1r])
                    qk, qkr = qk_rot.next()
                    tt(S, "pool", qk[:, :], t1[:, :], tmpK[:, :], ALU.add, [t1r, "tmpK"], [qkr])
                    dma(S, "sp", sc["kT_s"][h][:, tok], qk[:, :], [qkr], [("kT_s", h, t)])
                for s in range(NSUB):
                    pt, pres = psr.next()
                    mm(S, pt[:, :256], ckvn[:, 0, s * 128:(s + 1) * 128], Wv[:, :, :].rearrange("p h e -> p (h e)"), True, True,
                       ["Wv", ("ckvn", 0)], [pres])
                    cp(S, "act", vt[:, s, :, 0:64], pt[:, :256].rearrange("p (h e) -> p h e", e=64), [pres], ["vt"])
                dma(S, "sp", sc["v_s"][tok, :].rearrange("(s p) n -> p s n", p=128), vt[:, :, :, :].rearrange("p s h e -> p s (h e)"),
                    ["vt"], [("v_s", t)])
            if "gla" in do:
                pt, pres = psr.next()
                fproj(pt, pres, 1440, 16)
                cp(S, "act", glr[:, :], pt[:16, :TA], [pres], ["glr"])
                pt, pres = psr.next()
                mm(S, pt[:, :TA], wgate[:, :], glr[:, :], True, True, ["wgate", "glr"], [pres])
                act(S, g_l[:, :], pt[:, :TA], AF.Exp, [pres], ["g_l0"], scale=-1.0, bias=nbg[:, 0:1])
                act(S, g_l[:, :], g_l[:, :], AF.Ln, ["g_l0", "nbg"], ["g_l"], bias=1.0)
                S.add("dve", lambda e: e.tensor_tensor_scan(out=g_cum[:, :], data0=rm[:, :], data1=g_l[:, :], initial=0.0,
                                                            op0=ALU.mult, op1=ALU.add), reads=["rm", "g_l"], writes=["g_cum"])
                act(S, g_ec[:, :], g_cum[:, :], AF.Exp, ["g_cum"], ["g_ec"], scale=-1.0 / 16)
                act(S, g_en[:, :], g_cum[:, :], AF.Exp, ["g_cum"], ["g_en"], scale=1.0 / 16)
                pt, pres = psr.next()
                fproj(pt, pres, 928, 128)
                stt(S, g_qe[:, :], pt[:, :TA], float(32 ** -0.5), g_ec[:, :], ALU.mult, ALU.mult, [pres, "g_ec"], ["g_qe"])
                pt, pres = psr.next()
                fproj(pt, pres, 1056, 128)
                tt(S, "dve", g_ke[:, :], pt[:, :TA], g_en[:, :], ALU.mult, [pres, "g_en"], ["g_ke"])
                for c in range(NCH):
                    ts(S, "dve", g_kl[:, c * 64:(c + 1) * 64], g_ke[:, c * 64:(c + 1) * 64], g_ec[:, c * 64 + 63:c * 64 + 64], ALU.mult,
                       ["g_ke", "g_ec"], [("g_kl", c // 2)])
                for h in range(4):
                    ts(S, "pool", g_qp[:, h, :], g_qe[:, :], hcol[:, h:h + 1], ALU.mult, ["g_qe", "hcol"], [("g_qp", h)])
                tt(S, "pool", g_qA[:, :], g_qe[:, :], cmA[:, :], ALU.mult, ["g_qe", "cmA"], ["g_qA"])
                tt(S, "pool", g_qB[:, :], g_qe[:, :], g_qA[:, :], ALU.subtract, ["g_qe", "g_qA"], ["g_qB"])
            if "dn" in do:
                for i in range(12):
                    pt, pres = psr.next()
                    fproj(pt, pres, 1712 + i * 64, 64)
                    cp(S, "act", xc[:, i, 3:3 + TA], pt[:64, :TA], [pres], [("xc", i)])
                    yi = d_y[:, i, :]
                    ts(S, "dve", yi, xc[:, i, 3:3 + TA], cw[:, i, 3:4], ALU.mult, [("xc", i), "cw"], [("d_y", i)])
                    for kk in range(3):
                        stt(S, yi, xc[:, i, kk:kk + TA], cw[:, i, kk:kk + 1], yi, ALU.mult, ALU.add, [("xc", i), ("d_y", i), "cw"], [("d_y", i)])
                    cp(S, "pool", xc[:, i, 0:3], xc[:, i, TA:TA + 3], [("xc", i)], [("xc", i)])
                    act(S, yi, yi, AF.Silu, [("d_y", i)], [("d_y", i)])
                    if i < 8:
                        sq, sqr = d_sq.next()
                        act(S, sq[:, :], yi, AF.Square, [("d_y", i)], [sqr])
                        pt, pres = psr.next()
                        mm(S, pt[:64, :TA], ones64[:64, :64], sq[:, :], True, True, ["ones", sqr], [pres])
                        act(S, d_t[:, :], pt[:64, :TA], AF.Sqrt, [pres], ["d_t"], bias=EPS)
                        S.add("dve", lambda e: e.reciprocal(out=d_r[:, :], in_=d_t[:, :]), reads=["d_t"], writes=["d_r"])
                        stt(S, yi, yi, 0.125 if i < 4 else 1.0, d_r[:, :], ALU.mult, ALU.mult, [("d_y", i), "d_r"], [("d_y", i)])
                pt, pres = psr.next()
                fproj(pt, pres, 2480, 4)
                act(S, d_e[:, :], pt[:4, :TA], AF.Exp, [pres, "dtb"], ["d_e0"], bias=dtb[:, 0:1])
                act(S, d_e[:, :], d_e[:, :], AF.Ln, ["d_e0"], ["d_e"], bias=1.0)
                ts(S, "dve", d_g[:, :], d_e[:, :], negA[:, 0:1], ALU.mult, ["d_e", "negA"], ["d_g"])
                S.add("dve", lambda e: e.tensor_tensor_scan(out=d_gam[:, :], data0=rm[0:4, :], data1=d_g[:, :], initial=0.0,
                                                            op0=ALU.mult, op1=ALU.add), reads=["rm", "d_g"], writes=["d_gam"])
                pt, pres = psr.next()
                fproj(pt, pres, 2484, 4)
                act(S, d_b[:, :], pt[:4, :TA], AF.Sigmoid, [pres], ["d_b"])
                act(S, d_eg[:, :], d_gam[:, :], AF.Exp, ["d_gam"], ["d_eg"])
                tt(S, "dve", d_bk[:, :], d_b[:, :], d_eg[:, :], ALU.mult, ["d_b", "d_eg"], ["d_bk"])
                for c in range(NCH):
                    ts(S, "dve", d_lg[:, c * 64:(c + 1) * 64], d_gam[:, c * 64:(c + 1) * 64], d_gam[:, c * 64 + 63:c * 64 + 64], ALU.subtract,
                       ["d_gam"], ["d_lg0"])
                act(S, d_lg[:, :], d_lg[:, :], AF.Exp, ["d_lg0"], ["d_lg"], scale=-1.0)
                pt, pres = psr.next()
                for h in range(4):
                    mm(S, pt[:64, h * NCH:(h + 1) * NCH], sel4[:, h, 0:64], d_eg[:, 63:TA:64], True, True, ["sel4", "d_eg"], [pres])
                cp(S, "act", d_elb[:, :, :], pt[:64, 0:4 * NCH].rearrange("p (h c) -> p h c", c=NCH), [pres], ["d_elb"])
                for h in range(4):
                    pt, pres = psr.next()
                    mm(S, pt[:, :TA], sel4[:, h, :], d_gam[:, :], True, True, ["sel4", "d_gam"], [pres])
                    cp(S, "act", d_gbc[:, h, :], pt[:, :TA], [pres], [("d_gbc", h)])
                    pt, pres = psr.next()
                    mm(S, pt[:64, :TA], sel4[:, h, 0:64], d_bk[:, :], True, True, ["sel4", "d_bk"], [pres])
                    tt(S, "dve", d_kp[:, h, :], pt[:64, :TA], d_y[:, 4 + h, :], ALU.mult, [pres, ("d_y", 4 + h)], [("d_kp", h)])
                    pt, pres = psr.next()
                    mm(S, pt[:64, :TA], sel4[:, h, 0:64], d_eg[:, :], True, True, ["sel4", "d_eg"], [pres])
                    tt(S, "dve", d_qB[:, h, :], pt[:64, :TA], d_y[:, h, :], ALU.mult, [pres, ("d_y", h)], [("d_qB0", h)])
                    tt(S, "pool", d_qA[:, h, :], d_qB[:, h, :], cmA[:64, :], ALU.mult, [("d_qB0", h), "cmA"], [("d_qA", h)])
                    tt(S, "pool", d_qB[:, h, :], d_qB[:, h, :], d_qA[:, h, :], ALU.subtract, [("d_qB0", h), ("d_qA", h)], [("d_qB", h)])
            for s in range(NSUB):
                ssl = slice(s * 128, (s + 1) * 128)
                if "sg" in do:
                    pu, pures = psr.next()
                    tproj(pu, pures, s, 416, 512)
                    act(S, sg_t[:, :], pu[:, :], AF.Square, [pures], ["sg_t"])
                    ts(S, "dve", sg_i[:, :], sg_t[:, :], 0.044715, ALU.mult, ["sg_t"], ["sg_i0"], s2=1.0, op1=ALU.add)
                    tt(S, "dve", sg_i[:, :], sg_i[:, :], pu[:, :], ALU.mult, ["sg_i0", pures], ["sg_i"])
                    act(S, sg_t[:, :], sg_i[:, :], AF.Sigmoid, ["sg_i"], ["sg_t2"], scale=1.5957691216057308)
                    tt(S, "dve", sg_g[:, :], sg_t[:, :], pu[:, :], ALU.mult, ["sg_t2", pures], ["sg_g"])
                    S.add("dve", lambda e: e.reduce_sum(out=sg_s[:, 0:1], in_=sg_g[:, 256:512], axis=mybir.AxisListType.X),
                          reads=["sg_g"], writes=["sg_s0"])
                    ts(S, "dve", sg_s[:, 1:2], sg_s[:, 0:1], -1.0 / 256, ALU.mult, ["sg_s0"], ["sg_s1"])
                    ts(S, "dve", sg_vc[:, :], sg_g[:, 256:512], sg_s[:, 1:2], ALU.add, ["sg_g", "sg_s1"], ["sg_vc"])
                    act(S, sg_junk[:, :], sg_vc[:, :], AF.Square, ["sg_vc"], ["sg_junk"], accum_out=sg_s[:, 2:3])
                    act(S, sg_s[:, 3:4], sg_s[:, 2:3], AF.Sqrt, ["sg_junk"], ["sg_s3"], scale=1.0 / 256, bias=EPS)
                    S.add("dve", lambda e: e.reciprocal(out=sg_s[:, 4:5], in_=sg_s[:, 3:4]), reads=["sg_s3"], writes=["sg_s4"])
                    stt(S, sg_vn[:, :], sg_vc[:, :], sg_s[:, 4:5], lng[:, :], ALU.mult, ALU.mult, ["sg_vc", "sg_s4", "bc_lng"], ["sg_vn0"])
                    tt(S, "dve", sg_vn[:, :], sg_vn[:, :], lnb[:, :], ALU.add, ["sg_vn0", "bc_lnb"], ["sg_vn"])
                    pt, pres = psr.next()
                    for g in range(4):
                        mm(S, pt[:, g * 64:(g + 1) * 64], WmT[:, g, :], sg_vn[:, g * 64:(g + 1) * 64], True, True, ["WmT", "sg_vn"], [pres])
                    for g in range(4):
                        stt(S, sg_o[:, g * 64:(g + 1) * 64], pt[:, g * 64:(g + 1) * 64], sgb[:, g:g + 1], sg_g[:, g * 64:(g + 1) * 64],
                            ALU.add, ALU.mult, [pres, "sgb", "sg_g"], ["sg_o"])
                    pt, pres = psr.next()
                    for c in range(2):
                        tr(S, pt[:, c * 128:(c + 1) * 128], sg_o[:, c * 128:(c + 1) * 128], ident[:, :], ["sg_o", "ident"], [pres])
                    cp(S, "act", obT[:, :, ssl], pt[:, :256].rearrange("p (c t) -> p c t", c=2), [pres], ["obT"])
                if "gla" in do:
                    pv, pvres = psr.next()
                    tproj(pv, pvres, s, 1184, 256, 0)
                    tproj(pv, pvres, s, 1456, 256, 256)
                    cp(S, "act", g_v[:, :], pv[:, 0:256], [pvres], ["g_v"])
                    act(S, g_og[:, :], pv[:, 256:512], AF.Silu, [pvres], ["g_og0"])
                    tt(S, "pool", g_og[:, :], g_og[:, :], gnb[:, :], ALU.mult, ["g_og0"] + [f"bc_gn{h}" for h in range(4)], ["g_og"])
                    pt, pres = psr.next()
                    tr(S, pt[:, :128], g_kl[:, ssl], ident[:, :], [("g_kl", s), "ident"], [pres])
                    ts(S, "dve", g_klA[:, :], pt[:, :128], topbot[:, 0:1], ALU.mult, [pres, "topbot"], ["g_klA"])
                    ts(S, "dve", g_klB[:, :], pt[:, :128], topbot[:, 1:2], ALU.mult, [pres, "topbot"], ["g_klB"])
                    scs = []
                    for h in range(4):
                        pt, pres = psr.next()
                        mm(S, pt[:, :128], g_ke[:, ssl], g_qp[:, h, ssl], True, True, ["g_ke", ("g_qp", h)], [pres])
                        sct, scr = g_sc.next()
                        tt(S, "dve", sct[:, :], pt[:, :128], mg[:, :], ALU.mult, [pres, "mg"], [scr])
                        scs.append((sct, scr))
                    po, pores = psr.next()
                    mm(S, po[:, :256], g_qA[:, ssl], g_S[:, :], True, False, ["g_qA", "g_S"], [pores])
                    for (klt, klr, isB) in ((g_klA, "g_klA", False), (g_klB, "g_klB", True)):
                        if isB:
                            mm(S, po[:, :256], g_qB[:, ssl], g_S[:, :], False, False, ["g_qB", "g_S"], [pores])
                        pt, pres = psr.next()
                        mm(S, pt[:, :256], klt[:, :], g_v[:, :], True, True, [klr, "g_v"], [pres])
                        tt(S, "dve", g_tmp[:, :], pt[:, :256], hm[:, :], ALU.mult, [pres, "hm"], ["g_tmp"])
                        cidx = s * 2 + (1 if isB else 0)
                        stt(S, g_S[:, :], g_S[:, :], g_ec[:, cidx * 64 + 63:cidx * 64 + 64], g_tmp[:, :], ALU.mult, ALU.add,
                            ["g_S", "g_ec", "g_tmp"], ["g_S"])
                    for h in range(4):
                        mm(S, po[:, h * 64:(h + 1) * 64], scs[h][0][:, :], g_v[:, h * 64:(h + 1) * 64], False, h == 3, [scs[h][1], "g_v"], [pores])
                    post_norm_gate(po, pores, g_og, "g_og", g_ss, g_junk, g_o, "g_", ocT, s)
                if "dn" in do and DNSTAGE >= 1:
                    pz, pzres = psr.next()
                    tproj(pz, pzres, s, 2488, 256, 0)
                    act(S, d_z[:, :], pz[:, 0:256], AF.Silu, [pzres], ["d_z0"])
                    tt(S, "pool", d_z[:, :], d_z[:, :], dnb[:, :], ALU.mult, ["d_z0"] + [f"bc_dnn{h}" for h in range(4)], ["d_z"])
                    pt, pres = psr.next()
                    mm(S, pt[:, 0:4], d_gam[:, ssl], ident[0:4, 0:4], True, True, ["d_gam", "ident"], [pres])
                    mm(S, pt[:, 4:8], d_b[:, ssl], ident[0:4, 0:4], True, True, ["d_b", "ident"], [pres])
                    mm(S, pt[:, 8:12], d_lg[:, ssl], ident[0:4, 0:4], True, True, ["d_lg", "ident"], [pres])
                    cp(S, "act", d_cols[:, :], pt[:, 0:12], [pres], ["d_cols"])
                    for h in range(4 if DNSTAGE >= 1.2 else 0):
                        kTh = d_y[:, 4 + h, ssl]
                        qTh = d_y[:, h, ssl]
                        vTh = d_y[:, 8 + h, ssl]
                        pt, pres = psr.next()
                        mm(S, pt[:, 0:64], kTh, ident[0:64, 0:64], True, True, [("d_y", 4 + h), "ident"], [pres])
                        mm(S, pt[:, 64:128], vTh, ident[0:64, 0:64], True, True, [("d_y", 8 + h), "ident"], [pres])
                        ts(S, "dve", d_Vp[:, h * 64:(h + 1) * 64], pt[:, 64:128], d_cols[:, 4 + h:5 + h], ALU.mult, [pres, "d_cols"], [("d_Vp", h)])
                        ts(S, "dve", d_junk[:, :], pt[:, 0:64], d_cols[:, 8 + h:9 + h], ALU.mult, [pres, "d_cols"], ["d_kdf"])
                        ts(S, "pool", d_kdA[:, h, :], d_junk[:, :], topbot[:, 0:1], ALU.mult, ["d_kdf", "topbot"], [("d_kdA", h)])
                        ts(S, "pool", d_kdB[:, h, :], d_junk[:, :], topbot[:, 1:2], ALU.mult, ["d_kdf", "topbot"], [("d_kdB", h)])
                        if DNSTAGE < 1.3:
                            continue
                        pkk, pkkres = psr.next()
                        mm(S, pkk[:, 0:128], kTh, kTh, True, True, [("d_y", 4 + h)], [pkkres])
                        mm(S, pkk[:, 128:256], kTh, qTh, True, True, [("d_y", 4 + h), ("d_y", h)], [pkkres])
                        d1, d1r = d_d1.next()
                        stt(S, d1[:, :], d_gbc[:, h, ssl], d_cols[:, h:h + 1], cU[:, :], ALU.subtract, ALU.max, [("d_gbc", h), "d_cols", "cU"], [d1r])
                        Dm, Dmr = d_Dm.next()
                        act(S, Dm[:, :], d1[:, :], AF.Exp, [d1r], [Dmr], scale=-1.0)
                        Bp, Bt, Rr = d_Bp[h], d_Bt[h], d_R[h]
                        stt(S, Bp[:, 0, :], pkk[:, 0:128], d_cols[:, 4 + h:5 + h], Dm[:, :], ALU.mult, ALU.mult, [pkkres, "d_cols", Dmr], [("Bp", h, 0)])
                        tt(S, "pool", Bp[:, 0, :], Bp[:, 0, :], cst[:, :], ALU.mult, [("Bp", h, 0), "cst"], [("Bp", h, 0)])
                        d2, d2r = d_d1.next()
                        stt(S, d2[:, :], d_gbc[:, h, ssl], d_cols[:, h:h + 1], cL[:, :], ALU.subtract, ALU.min, [("d_gbc", h), "d_cols", "cL"], [d2r])
                        DmT, DmTr = d_Dm.next()
                        act(S, DmT[:, :], d2[:, :], AF.Exp, [d2r], [DmTr])
                        tt(S, "dve", d_at[:, h, :], pkk[:, 128:256], DmT[:, :], ALU.mult, [pkkres, DmTr], [("d_at", h)])
                    ptAT, ptATres = psr.next()
                    for h in range(4):
                        mm(S, ptAT[:, h * 128:(h + 1) * 128], d_Bp[h][:, 0, :], ident[:, :], True, True, [("Bp", h, 0), "ident"], [ptATres])
                    cp(S, "act", d_BtT[:, 0, :, :].rearrange("p h c -> p (h c)"), ptAT[:, :512], [ptATres], [("Bt", h, 0) for h in range(4)])
                    for h in range(4):
                        tt(S, "pool", d_R[h][:, :], ident[:, :], d_Bt[h][:, 0, :], ALU.subtract, [("Bt", h, 0), "ident"], [("R", h)])
                    for lev in range(1, 6 if DNSTAGE >= 2 else 1):
                        a, b = (lev - 1) % 2, lev % 2
                        ptA, ptAres = psr.next()
                        if lev < 5:
                            ptT, ptTres = psr.next()
                        for h in range(4):
                            mm(S, ptA[:, h * 128:(h + 1) * 128], d_Bt[h][:, a, :], d_Bp[h][:, a, :], True, True, [("Bt", h, a), ("Bp", h, a)], [ptAres])
                            if lev < 5:
                                mm(S, ptT[:, h * 128:(h + 1) * 128], d_Bp[h][:, a, :], d_Bt[h][:, a, :], True, True, [("Bt", h, a), ("Bp", h, a)], [ptTres])
                        cp(S, "act", d_BpT[:, b, :, :].rearrange("p h c -> p (h c)"), ptA[:, :512], [ptAres], [("Bp", h, b) for h in range(4)])
                        if lev < 5:
                            cp(S, "dve", d_BtT[:, b, :, :].rearrange("p h c -> p (h c)"), ptT[:, :512], [ptTres], [("Bt", h, b) for h in range(4)])
                        pt2, pres2 = psr.next()
                        for h in range(4):
                            mm(S, pt2[:, h * 128:(h + 1) * 128], d_Bp[h][:, b, :], d_R[h][:, :], True, True, [("Bp", h, b), ("R", h)], [pres2])
                        tt(S, "dve", d_RT[:, :, :].rearrange("p h c -> p (h c)"), d_RT[:, :, :].rearrange("p h c -> p (h c)"), pt2[:, :512], ALU.add,
                           [pres2] + [("R", h) for h in range(4)], [("R", h) for h in range(4)])
                    if DNSTAGE < 1.5:
                        continue
                    po, pores = psr.next()
                    mm(S, po[:, :256], zrow[0:1, 0:128], zrow[0:1, :], True, False, ["zrow"], [pores])
                    for ci, (kd, kdn, qd, qdn) in enumerate(((d_kdA, "d_kdA", d_qA, "d_qA"), (d_kdB, "d_kdB", d_qB, "d_qB")) if DNSTAGE >= 3 else ()):
                        rows = slice(ci * 64, (ci + 1) * 64)
                        pks, pksres = psr.next()
                        for h in range(4):
                            mm(S, pks[:, h * 64:(h + 1) * 64], d_kp[:, h, ssl], d_S[:, h, :], True, True, [("d_kp", h), "d_S"], [pksres])
                            mm(S, po[:, h * 64:(h + 1) * 64], qd[:, h, ssl], d_S[:, h, :], False, False, [(qdn, h), "d_S"], [pores])
                        tt(S, "dve", d_rhs2[:, :], d_Vp[:, :], pks[:, :256], ALU.subtract, [pksres] + [("d_Vp", h) for h in range(4)], ["d_rhs2"])
                        pu, pures = psr.next()
                        for h in range(4):
                            mm(S, pu[:, h * 64:(h + 1) * 64], d_R[h][:, :], d_rhs2[:, h * 64:(h + 1) * 64], True, True, [("R", h), "d_rhs2"], [pures])
                        cp(S, "act", d_u[rows, :], pu[rows, :256], [pures], ["d_u"])
                        psu, psures = psr.next()
                        for h in range(4):
                            mm(S, psu[:64, h * 64:(h + 1) * 64], kd[:, h, :], d_u[:, h * 64:(h + 1) * 64], True, True, [(kdn, h), "d_u"], [psures])
                        cidx = s * 2 + ci
                        for h in range(4):
                            stt(S, d_S[:, h, :], d_S[:, h, :], d_elb[:, h, cidx:cidx + 1], psu[:64, h * 64:(h + 1) * 64], ALU.mult, ALU.add,
                                ["d_S", "d_elb", psures], ["d_S"])
                    for h in range(4):
                        mm(S, po[:, h * 64:(h + 1) * 64], d_at[:, h, :], d_u[:, h * 64:(h + 1) * 64], False, h == 3, [("d_at", h), "d_u"], [pores])
                    post_norm_gate(po, pores, d_z, "d_z", d_ss, d_junk2, d_o, "d_", odT, s)
            if "sg" in do:
                dma(S, "sp", sc["oT_s"][0].rearrange("(c p) n -> p c n", p=128)[:, :, tok], obT[:, :, :], ["obT"], [("oT_s", 0, t)])
            if "gla" in do:
                dma(S, "sp", sc["oT_s"][1].rearrange("(c p) n -> p c n", p=128)[:, :, tok], ocT[:, :, :], ["g_oT"], [("oT_s", 1, t)])
            if "dn" in do:
                dma(S, "sp", sc["oT_s"][2].rearrange("(c p) n -> p c n", p=128)[:, :, tok], odT[:, :, :], ["d_oT"], [("oT_s", 2, t)])
    S.barrier()

PARAM_SHAPES = {
    "norm_mix": [NL, D], "w_in": [NL, D, INW], "mla_norm_q": [NL, 256], "mla_norm_kv": [NL, 128],
    "mla_w_uq": [NL, 256, 384], "mla_w_ukv": [NL, 128, 512], "sg_ln_g": [NL, 256], "sg_ln_b": [NL, 256],
    "sg_w": [NL, 4, 128, 128], "sg_b": [NL, 4, 128], "gla_w_gate": [NL, 16, 128], "gla_b_gate": [NL, 128],
    "gla_norm": [NL, 64], "dn_conv": [NL, 4, 768], "dn_a_log": [NL, 4], "dn_dt_bias": [NL, 4], "dn_norm": [NL, 64],
    "w_branch": [NL, 4, 256, D], "w_out": [NL, D, D], "norm_xattn": [NL, D], "norm_mem": [NL, D],
    "xattn_wq": [NL, D, D], "xattn_wk": [NL, D, D], "xattn_wv": [NL, D, D], "xattn_wo": [NL, D, D],
    "norm_mlp": [NL, D], "w_up": [NL, D, HID], "w_down": [NL, HID, D], "norm_final": [D],
}


def make_consts():
    p = np.arange(128)
    same = (p[:, None] // 64) == (p[None, :] // 64)
    c = {}
    c["ident"] = np.eye(128, dtype=np.float32)
    c["trilT"] = (p[:, None] <= p[None, :]).astype(np.float32)
    t = np.arange(TT)
    c["rm"] = np.broadcast_to((t % 64 != 0).astype(np.float32), (128, TT)).copy()
    c["mg"] = (same & (p[:, None] <= p[None, :])).astype(np.float32)
    c["hm"] = ((p[:, None] // 32) == (np.arange(256)[None, :] // 64)).astype(np.float32)
    c["hcol"] = ((p[:, None] // 32) == np.arange(4)[None, :]).astype(np.float32)
    c["topbot"] = np.stack([(p < 64), (p >= 64)], axis=1).astype(np.float32)
    c["cmA"] = np.broadcast_to(((t % 128) < 64).astype(np.float32), (128, TT)).copy()
    c["U"] = np.where(same & (p[None, :] <= p[:, None]), 0.0, 80.0).astype(np.float32)
    c["L"] = np.where(same & (p[None, :] >= p[:, None]), 0.0, -80.0).astype(np.float32)
    c["strict"] = (same & (p[None, :] < p[:, None])).astype(np.float32)
    sel = np.zeros((4, 4, 128), np.float32)
    for h in range(4):
        sel[h, h, :] = 1.0
    c["sel4"] = sel
    cc = np.zeros((96, 2), np.float32)
    f = (10000.0 ** (-np.arange(0, 32, 2, dtype=np.float32) / 32)).astype(np.float32)
    cc[64:80, 0] = f
    cc[80:96, 0] = f
    cc[64:80, 1] = -1.0
    cc[80:96, 1] = 1.0
    c["cconst"] = cc
    return c


CONST_SHAPES = {k: list(v.shape) for k, v in make_consts().items()}


def build(nl=NL, ntiles=NT, phases=("rope", "A", "mla", "merge", "xattn", "mlp", "final"), dbg=(), doA=("mla", "sg", "gla", "dn")):
    nc = bass.Bass("TRN2", target_bir_lowering=False)

    def dt(name, shape, dtype, kind):
        return nc.dram_tensor(name, shape, dtype, kind=kind).ap()

    xT = dt("xT", [D, SEQ], F32, "ExternalInput")
    memT = dt("memT", [D, 256], F32, "ExternalInput")
    posrep = dt("posrep", [96, SEQ], I32, "ExternalInput")
    P = {k: dt(k, s, F32, "ExternalInput") for k, s in PARAM_SHAPES.items()}
    K = {k: dt("c_" + k, s, F32, "ExternalInput") for k, s in CONST_SHAPES.items()}
    outT = dt("outT", [D, SEQ], F32, "ExternalOutput")

    def scr(name, shape, dtype):
        return dt(name, shape, dtype, "ExternalOutput" if name in dbg else "Internal")

    sc = {
        "hT_s": scr("hT_s", [D, SEQ], BF16), "qT_s": scr("qT_s", [4, 96, SEQ], BF16), "kT_s": scr("kT_s", [4, 96, SEQ], BF16),
        "v_s": scr("v_s", [SEQ, 260], BF16), "oaT_s": scr("oaT_s", [4, 64, SEQ], BF16), "oT_s": scr("oT_s", [3, 256, SEQ], BF16),
        "cos_s": scr("cos_s", [96, SEQ], F32), "sin_s": scr("sin_s", [96, SEQ], F32),
        "wup_bf": scr("wup_bf", [NL, D, HID], BF16), "wdn_bf": scr("wdn_bf", [NL, HID, D], BF16), "wg_bf": scr("wg_bf", [NL, D, 4096], BF16),
        "xa": scr("xa", [D, SEQ], F32), "xb": scr("xb", [D, SEQ], F32), "xc": scr("xc", [D, SEQ], F32),
    }
    S = Sched(nc)
    phase_precast(S, nc, P, sc)
    if "rope" in phases:
        phase_rope(S, nc, posrep, K["cconst"], sc["cos_s"], sc["sin_s"])
    cur = xT
    for l in range(nl):
        if "A" in phases:
            phase_A(S, nc, l, cur, P, K, sc, ntiles=(None if ntiles == NT else 2 * ntiles), do=doA)
        if "mla" in phases:
            phase_mla(S, nc, sc["qT_s"], sc["kT_s"], sc["v_s"], sc["oaT_s"], K["trilT"], nq=ntiles)
        if "merge" in phases:
            phase_merge(S, nc, l, cur, sc["xa"], sc["hT_s"], sc["oaT_s"], sc["oT_s"], sc["wg_bf"], P["w_branch"], P["w_out"], ntiles=ntiles)
            cur = sc["xa"]
        if "xattn" in phases:
            phase_xattn(S, nc, l, cur, sc["xb"], memT, P["norm_xattn"], P["norm_mem"], P["xattn_wq"], P["xattn_wk"], P["xattn_wv"], P["xattn_wo"], ntiles=ntiles)
            cur = sc["xb"]
        if "mlp" in phases:
            phase_mlp(S, nc, l, cur, sc["xc"], P["norm_mlp"], sc["wup_bf"], sc["wdn_bf"], ntiles=ntiles)
            cur = sc["xc"]
    if "final" in phases:
        phase_final(S, nc, cur, outT, P["norm_final"], ntiles=ntiles)
    S.emit()
    return nc


def make_in_maps(inputs, cores):
    consts = make_consts()
    x = np.asarray(inputs["x"], dtype=np.float32)
    mem = np.asarray(inputs["mem"], dtype=np.float32)
    pos = np.asarray(inputs["positions"]).astype(np.int32)
    shared = {k: np.ascontiguousarray(np.asarray(inputs[k], dtype=np.float32)) for k in PARAM_SHAPES}
    for k, v in consts.items():
        shared["c_" + k] = v
    in_maps = []
    for b in cores:
        m = dict(shared)
        m["xT"] = np.ascontiguousarray(x[b].T)
        m["memT"] = np.ascontiguousarray(mem[b].T)
        m["posrep"] = np.ascontiguousarray(np.broadcast_to(pos[b][None, :], (96, SEQ)))
        in_maps.append(m)
    return in_maps


def kernel(**inputs):
    B = np.asarray(inputs["x"]).shape[0]
    nc = build()
    in_maps = make_in_maps(inputs, list(range(B)))
    res = run_bass_kernel_spmd(nc, in_maps, core_ids=list(range(B)))
    out = np.stack([np.ascontiguousarray(np.asarray(r["outT"]).T) for r in res.results], axis=0)
    return out.astype(np.float32)
```
